# Optimizing a Trainium2 kernel written in Bass

```python
import math
import jax, jax.numpy as jnp
from jax import lax
import numpy as np

D_MODEL = 1024
BATCH = 8
SEQ = 4096
DEPTH = 2

CHUNK = 64
N_EVEN = (DEPTH + 1) // 2
N_ODD = DEPTH // 2
A_WIDTH = D_MODEL // 2
A_KEY = 128
A_HEADS = A_WIDTH // A_KEY
A_VAL = A_WIDTH // A_HEADS
B_WIDTH = D_MODEL // 2
B_HEAD = 64
B_HEADS = B_WIDTH // B_HEAD
B_DECAY_LORA = 32
B_AAA_LORA = 32
B_GATE_LORA = 96
B_SHIFT_COLS = 3 * B_WIDTH + B_DECAY_LORA + B_AAA_LORA + B_GATE_LORA
EVEN_IN = 4 * A_WIDTH + B_SHIFT_COLS
C_WIDTH = D_MODEL // 4
C_GROUP = 16
C_GROUPS = C_WIDTH // C_GROUP
C_STATE = 64
D_WIDTH = D_MODEL - C_WIDTH
D_HEADS = 4
D_HEAD = D_WIDTH // D_HEADS
D_CONV = 4
ODD_IN = C_WIDTH + 4 * D_WIDTH + 2 * D_HEADS
FFN_HIDDEN = ((8 * D_MODEL // 3 + 255) // 256) * 256

RMS_EPS = 1e-6
GN_EPS = 64e-5
M_INIT = -1e30
F32 = jnp.float32

kernel_name = "hybrid_hgrn2_rwkv7_s5_mlstm_trunk"


def rms_norm(x, g):
    xf = x.astype(F32)
    y = xf * lax.rsqrt(jnp.mean(xf * xf, axis=-1, keepdims=True) + RMS_EPS)
    return (y * g.astype(F32)).astype(x.dtype)


def head_rms_norm(y, g, n_heads):
    bn, s, w = y.shape
    yh = y.reshape(bn, s, n_heads, w // n_heads)
    yh = yh * lax.rsqrt(jnp.mean(yh * yh, axis=-1, keepdims=True) + RMS_EPS)
    return yh.reshape(bn, s, w) * g.astype(F32)


def split_cols(u, sizes):
    return jnp.split(u, np.cumsum(sizes)[:-1].tolist(), axis=-1)


def token_shift(u):
    return jnp.pad(u, ((0, 0), (1, 0), (0, 0)))[:, :-1]


def causal_depthwise_conv(u, w):
    k, c = w.shape
    return lax.conv_general_dilated(
        u, w[:, None, :].astype(u.dtype), window_strides=(1,), padding=[(k - 1, 0)],
        dimension_numbers=("NWC", "WIO", "NWC"), feature_group_count=c)


def to_chunks(t):
    bn, s, h, d = t.shape
    return t.reshape(bn, s // CHUNK, CHUNK, h, d).transpose(1, 0, 3, 2, 4)


def from_chunks(t):
    nc, bn, h, l, d = t.shape
    return t.transpose(1, 0, 3, 2, 4).reshape(bn, nc * l, h, d)


def gate_chunks(t):
    bn, s, h = t.shape
    return t.reshape(bn, s // CHUNK, CHUNK, h).transpose(1, 0, 3, 2)


def hgrn2_mix(q, f_pre, i, g, lb, gain):
    bn, s, _ = q.shape
    q = jax.nn.silu(q.astype(F32))
    lb = lb.astype(F32)
    f = lb + (1.0 - lb) * jax.nn.sigmoid(f_pre.astype(F32))
    logf = jnp.log(f)
    k = 1.0 - f
    heads = lambda t: to_chunks(t.reshape(bn, s, A_HEADS, -1))
    mask = jnp.tril(jnp.ones((CHUNK, CHUNK), dtype=bool))[:, :, None]

    def chunk_step(state, inp):
        qc, kc, lfc, vc = inp
        b = jnp.cumsum(lfc, axis=2)
        diff = b[:, :, :, None, :] - b[:, :, None, :, :]
        decay = jnp.exp(jnp.where(mask, diff, -jnp.inf))
        att = jnp.einsum("bhtk,bhsk,bhtsk->bhts", qc, kc, decay)
        o = (jnp.einsum("bhtk,bhkv->bhtv", qc * jnp.exp(b), state)
             + jnp.einsum("bhts,bhsv->bhtv", att, vc))
        b_last = b[:, :, -1:, :]
        state = (jnp.exp(b_last[:, :, 0, :])[..., None] * state
                 + jnp.einsum("bhsk,bhsv->bhkv", kc * jnp.exp(b_last - b), vc))
        return state, o

    state0 = jnp.zeros((bn, A_HEADS, A_KEY, A_VAL), F32)
    _, o = lax.scan(chunk_step, state0,
                    (heads(q), heads(k), heads(logf), heads(i.astype(F32))))
    o = from_chunks(o).reshape(bn, s, A_WIDTH)
    return head_rms_norm(o, gain, A_HEADS) * jax.nn.silu(g.astype(F32))


def rwkv7_mix(u, mu, w0, w2, a0, a2, g2, k_k, k_a, r_k, ln_w, ln_b):
    bn, s, _ = u.shape
    u = u.astype(F32)
    u = u + (token_shift(u) - u) * mu.astype(F32)
    r, k, v, wd, ad, gd = split_cols(
        u, (B_WIDTH, B_WIDTH, B_WIDTH, B_DECAY_LORA, B_AAA_LORA, B_GATE_LORA))
    w_log = -jax.nn.softplus(-(w0 + jnp.tanh(wd) @ w2)) - 0.5
    decay = jnp.exp(-jnp.exp(w_log))
    a = jax.nn.sigmoid(a0 + ad @ a2)
    g = jax.nn.sigmoid(gd) @ g2
    heads = lambda t: t.reshape(bn, s, B_HEADS, B_HEAD)
    kk = heads(k * k_k)
    kk = kk / jnp.maximum(jnp.sqrt(jnp.sum(kk * kk, axis=-1, keepdims=True)), 1e-12)
    k = heads(k * (1.0 + (a - 1.0) * k_a))
    r, v, decay, a = heads(r), heads(v), heads(decay), heads(a)

    def step(state, inp):
        r_t, w_t, k_t, v_t, a_t, b_t = inp
        sa = jnp.einsum("bhvk,bhk->bhv", state, a_t)
        state = (state * w_t[:, :, None, :] + sa[..., None] * b_t[:, :, None, :]
                 + v_t[..., None] * k_t[:, :, None, :])
        return state, jnp.einsum("bhvk,bhk->bhv", state, r_t)

    tm = lambda t: t.transpose(1, 0, 2, 3)
    state0 = jnp.zeros((bn, B_HEADS, B_HEAD, B_HEAD), F32)
    _, y = lax.scan(step, state0,
                    (tm(r), tm(decay), tm(k), tm(v), tm(-kk), tm(kk * a)))
    y = y.transpose(1, 0, 2, 3)
    mean = jnp.mean(y, axis=-1, keepdims=True)
    var = jnp.mean(jnp.square(y - mean), axis=-1, keepdims=True)
    y = ((y - mean) * lax.rsqrt(var + GN_EPS) * ln_w.astype(F32).reshape(B_HEADS, B_HEAD)
         + ln_b.astype(F32).reshape(B_HEADS, B_HEAD))
    y = y + jnp.sum(r * k * r_k.astype(F32).reshape(B_HEADS, B_HEAD), axis=-1, keepdims=True) * v
    return y.reshape(bn, s, B_WIDTH) * g


def _complex_affine_combine(e1, e2):
    a1r, a1i, b1r, b1i = e1
    a2r, a2i, b2r, b2i = e2
    return (a2r * a1r - a2i * a1i,
            a2r * a1i + a2i * a1r,
            a2r * b1r - a2i * b1i + b2r,
            a2r * b1i + a2i * b1r + b2i)


def s5_mix(u, lam_re, lam_im, log_step, b_re, b_im, c_re, c_im, d_skip, glu_w, glu_b):
    bn, s, _ = u.shape
    u = u.astype(F32)
    lam_re, lam_im = lam_re.astype(F32), lam_im.astype(F32)
    step = jnp.exp(log_step.astype(F32))[:, None]
    mag = jnp.exp(lam_re * step)
    lb_re, lb_im = mag * jnp.cos(lam_im * step), mag * jnp.sin(lam_im * step)
    den = lam_re * lam_re + lam_im * lam_im
    nr, ni = lb_re - 1.0, lb_im
    coef_re = (nr * lam_re + ni * lam_im) / den
    coef_im = (ni * lam_re - nr * lam_im) / den
    b_re, b_im = b_re.astype(F32), b_im.astype(F32)
    bb_re = coef_re[..., None] * b_re - coef_im[..., None] * b_im
    bb_im = coef_re[..., None] * b_im + coef_im[..., None] * b_re
    ug = u.reshape(bn, s, C_GROUPS, C_GROUP)
    xr = jnp.einsum("bsgh,gph->sbgp", ug, bb_re)
    xi = jnp.einsum("bsgh,gph->sbgp", ug, bb_im)
    a_re = jnp.broadcast_to(lb_re, (s, 1, C_GROUPS, C_STATE))
    a_im = jnp.broadcast_to(lb_im, (s, 1, C_GROUPS, C_STATE))
    _, _, hr, hi = lax.associative_scan(_complex_affine_combine, (a_re, a_im, xr, xi), axis=0)
    y = (jnp.einsum("sbgp,ghp->bsgh", hr, c_re.astype(F32))
         - jnp.einsum("sbgp,ghp->bsgh", hi, c_im.astype(F32)))
    y = y.reshape(bn, s, C_WIDTH) + d_skip.astype(F32) * u
    z = jax.nn.gelu(y)
    return z * jax.nn.sigmoid(z @ glu_w.astype(F32) + glu_b.astype(F32))


def mlstm_mix(q, k, v, o, i_pre, f_pre, conv_q, conv_k, gain):
    bn, s, _ = q.shape
    q = jax.nn.silu(causal_depthwise_conv(q.astype(F32), conv_q.astype(F32)))
    k = jax.nn.silu(causal_depthwise_conv(k.astype(F32), conv_k.astype(F32))) / math.sqrt(D_HEAD)
    heads = lambda t: to_chunks(t.reshape(bn, s, D_HEADS, D_HEAD))
    log_f = jax.nn.log_sigmoid(f_pre.astype(F32))
    log_i = i_pre.astype(F32)
    mask = jnp.tril(jnp.ones((CHUNK, CHUNK), dtype=bool))

    def chunk_step(carry, inp):
        c_st, n_st, m_st = carry
        qc, kc, vc, lfc, ic = inp
        b = jnp.cumsum(lfc, axis=-1)
        dmat = jnp.where(mask, b[..., :, None] - b[..., None, :] + ic[..., None, :], -jnp.inf)
        e_inter = b + m_st[..., None]
        m_t = jnp.maximum(e_inter, jnp.max(dmat, axis=-1))
        w_intra = jnp.exp(dmat - m_t[..., None])
        s_inter = jnp.exp(e_inter - m_t)
        qk = jnp.einsum("bhtd,bhsd->bhts", qc, kc) * w_intra
        num = (s_inter[..., None] * jnp.einsum("bhtd,bhdv->bhtv", qc, c_st)
               + jnp.einsum("bhts,bhsv->bhtv", qk, vc))
        den = s_inter * jnp.einsum("bhtd,bhd->bht", qc, n_st) + jnp.sum(qk, axis=-1)
        h = num / jnp.maximum(jnp.abs(den), jnp.exp(-m_t))[..., None]
        m_new = m_t[..., -1]
        w_last = jnp.exp(b[..., -1:] - b + ic - m_new[..., None])
        d_prev = jnp.exp(b[..., -1] + m_st - m_new)
        c_new = d_prev[..., None, None] * c_st + jnp.einsum("bhs,bhsd,bhsv->bhdv", w_last, kc, vc)
        n_new = d_prev[..., None] * n_st + jnp.einsum("bhs,bhsd->bhd", w_last, kc)
        return (c_new, n_new, m_new), h

    carry0 = (jnp.zeros((bn, D_HEADS, D_HEAD, D_HEAD), F32),
              jnp.zeros((bn, D_HEADS, D_HEAD), F32),
              jnp.full((bn, D_HEADS), M_INIT, F32))
    _, h = lax.scan(chunk_step, carry0,
                    (heads(q), heads(k), heads(v.astype(F32)), gate_chunks(log_f), gate_chunks(log_i)))
    h = from_chunks(h).reshape(bn, s, D_WIDTH)
    return head_rms_norm(h, gain, D_HEADS) * jax.nn.sigmoid(o.astype(F32))


def swiglu(h, w_gate, w_up, w_down):
    return (jax.nn.silu(h @ w_gate) * (h @ w_up)) @ w_down


def setup_inputs(seed: int = 0) -> dict:
    key = jax.random.key(seed)
    ks = iter(jax.random.split(key, 64))
    nrm = lambda shape, scale: jax.random.normal(next(ks), shape, F32) * scale
    d = D_MODEL
    lam_im0 = jnp.float32(np.pi) * jnp.arange(C_STATE, dtype=F32)
    return {
        "x": nrm((BATCH, SEQ, d), 1.0),
        "norm_mix": 1.0 + nrm((DEPTH, d), 0.02),
        "norm_ffn": 1.0 + nrm((DEPTH, d), 0.02),
        "norm_final": 1.0 + nrm((d,), 0.02),
        "w_in_even": nrm((N_EVEN, d, EVEN_IN), d ** -0.5),
        "w_out_even": nrm((N_EVEN, A_WIDTH + B_WIDTH, d), (A_WIDTH + B_WIDTH) ** -0.5),
        "lb_table": nrm((DEPTH + 1, A_WIDTH), 0.1),
        "a_norm": 1.0 + nrm((N_EVEN, A_WIDTH), 0.02),
        "b_mu": jax.random.uniform(next(ks), (N_EVEN, B_SHIFT_COLS), F32),
        "b_w0": jnp.linspace(-6.0, -1.0, B_WIDTH, dtype=F32) + nrm((N_EVEN, B_WIDTH), 0.1),
        "b_w2": nrm((N_EVEN, B_DECAY_LORA, B_WIDTH), 0.1 * B_DECAY_LORA ** -0.5),
        "b_a0": nrm((N_EVEN, B_WIDTH), 0.1),
        "b_a2": nrm((N_EVEN, B_AAA_LORA, B_WIDTH), 0.3 * B_AAA_LORA ** -0.5),
        "b_g2": nrm((N_EVEN, B_GATE_LORA, B_WIDTH), B_GATE_LORA ** -0.5),
        "b_kk": 0.85 + nrm((N_EVEN, B_WIDTH), 0.02),
        "b_ka": 1.0 + nrm((N_EVEN, B_WIDTH), 0.02),
        "b_rk": nrm((N_EVEN, B_WIDTH), 0.1),
        "b_ln_w": 1.0 + nrm((N_EVEN, B_WIDTH), 0.02),
        "b_ln_b": nrm((N_EVEN, B_WIDTH), 0.02),
        "w_in_odd": nrm((N_ODD, d, ODD_IN), d ** -0.5),
        "w_out_odd": nrm((N_ODD, C_WIDTH + D_WIDTH, d), (C_WIDTH + D_WIDTH) ** -0.5),
        "c_lam_re": -0.5 + nrm((N_ODD, C_GROUPS, C_STATE), 0.01),
        "c_lam_im": lam_im0 + nrm((N_ODD, C_GROUPS, C_STATE), 0.01),
        "c_log_step": jax.random.uniform(next(ks), (N_ODD, C_GROUPS), F32,
                                         minval=math.log(1e-3), maxval=math.log(1e-1)),
        "c_b_re": nrm((N_ODD, C_GROUPS, C_STATE, C_GROUP), (2 * C_GROUP) ** -0.5),
        "c_b_im": nrm((N_ODD, C_GROUPS, C_STATE, C_GROUP), (2 * C_GROUP) ** -0.5),
        "c_c_re": nrm((N_ODD, C_GROUPS, C_GROUP, C_STATE), C_STATE ** -0.5),
        "c_c_im": nrm((N_ODD, C_GROUPS, C_GROUP, C_STATE), C_STATE ** -0.5),
        "c_d": nrm((N_ODD, C_WIDTH), 1.0),
        "c_glu_w": nrm((N_ODD, C_WIDTH, C_WIDTH), C_WIDTH ** -0.5),
        "c_glu_b": nrm((N_ODD, C_WIDTH), 0.02),
        "d_conv_q": nrm((N_ODD, D_CONV, D_WIDTH), D_CONV ** -0.5),
        "d_conv_k": nrm((N_ODD, D_CONV, D_WIDTH), D_CONV ** -0.5),
        "d_i_bias": nrm((N_ODD, D_HEADS), 0.1),
        "d_f_bias": jnp.linspace(3.0, 6.0, D_HEADS, dtype=F32) + nrm((N_ODD, D_HEADS), 0.1),
        "d_norm": 1.0 + nrm((N_ODD, D_WIDTH), 0.02),
        "ffn_gate": nrm((DEPTH, d, FFN_HIDDEN), d ** -0.5),
        "ffn_up": nrm((DEPTH, d, FFN_HIDDEN), d ** -0.5),
        "ffn_down": nrm((DEPTH, FFN_HIDDEN, d), FFN_HIDDEN ** -0.5),
    }


def reference(x, norm_mix, norm_ffn, norm_final, w_in_even, w_out_even, lb_table, a_norm,
              b_mu, b_w0, b_w2, b_a0, b_a2, b_g2, b_kk, b_ka, b_rk, b_ln_w, b_ln_b,
              w_in_odd, w_out_odd, c_lam_re, c_lam_im, c_log_step, c_b_re, c_b_im,
              c_c_re, c_c_im, c_d, c_glu_w, c_glu_b, d_conv_q, d_conv_k, d_i_bias,
              d_f_bias, d_norm, ffn_gate, ffn_up, ffn_down):
    lower_bounds = jnp.cumsum(jax.nn.softmax(lb_table.astype(F32), axis=0), axis=0)
    for l in range(DEPTH):
        h = rms_norm(x, norm_mix[l])
        if l % 2 == 0:
            e = l // 2
            u = h @ w_in_even[e]
            aq, af, ai, ag, ub = split_cols(u, (A_WIDTH, A_WIDTH, A_WIDTH, A_WIDTH, B_SHIFT_COLS))
            ya = hgrn2_mix(aq, af, ai, ag, lower_bounds[l], a_norm[e])
            yb = rwkv7_mix(ub, b_mu[e], b_w0[e], b_w2[e], b_a0[e], b_a2[e], b_g2[e],
                           b_kk[e], b_ka[e], b_rk[e], b_ln_w[e], b_ln_b[e])
            y = jnp.concatenate([ya, yb], axis=-1).astype(x.dtype) @ w_out_even[e]
        else:
            o_ = l // 2
            u = h @ w_in_odd[o_]
            uc, dq, dk, dv, do, di, df = split_cols(
                u, (C_WIDTH, D_WIDTH, D_WIDTH, D_WIDTH, D_WIDTH, D_HEADS, D_HEADS))
            yc = s5_mix(uc, c_lam_re[o_], c_lam_im[o_], c_log_step[o_], c_b_re[o_], c_b_im[o_],
                        c_c_re[o_], c_c_im[o_], c_d[o_], c_glu_w[o_], c_glu_b[o_])
            yd = mlstm_mix(dq, dk, dv, do, di + d_i_bias[o_], df + d_f_bias[o_],
                           d_conv_q[o_], d_conv_k[o_], d_norm[o_])
            y = jnp.concatenate([yc, yd], axis=-1).astype(x.dtype) @ w_out_odd[o_]
        x = x + y
        h = rms_norm(x, norm_ffn[l])
        x = x + swiglu(h, ffn_gate[l], ffn_up[l], ffn_down[l])
    return rms_norm(x, norm_final)
```

```python
from contextlib import ExitStack
import numpy as np
import concourse.bass as bass
import concourse.mybir as mybir
from concourse.bass_utils import run_bass_kernel_spmd

F32 = mybir.dt.float32
BF16 = mybir.dt.bfloat16
ALU = mybir.AluOpType
AF = mybir.ActivationFunctionType
AX = mybir.AxisListType

D = 1024
EVEN_IN = 3744
ODD_IN = 3336
FFN_H = 2816
RMS_EPS = 1e-6
GN_EPS = 64e-5

ENGS = ["tensor", "vector", "scalar", "gpsimd", "sync"]
SAME_ENGINE_SYNC = True
DMA_RING = 12


class KB:
    def __init__(self, nc, st):
        self.nc = nc
        self.sem = {}
        self.semh = []
        for e in ENGS:
            h = st.enter_context(nc.semaphore("s_" + e))
            self.sem[e] = len(self.semh)
            self.semh.append(h)
        self.ring = {}
        self.ring_val = {}
        self.ring_pos = {}
        for q in ["sync", "gpsimd", "scalar"]:
            ids = []
            for j in range(DMA_RING):
                h = st.enter_context(nc.semaphore("d_%s%d" % (q, j)))
                ids.append(len(self.semh))
                self.semh.append(h)
            self.ring[q] = ids
            self.ring_val[q] = [0] * DMA_RING
            self.ring_pos[q] = 0
        self.cnt = {e: 0 for e in ENGS}
        self.ops = {e: [] for e in ENGS}
        self.waited = {e: {} for e in ENGS}
        self.res = {}
        self.nops = 0

    def _wait(self, eng, s, v):
        if s == self.sem[eng]:
            if eng == "tensor" or not SAME_ENGINE_SYNC:
                return
        if self.waited[eng].get(s, 0) >= v:
            return
        self.waited[eng][s] = v
        h = self.semh[s]
        self.ops[eng].append(lambda e, h=h, v=v: e.wait_ge(h, v))

    def _deps(self, eng, reads, writes):
        for k in reads:
            r = self.res.get(k)
            if r:
                for s, v in r[0].items():
                    self._wait(eng, s, v)
        for k in writes:
            r = self.res.get(k)
            if r:
                for s, v in r[0].items():
                    self._wait(eng, s, v)
                for s, v in r[1].items():
                    self._wait(eng, s, v)

    def _mark(self, s, v, reads, writes):
        for k in reads:
            r = self.res.setdefault(k, ({}, {}))
            if r[1].get(s, 0) < v:
                r[1][s] = v
        for k in writes:
            self.res[k] = ({s: v}, {})

    @staticmethod
    def _excl(reads, writes):
        ps = [k for k in reads if isinstance(k, str) and k.startswith("ps")]
        if ps:
            reads = [k for k in reads if k not in ps]
            writes = list(writes) + ps
        return reads, writes

    def op(self, eng, fn, reads=(), writes=()):
        reads, writes = self._excl(reads, writes)
        self._deps(eng, reads, writes)
        self.cnt[eng] += 1
        s = self.sem[eng]
        h = self.semh[s]
        self.ops[eng].append(lambda e, fn=fn, h=h: fn(e).then_inc(h, 1))
        self._mark(s, self.cnt[eng], reads, writes)
        self.nops += 1

    def dma(self, q, out, in_, reads=(), writes=(), **kw):
        self._deps(q, reads, writes)
        j = self.ring_pos[q] % DMA_RING
        self.ring_pos[q] += 1
        s = self.ring[q][j]
        prev = self.ring_val[q][j]
        if prev:
            self._wait(q, s, prev)
        v = prev + 16
        self.ring_val[q][j] = v
        h = self.semh[s]
        self.ops[q].append(
            lambda e, h=h, out=out, in_=in_, kw=kw: e.dma_start(out=out, in_=in_, **kw).then_inc(h, 16))
        self._mark(s, v, reads, writes)
        self.nops += 1

    def flush(self):
        for e in ENGS:
            if e != "sync" and self.cnt[e]:
                self._wait("sync", self.sem[e], self.cnt[e])
        for q in self.ring:
            for j in range(DMA_RING):
                if self.ring_val[q][j]:
                    self._wait("sync", self.ring[q][j], self.ring_val[q][j])
        with self.nc.Block() as block:
            for e in ENGS:
                ops = self.ops[e]
                if not ops:
                    continue

                def body(eng, ops=ops):
                    for f in ops:
                        f(eng)
                getattr(block, e)(body)
        self.ops = {e: [] for e in ENGS}
        self.res = {}


_UID = [0]


def sb(st, nc, name, shape, dt):
    _UID[0] += 1
    return st.enter_context(nc.sbuf_tensor("%s_%d" % (name, _UID[0]), list(shape), dt))


class Prog:
    def __init__(self, nc, st, T):
        self.nc = nc
        self.T = T
        self.kb = KB(nc, st)
        self.uid = 0
        self.psf = [st.enter_context(nc.psum_tensor("psf%d" % i, [128, 512], F32)) for i in range(6)]
        self.psb = [st.enter_context(nc.psum_tensor("psb%d" % i, [128, 1024], BF16)) for i in range(2)]
        self.psf_i = 0
        self.psb_i = 0
        self.evac_i = 0
        self.ident = sb(st, nc, "ident", [128, 128], BF16)
        self.identf = sb(st, nc, "identf", [128, 128], F32)
        self.ones_f = sb(st, nc, "ones_f", [128, 128], F32)
        self.ones_b = sb(st, nc, "ones_b", [128, 128], BF16)
        kb = self.kb
        kb.op("gpsimd", lambda e: e.memset(self.ones_f[:], 1.0), writes=["ones_f"])
        kb.op("gpsimd", lambda e: e.memset(self.ones_b[:], 1.0), writes=["ones_b"])
        kb.op("gpsimd", lambda e: e.affine_select(
            out=self.identf[:], in_=self.ones_f[:], pattern=[[-1, 128]], compare_op=ALU.is_equal,
            fill=0.0, base=0, channel_multiplier=1), reads=["ones_f"], writes=["identf"])
        kb.op("gpsimd", lambda e: e.tensor_copy(out=self.ident[:], in_=self.identf[:]),
              reads=["identf"], writes=["ident"])

    def key(self, name):
        self.uid += 1
        return "%s#%d" % (name, self.uid)

    def next_psf(self):
        i = self.psf_i % len(self.psf)
        self.psf_i += 1
        return self.psf[i], "psf%d" % i

    def bank(self, i):
        return self.psf[i], "psf%d" % i

    def next_psb(self):
        i = self.psb_i % len(self.psb)
        self.psb_i += 1
        return self.psb[i], "psb%d" % i

    def evac_eng(self):
        self.evac_i += 1
        return "scalar" if self.evac_i % 2 else "vector"

    def copy(self, eng, out, in_, reads, writes):
        if eng == "scalar":
            self.kb.op("scalar", lambda e: e.copy(out=out, in_=in_), reads=reads, writes=writes)
        else:
            self.kb.op(eng, lambda e: e.tensor_copy(out=out, in_=in_), reads=reads, writes=writes)

    def norm_tile_T(self, xt, xk, g_bc, tmp, dstT, dst_keys, eps=RMS_EPS):
        kb = self.kb
        junk, ms, rstd, hb = tmp["junk"], tmp["ms"], tmp["rstd"], tmp["hb"]
        kb.op("vector", lambda e: e.scalar_tensor_tensor(
            out=junk[:], in0=xt, scalar=1.0, in1=xt, op0=ALU.mult, op1=ALU.mult, accum_out=ms[:]),
            reads=[xk], writes=["junk", "ms"])
        kb.op("scalar", lambda e: e.activation(out=rstd[:], in_=ms[:], func=AF.Sqrt, bias=tmp["epsc"][:], scale=1.0 / D),
              reads=["ms"], writes=["rstd"])
        kb.op("vector", lambda e: e.reciprocal(out=rstd[:], in_=rstd[:]), reads=["rstd"], writes=["rstd"])
        kb.op("vector", lambda e: e.scalar_tensor_tensor(
            out=hb[:], in0=xt, scalar=rstd[:], in1=g_bc[:], op0=ALU.mult, op1=ALU.mult),
            reads=[xk, "rstd", "g_bc"], writes=["hb"])
        self.transpose_tile(hb, "hb", 8, dstT, dst_keys)

    def transpose_tile(self, src, sk, nchunks, dstT, dst_keys):
        kb = self.kb
        ps, pk = self.next_psb()
        for kc in range(nchunks):
            kb.op("tensor", lambda e, kc=kc: e.transpose(
                out=ps[:, kc * 128:(kc + 1) * 128], in_=src[:, kc * 128:(kc + 1) * 128], identity=self.ident[:]),
                reads=[sk, "ident"], writes=[pk])
        eng = self.evac_eng()
        self.copy(eng, dstT, ps[:, 0:nchunks * 128].rearrange("p (a b) -> p a b", b=128), [pk], dst_keys)

    def norm_tmp(self, st, pfx=""):
        nc = self.nc
        tmp = {
            "junk": sb(st, nc, pfx + "junk", [128, D], BF16),
            "ms": sb(st, nc, pfx + "ms", [128, 1], F32),
            "rstd": sb(st, nc, pfx + "rstd", [128, 1], F32),
            "hb": sb(st, nc, pfx + "hb", [128, D], BF16),
            "epsc": sb(st, nc, pfx + "epsc", [128, 1], F32),
        }
        self.kb.op("gpsimd", lambda e: e.memset(tmp["epsc"][:], RMS_EPS), writes=["epsc"])
        return tmp

    def load_bcast(self, dst, row_ap, n, key):
        self.kb.dma("sync", dst, row_ap.partition_broadcast(128), writes=[key])

    def phase_proj(self, x_d, g_row, W_d, outs):
        nc, kb, T = self.nc, self.kb, self.T
        NT = T // 128
        TB = min(512, T)
        with ExitStack() as st:
            hT = sb(st, nc, "hT", [128, 8, T], BF16)
            xts = [sb(st, nc, "xt%d" % i, [128, D], F32) for i in range(2)]
            g_bc = sb(st, nc, "g_bc", [128, D], F32)
            tmp = self.norm_tmp(st)
            self.load_bcast(g_bc[:], g_row, D, "g_bc")
            kb.dma("sync", xts[0][:], x_d[0:128, :], writes=["xt0"])
            for i in range(NT):
                if i + 1 < NT:
                    kb.dma("sync", xts[(i + 1) % 2][:], x_d[(i + 1) * 128:(i + 2) * 128, :], writes=["xt%d" % ((i + 1) % 2)])
                self.norm_tile_T(xts[i % 2][:], "xt%d" % (i % 2), g_bc, tmp,
                                 hT[:, :, i * 128:(i + 1) * 128], [("hT", i)])
            hkeys = [("hT", i) for i in range(NT)]
            Wv = W_d.rearrange("(kc p) n -> p kc n", p=128)
            wts = [sb(st, nc, "wt%d" % i, [128, 8, 512], BF16) for i in range(2)]
            stg = [sb(st, nc, "stg%d" % i, [128, 512], F32) for i in range(4)]
            wi = 0
            si = 0
            for (c0, ncols, kind, dst) in outs:
                for g0 in range(0, ncols, 512):
                    gw = min(512, ncols - g0)
                    wt = wts[wi % 2]
                    wk = "wt%d" % (wi % 2)
                    wi += 1
                    kb.dma("gpsimd", wt[:, :, 0:gw], Wv[:, :, c0 + g0:c0 + g0 + gw], writes=[wk])
                    if kind == "fm":
                        for b0 in range(0, gw, 128):
                            M = min(128, gw - b0)
                            for tb in range(T // TB):
                                ps, pk = self.next_psf()
                                for kc in range(8):
                                    kb.op("tensor", lambda e, kc=kc, ps=ps, wt=wt, b0=b0, M=M, tb=tb: e.matmul(
                                        out=ps[0:M, 0:TB], lhsT=wt[:, kc, b0:b0 + M], rhs=hT[:, kc, tb * TB:(tb + 1) * TB],
                                        start=(kc == 0), stop=(kc == 7)),
                                        reads=[wk] + hkeys[tb * TB // 128:(tb + 1) * TB // 128], writes=[pk])
                                sg = stg[si % 4]
                                sk = "stg%d" % (si % 4)
                                si += 1
                                self.copy(self.evac_eng(), sg[0:M, 0:TB], ps[0:M, 0:TB], [pk], [sk])
                                kb.dma("sync", dst[g0 + b0:g0 + b0 + M, tb * TB:(tb + 1) * TB], sg[0:M, 0:TB], reads=[sk])
                    else:
                        for i in range(NT):
                            ps, pk = self.next_psf()
                            for kc in range(8):
                                kb.op("tensor", lambda e, kc=kc, ps=ps, wt=wt, i=i, gw=gw: e.matmul(
                                    out=ps[:, 0:gw], lhsT=hT[:, kc, i * 128:(i + 1) * 128], rhs=wt[:, kc, 0:gw],
                                    start=(kc == 0), stop=(kc == 7)),
                                    reads=[wk, hkeys[i]], writes=[pk])
                            sg = stg[si % 4]
                            sk = "stg%d" % (si % 4)
                            si += 1
                            self.copy(self.evac_eng(), sg[:, 0:gw], ps[:, 0:gw], [pk], [sk])
                            kb.dma("sync", dst[i * 128:(i + 1) * 128, g0:g0 + gw], sg[:, 0:gw], reads=[sk])
            kb.flush()

    def phase_outproj(self, x_d, srcs, W_d, xo_d):
        nc, kb, T = self.nc, self.kb, self.T
        NT = T // 128
        with ExitStack() as st:
            Wt = sb(st, nc, "Wo", [128, 8, D], BF16)
            Wv = W_d.rearrange("(kc p) n -> p kc n", p=128)
            kb.dma("gpsimd", Wt[:, :, 0:512], Wv[:, :, 0:512], writes=["Wo0"])
            kb.dma("gpsimd", Wt[:, :, 512:1024], Wv[:, :, 512:1024], writes=["Wo1"])
            xts = [sb(st, nc, "xt%d" % i, [128, D], F32) for i in range(2)]
            yts = [sb(st, nc, "yt%d" % i, [128, D], BF16) for i in range(2)]
            yTs = [sb(st, nc, "yT%d" % i, [128, 8, 128], BF16) for i in range(2)]
            stg = [sb(st, nc, "stg%d" % i, [128, D], F32) for i in range(2)]

            def load(i):
                b = i % 2
                kb.dma("sync", xts[b][:], x_d[i * 128:(i + 1) * 128, :], writes=["xt%d" % b])
                for (kind, ap, kc0, nkc) in srcs:
                    if kind == "tm":
                        kb.dma("sync", yts[b][:, kc0 * 128:(kc0 + nkc) * 128], ap[i * 128:(i + 1) * 128, :],
                               writes=[("yt", b, kc0)])
                    else:
                        kb.dma("sync", yTs[b][:, kc0:kc0 + nkc, :],
                               ap.rearrange("(c p) t -> p c t", p=128)[:, :, i * 128:(i + 1) * 128],
                               writes=[("yT", b, kc0)])
            load(0)
            for i in range(NT):
                b = i % 2
                if i + 1 < NT:
                    load(i + 1)
                ykeys = []
                for (kind, ap, kc0, nkc) in srcs:
                    if kind == "tm":
                        self.transpose_tile(yts[b][:, kc0 * 128:(kc0 + nkc) * 128], ("yt", b, kc0), nkc,
                                            yTs[b][:, kc0:kc0 + nkc, :], [("yT", b, kc0)])
                    ykeys.append(("yT", b, kc0))
                for c in range(2):
                    ps, pk = self.next_psf()
                    for kc in range(8):
                        kb.op("tensor", lambda e, kc=kc, ps=ps, b=b, c=c: e.matmul(
                            out=ps[:, :], lhsT=yTs[b][:, kc, :], rhs=Wt[:, kc, c * 512:(c + 1) * 512],
                            start=(kc == 0), stop=(kc == 7)), reads=ykeys + ["Wo%d" % c], writes=[pk])
                    kb.op("vector", lambda e, ps=ps, b=b, c=c: e.tensor_tensor(
                        out=stg[b][:, c * 512:(c + 1) * 512], in0=ps[:, :], in1=xts[b][:, c * 512:(c + 1) * 512], op=ALU.add),
                        reads=[pk, "xt%d" % b], writes=[("stg", b, c)])
                kb.dma("sync", xo_d[i * 128:(i + 1) * 128, :], stg[b][:], reads=[("stg", b, 0), ("stg", b, 1)])
            kb.flush()

    def phase_ffn(self, x_d, g_row, Wg_d, Wu_d, Wd_d, xo_d, gfin_row=None):
        nc, kb, T = self.nc, self.kb, self.T
        TB = min(512, T)
        NTB = TB // 128
        NJ = FFN_H // 128
        with ExitStack() as st:
            Wd = sb(st, nc, "Wd", [128, NJ, D], BF16)
            Wdv = Wd_d.rearrange("(j p) n -> p j n", p=128)
            def load_wd():
                for j0 in range(0, NJ, 4):
                    j1 = min(NJ, j0 + 4)
                    kb.dma("gpsimd", Wd[:, j0:j1, :], Wdv[:, j0:j1, :], writes=[("Wd", j0 // 4)])
            wdkeys = [("Wd", j) for j in range((NJ + 3) // 4)]
            Wgv = Wg_d.rearrange("(kc p) n -> p kc n", p=128)
            Wuv = Wu_d.rearrange("(kc p) n -> p kc n", p=128)
            wgs = [sb(st, nc, "wg%d" % i, [128, 8, 512], BF16) for i in range(2)]
            wus = [sb(st, nc, "wu%d" % i, [128, 8, 512], BF16) for i in range(2)]
            xb = sb(st, nc, "xb", [128, NTB, D], F32)
            hT = sb(st, nc, "hT", [128, 8, TB], BF16)
            aT = sb(st, nc, "aT", [128, NJ, TB], BF16)
            sg = [sb(st, nc, "sg%d" % i, [128, TB], F32) for i in range(2)]
            stg = [sb(st, nc, "stg%d" % i, [128, D], F32) for i in range(2)]
            g_bc = sb(st, nc, "g_bc", [128, D], F32)
            tmp = self.norm_tmp(st)
            self.load_bcast(g_bc[:], g_row, D, "g_bc")
            if gfin_row is not None:
                gf_bc = sb(st, nc, "gf_bc", [128, D], F32)
                self.load_bcast(gf_bc[:], gfin_row, D, "gf_bc")
            wi = 0
            si = 0
            for tb in range(T // TB):
                for i in range(NTB):
                    r0 = tb * TB + i * 128
                    kb.dma("sync", xb[:, i, :], x_d[r0:r0 + 128, :], writes=[("xb", i)])
                for i in range(NTB):
                    self.norm_tile_T(xb[:, i, :], ("xb", i), g_bc, tmp, hT[:, :, i * 128:(i + 1) * 128], [("hT", i)])
                hkeys = [("hT", i) for i in range(NTB)]
                for g0 in range(0, FFN_H, 512):
                    gw = min(512, FFN_H - g0)
                    b = wi % 2
                    wi += 1
                    kb.dma("gpsimd", wgs[b][:, :, 0:gw], Wgv[:, :, g0:g0 + gw], writes=["wg%d" % b])
                    kb.dma("gpsimd", wus[b][:, :, 0:gw], Wuv[:, :, g0:g0 + gw], writes=["wu%d" % b])
                    if tb == 0 and g0 == 512:
                        load_wd()
                    for b0 in range(0, gw, 128):
                        j = (g0 + b0) // 128
                        psg, pkg = self.next_psf()
                        for kc in range(8):
                            kb.op("tensor", lambda e, kc=kc, ps=psg, b=b, b0=b0: e.matmul(
                                out=ps[:, 0:TB], lhsT=wgs[b][:, kc, b0:b0 + 128], rhs=hT[:, kc, :],
                                start=(kc == 0), stop=(kc == 7)), reads=["wg%d" % b] + hkeys, writes=[pkg])
                        psu, pku = self.next_psf()
                        for kc in range(8):
                            kb.op("tensor", lambda e, kc=kc, ps=psu, b=b, b0=b0: e.matmul(
                                out=ps[:, 0:TB], lhsT=wus[b][:, kc, b0:b0 + 128], rhs=hT[:, kc, :],
                                start=(kc == 0), stop=(kc == 7)), reads=["wu%d" % b] + hkeys, writes=[pku])
                        s = sg[si % 2]
                        sk = "sg%d" % (si % 2)
                        si += 1
                        kb.op("scalar", lambda e, s=s, ps=psg: e.activation(out=s[:, 0:TB], in_=ps[:, 0:TB], func=AF.Silu),
                              reads=[pkg], writes=[sk])
                        kb.op("vector", lambda e, s=s, ps=psu, j=j: e.tensor_tensor(
                            out=aT[:, j, :], in0=ps[:, 0:TB], in1=s[:, 0:TB], op=ALU.mult),
                            reads=[pku, sk], writes=[("aT", j)])
                akeys = [("aT", j) for j in range(NJ)]
                for i in range(NTB):
                    r0 = tb * TB + i * 128
                    sb_ = (tb * NTB + i) % 2
                    for c in range(2):
                        ps, pk = self.next_psf()
                        for j in range(NJ):
                            kb.op("tensor", lambda e, j=j, ps=ps, i=i, c=c: e.matmul(
                                out=ps[:, :], lhsT=aT[:, j, i * 128:(i + 1) * 128], rhs=Wd[:, j, c * 512:(c + 1) * 512],
                                start=(j == 0), stop=(j == NJ - 1)), reads=akeys + wdkeys, writes=[pk])
                        kb.op("vector", lambda e, ps=ps, i=i, c=c, sb_=sb_: e.tensor_tensor(
                            out=stg[sb_][:, c * 512:(c + 1) * 512], in0=ps[:, :], in1=xb[:, i, c * 512:(c + 1) * 512], op=ALU.add),
                            reads=[pk, ("xb", i)], writes=[("stg", sb_, c)])
                    skeys = [("stg", sb_, 0), ("stg", sb_, 1)]
                    if gfin_row is not None:
                        junk, ms, rstd = tmp["junk"], tmp["ms"], tmp["rstd"]
                        so = stg[sb_]
                        kb.op("vector", lambda e, so=so: e.scalar_tensor_tensor(
                            out=junk[:], in0=so[:], scalar=1.0, in1=so[:], op0=ALU.mult, op1=ALU.mult, accum_out=ms[:]),
                            reads=skeys, writes=["junk", "ms"])
                        kb.op("scalar", lambda e: e.activation(out=rstd[:], in_=ms[:], func=AF.Sqrt, bias=tmp["epsc"][:], scale=1.0 / D),
                              reads=["ms"], writes=["rstd"])
                        kb.op("vector", lambda e: e.reciprocal(out=rstd[:], in_=rstd[:]), reads=["rstd"], writes=["rstd"])
                        kb.op("vector", lambda e, so=so: e.scalar_tensor_tensor(
                            out=so[:], in0=so[:], scalar=rstd[:], in1=gf_bc[:], op0=ALU.mult, op1=ALU.mult),
                            reads=skeys + ["rstd", "gf_bc"], writes=skeys)
                    kb.dma("sync", xo_d[r0:r0 + 128, :], stg[sb_][:], reads=skeys)
            kb.flush()

    def bc_last(self, ap, n):
        return bass.AP(ap.tensor, ap.offset, [list(p) for p in ap.ap] + [[0, n]])

    def make_masks(self, st):
        nc, kb = self.nc, self.kb
        self.m_ge = sb(st, nc, "m_ge", [128, 128], F32)
        kb.op("gpsimd", lambda e: e.affine_select(
            out=self.m_ge[:], in_=self.ones_f[:], pattern=[[1, 128]], compare_op=ALU.is_ge,
            fill=0.0, base=0, channel_multiplier=-1), reads=["ones_f"], writes=["m_ge"])

    def phase_hgrn2(self, q_fm, f_fm, i_tm, g_tm, lbt_d, gain_row, y_tm):
        nc, kb, T = self.nc, self.kb, self.T
        TB = min(512, T)
        NC = TB // 128
        H = 4
        with ExitStack() as st:
            S = lambda name, shape, dt: sb(st, nc, name, shape, dt)
            lbt = S("lbt", [128, 3, 4], F32)
            lbe = S("lbe", [128, 3, 4], F32)
            lbs = S("lbs", [128, 4], F32)
            lb = S("lb", [128, 4], F32)
            oml = S("oml", [128, 4], F32)
            kb.dma("sync", lbt[:], lbt_d.rearrange("r (c p) -> p r c", p=128), writes=["lbt"], allow_slow_non_contiguous=True)
            kb.op("scalar", lambda e: e.activation(out=lbe[:], in_=lbt[:], func=AF.Exp), reads=["lbt"], writes=["lbe"])
            kb.op("vector", lambda e: e.tensor_tensor(out=lbs[:], in0=lbe[:, 0, :], in1=lbe[:, 1, :], op=ALU.add), reads=["lbe"], writes=["lbs"])
            kb.op("vector", lambda e: e.tensor_tensor(out=lbs[:], in0=lbs[:], in1=lbe[:, 2, :], op=ALU.add), reads=["lbe", "lbs"], writes=["lbs"])
            kb.op("vector", lambda e: e.reciprocal(out=lbs[:], in_=lbs[:]), reads=["lbs"], writes=["lbs"])
            kb.op("vector", lambda e: e.tensor_tensor(out=lb[:], in0=lbe[:, 0, :], in1=lbs[:], op=ALU.mult), reads=["lbe", "lbs"], writes=["lb"])
            kb.op("vector", lambda e: e.tensor_scalar(out=oml[:], in0=lb[:], scalar1=-1.0, scalar2=1.0, op0=ALU.mult, op1=ALU.add),
                  reads=["lb"], writes=["oml"])
            gn_bc = S("gn_bc", [128, 512], F32)
            self.load_bcast(gn_bc[:], gain_row, 512, "gn_bc")
            rmask = S("rmask", [128, TB], F32)
            kb.op("gpsimd", lambda e: e.memset(rmask[:], 1.0), writes=["rmask"])
            kb.op("gpsimd", lambda e: e.memset(rmask[:].rearrange("p (c l) -> p c l", l=128)[:, :, 0:1], 0.0), writes=["rmask"])
            qin = S("qin", [128, H, TB], F32)
            fin = S("fin", [128, H, TB], F32)
            t_f_l = [S("t_f%d" % i, [128, TB], F32) for i in range(H)]
            t_lf_l = [S("t_lf%d" % i, [128, TB], F32) for i in range(H)]
            t_b_l = [S("t_b%d" % i, [128, TB], F32) for i in range(H)]
            t_d_l = [S("t_d%d" % i, [128, TB], F32) for i in range(H)]
            t_e_l = [S("t_e%d" % i, [128, TB], F32) for i in range(H)]
            t_e2_l = [S("t_e2%d" % i, [128, TB], F32) for i in range(H)]
            t_q_l = [S("t_q%d" % i, [128, TB], F32) for i in range(H)]
            QT = S("QT", [128, H, TB], BF16)
            KT = S("KT", [128, H, TB], BF16)
            bm = S("bm", [128, H, NC], F32)
            dl = S("dl", [128, H, NC], F32)
            emid = S("emid", [128, H, NC], F32)
            e1 = S("e1", [128, H, NC], F32)
            e2 = S("e2", [128, H, NC], F32)
            vin = [S("vin%d" % i, [128, 512], F32) for i in range(2)]
            gin = [S("gin%d" % i, [128, 512], F32) for i in range(2)]
            vb_l = [S("vb%d" % i, [128, 512], BF16) for i in range(2)]
            gg_l = [S("gg%d" % i, [128, 512], F32) for i in range(2)]
            St = S("St", [128, H, 128], F32)
            Sb = S("Sb", [128, H, 128], BF16)
            PT4 = [S("PT4_%d" % i, [128, H, 128], BF16) for i in range(2)]
            Ktm4 = [S("Ktm4_%d" % i, [128, H, 128], BF16) for i in range(2)]
            dS4 = S("dS4", [128, H, 128], F32)
            mge4 = bass.AP(self.m_ge[:].tensor, self.m_ge[:].offset, [list(self.m_ge[:].ap[0]), [0, H], [1, 128]])
            dS_l = [S("dS%d" % i, [128, 128], F32) for i in range(2)]
            osq = S("osq", [128, 512], F32)
            ssq = S("ssq", [128, 4], F32)
            rs = S("rs", [128, 4], F32)
            yo = [S("yo%d" % i, [128, 512], BF16) for i in range(2)]
            epsc = S("epsc", [128, 1], F32)
            kb.op("gpsimd", lambda e: e.memset(epsc[:], RMS_EPS), writes=["epsc"])
            kb.op("gpsimd", lambda e: e.memset(St[:], 0.0), writes=["St"])
            kb.op("gpsimd", lambda e: e.memset(Sb[:], 0.0), writes=["Sb"])
            pi = 0
            for tb in range(T // TB):
                t0 = tb * TB
                kb.dma("sync", qin[:], q_fm.rearrange("(h p) t -> p h t", p=128)[:, :, t0:t0 + TB], writes=["qin"])
                kb.dma("sync", fin[:], f_fm.rearrange("(h p) t -> p h t", p=128)[:, :, t0:t0 + TB], writes=["fin"])
                def _head(h):
                    t_f, t_lf, t_b, t_d, t_e, t_q, t_e2 = t_f_l[h], t_lf_l[h], t_b_l[h], t_d_l[h], t_e_l[h], t_q_l[h], t_e2_l[h]
                    kb.op("scalar", lambda e, h=h: e.activation(out=t_f[:], in_=fin[:, h, :], func=AF.Sigmoid), reads=["fin"], writes=["t_f%d" % h])
                    kb.op("vector", lambda e, h=h: e.tensor_scalar(out=t_f[:], in0=t_f[:], scalar1=oml[:, h:h + 1], scalar2=lb[:, h:h + 1],
                                                                 op0=ALU.mult, op1=ALU.add), reads=["t_f%d" % h, "oml", "lb"], writes=["t_f%d" % h])
                    kb.op("scalar", lambda e: e.activation(out=t_lf[:], in_=t_f[:], func=AF.Ln), reads=["t_f%d" % h], writes=["t_lf%d" % h])
                    kb.op("vector", lambda e: e.tensor_tensor_scan(out=t_b[:], data0=rmask[:], data1=t_lf[:], initial=0.0,
                                                                   op0=ALU.mult, op1=ALU.add), reads=["rmask", "t_lf%d" % h], writes=["t_b%d" % h])
                    b3 = t_b[:].rearrange("p (c l) -> p c l", l=128)
                    kb.op("vector", lambda e, h=h, b3=b3: e.tensor_copy(out=bm[:, h, :], in_=b3[:, :, 63]), reads=["t_b%d" % h], writes=["bm"])
                    kb.op("vector", lambda e, h=h, b3=b3: e.tensor_tensor(out=dl[:, h, :], in0=b3[:, :, 127], in1=b3[:, :, 63], op=ALU.subtract),
                          reads=["t_b%d" % h], writes=["dl"])
                    kb.op("vector", lambda e, h=h, b3=b3: e.tensor_tensor(
                        out=t_d[:].rearrange("p (c l) -> p c l", l=128), in0=b3, in1=self.bc_last(bm[:, h, :], 128), op=ALU.subtract),
                        reads=["t_b%d" % h, "bm"], writes=["t_d%d" % h])
                    kb.op("scalar", lambda e: e.activation(out=t_e[:], in_=t_d[:], func=AF.Exp), reads=["t_d%d" % h], writes=["t_e%d" % h])
                    kb.op("scalar", lambda e, h=h: e.activation(out=t_q[:], in_=qin[:, h, :], func=AF.Silu), reads=["qin"], writes=["t_q%d" % h])
                    kb.op("vector", lambda e, h=h: e.tensor_tensor(out=QT[:, h, :], in0=t_q[:], in1=t_e[:], op=ALU.mult),
                          reads=["t_q%d" % h, "t_e%d" % h], writes=[("QT", h)])
                    kb.op("scalar", lambda e: e.activation(out=t_e2[:], in_=t_d[:], func=AF.Exp, scale=-1.0), reads=["t_d%d" % h], writes=["t_e2%d" % h])
                    kb.op("vector", lambda e: e.tensor_scalar(out=t_f[:], in0=t_f[:], scalar1=-1.0, scalar2=1.0, op0=ALU.mult, op1=ALU.add),
                          reads=["t_f%d" % h], writes=["t_f%d" % h])
                    kb.op("vector", lambda e, h=h: e.tensor_tensor(out=KT[:, h, :], in0=t_f[:], in1=t_e2[:], op=ALU.mult),
                          reads=["t_f%d" % h, "t_e2%d" % h], writes=[("KT", h)])
                    kb.op("scalar", lambda e, h=h: e.activation(out=emid[:, h, :], in_=bm[:, h, :], func=AF.Exp), reads=["bm"], writes=["emid"])
                    kb.op("scalar", lambda e, h=h: e.activation(out=e2[:, h, :], in_=dl[:, h, :], func=AF.Exp), reads=["dl"], writes=["e2"])
                    kb.op("vector", lambda e, h=h: e.tensor_tensor(out=e1[:, h, :], in0=e2[:, h, :], in1=emid[:, h, :], op=ALU.mult),
                          reads=["e2", "emid"], writes=["e1"])
                for h in range(H):
                    _head(h)
                def _front(c):
                    r0 = t0 + c * 128
                    ib = (tb * NC + c) % 2
                    vb, gg = vb_l[ib], gg_l[ib]
                    kvb, kgg = 'vb%d' % ib, 'gg%d' % ib
                    cs = slice(c * 128, (c + 1) * 128)
                    kb.dma("sync", vin[ib][:], i_tm[r0:r0 + 128, :], writes=["vin%d" % ib])
                    kb.dma("sync", gin[ib][:], g_tm[r0:r0 + 128, :], writes=["gin%d" % ib])
                    kb.op("vector", lambda e, ib=ib: e.tensor_copy(out=vb[:], in_=vin[ib][:]), reads=["vin%d" % ib], writes=[kvb])
                    kb.op("scalar", lambda e, ib=ib: e.activation(out=gg[:], in_=gin[ib][:], func=AF.Silu), reads=["gin%d" % ib], writes=[kgg])
                    kb.op("gpsimd", lambda e: e.tensor_tensor(out=gg[:], in0=gg[:], in1=gn_bc[:], op=ALU.mult), reads=[kgg, "gn_bc"], writes=[kgg])
                    psa, pka = self.bank(2 + ib)
                    for h in range(H):
                        kb.op("tensor", lambda e, h=h, cs=cs, psa=psa: e.matmul(out=psa[:, h * 128:(h + 1) * 128], lhsT=KT[:, h, cs], rhs=QT[:, h, cs], start=True, stop=True),
                              reads=[("KT", h), ("QT", h)], writes=[pka])
                    kb.op("vector", lambda e, psa=psa, ib=ib: e.tensor_tensor(out=PT4[ib][:], in0=psa[:, :].rearrange("p (h t) -> p h t", t=128), in1=mge4, op=ALU.mult),
                          reads=[pka, "m_ge"], writes=[("PT4", ib)])
                    psk, pkk = self.next_psb()
                    for h in range(H):
                        kb.op("tensor", lambda e, h=h, cs=cs, psk=psk: e.transpose(out=psk[:, h * 128:(h + 1) * 128], in_=KT[:, h, cs], identity=self.ident[:]),
                              reads=[("KT", h), "ident"], writes=[pkk])
                    kb.op("scalar", lambda e, psk=psk, ib=ib: e.copy(out=Ktm4[ib][:], in_=psk[:, 0:512].rearrange("p (h k) -> p h k", k=128)), reads=[pkk], writes=[("Ktm4", ib)])

                def _back(c):
                    r0 = t0 + c * 128
                    ib = (tb * NC + c) % 2
                    vb, gg = vb_l[ib], gg_l[ib]
                    kvb, kgg = 'vb%d' % ib, 'gg%d' % ib
                    cs = slice(c * 128, (c + 1) * 128)
                    pso, pko = self.bank(ib)
                    kb.op("vector", lambda e, c=c: e.tensor_tensor(out=Sb[:], in0=St[:], in1=self.bc_last(emid[:, :, c], 128), op=ALU.mult),
                          reads=["St", "emid"], writes=["Sb"])
                    for h in range(H):
                        hs = slice(h * 128, (h + 1) * 128)
                        kb.op("tensor", lambda e, h=h, hs=hs, pso=pso, ib=ib: e.matmul(out=pso[:, hs], lhsT=PT4[ib][:, h, :], rhs=vb[:, hs], start=True, stop=False),
                              reads=[("PT4", ib), kvb], writes=[pko])
                        kb.op("tensor", lambda e, h=h, cs=cs, hs=hs, pso=pso: e.matmul(out=pso[:, hs], lhsT=QT[:, h, cs], rhs=Sb[:, h, :], start=False, stop=True),
                              reads=[("QT", h), "Sb"], writes=[pko])
                    pss, pks = self.bank(4 + ib)
                    for h in range(H):
                        hs = slice(h * 128, (h + 1) * 128)
                        kb.op("tensor", lambda e, h=h, hs=hs, pss=pss, ib=ib: e.matmul(out=pss[:, hs], lhsT=Ktm4[ib][:, h, :], rhs=vb[:, hs], start=True, stop=True),
                              reads=[("Ktm4", ib), kvb], writes=[pks])
                    kb.op("vector", lambda e, c=c, pss=pss: e.tensor_tensor(out=dS4[:], in0=pss[:, :].rearrange("p (h v) -> p h v", v=128), in1=self.bc_last(e2[:, :, c], 128), op=ALU.mult),
                          reads=[pks, "e2"], writes=["dS4"])
                    kb.op("vector", lambda e, c=c: e.tensor_tensor(out=St[:], in0=St[:], in1=self.bc_last(e1[:, :, c], 128), op=ALU.mult),
                          reads=["St", "e1", "Sb"], writes=["St"])
                    kb.op("gpsimd", lambda e: e.tensor_tensor(out=St[:], in0=St[:], in1=dS4[:], op=ALU.add), reads=["St", "dS4"], writes=["St"])
                    kb.op("scalar", lambda e, pso=pso: e.activation(out=osq[:], in_=pso[:, :], func=AF.Square), reads=[pko], writes=["osq"])
                    kb.op("vector", lambda e: e.tensor_reduce(out=ssq[:], in_=osq[:].rearrange("p (h v) -> p h v", v=128), axis=AX.X, op=ALU.add),
                          reads=["osq"], writes=["ssq"])
                    kb.op("scalar", lambda e: e.activation(out=rs[:], in_=ssq[:], func=AF.Sqrt, bias=epsc[:], scale=1.0 / 128), reads=["ssq", "epsc"], writes=["rs"])
                    kb.op("vector", lambda e: e.reciprocal(out=rs[:], in_=rs[:]), reads=["rs"], writes=["rs"])
                    for h in range(H):
                        hs = slice(h * 128, (h + 1) * 128)
                        kb.op("vector", lambda e, h=h, hs=hs, pso=pso, ib=ib: e.scalar_tensor_tensor(
                            out=yo[ib][:, hs], in0=pso[:, hs], scalar=rs[:, h:h + 1], in1=gg[:, hs], op0=ALU.mult, op1=ALU.mult),
                            reads=[pko, "rs", kgg], writes=["yo%d" % ib])
                    kb.dma("gpsimd", y_tm[r0:r0 + 128, :], yo[ib][:], reads=["yo%d" % ib])
                _front(0)
                for c in range(NC):
                    if c + 1 < NC:
                        _front(c + 1)
                    _back(c)
            kb.flush()

    def phase_mlstm(self, q_fm, k_fm, v_tm, o_tm, if_tm, cq_d, ck_d, ib_row, fb_row, gain_row, y_tm):
        nc, kb, T = self.nc, self.kb, self.T
        TB = min(512, T)
        NC = TB // 128
        H, DH, NJ = 4, 192, 6
        pieces = {0: [(0, 0, 128, 0), (1, 0, 64, 128)], 1: [(2, 0, 128, 64), (1, 64, 128, 0)],
                  2: [(3, 0, 128, 0), (4, 0, 64, 128)], 3: [(5, 0, 128, 64), (4, 64, 128, 0)]}
        with ExitStack() as st:
            S = lambda name, shape, dt: sb(st, nc, name, shape, dt)
            cw = {"q": S("cwq", [128, 4, NJ], F32), "k": S("cwk", [128, 4, NJ], F32)}
            for tap in range(4):
                kb.dma("sync", cw["q"][:, tap, :], cq_d[tap].rearrange("(c p) -> p c", p=128), writes=[("cwq", tap)], allow_slow_non_contiguous=True)
                kb.dma("sync", cw["k"][:, tap, :], ck_d[tap].rearrange("(c p) -> p c", p=128), writes=[("cwk", tap)], allow_slow_non_contiguous=True)
            gb_bc = S("gb_bc", [128, 8], F32)
            kb.dma("sync", gb_bc[:, 0:4], ib_row.partition_broadcast(128), writes=["gbi"])
            kb.dma("sync", gb_bc[:, 4:8], fb_row.partition_broadcast(128), writes=["gbf"])
            gn_bc = S("gn_bc", [128, 768], F32)
            self.load_bcast(gn_bc[:], gain_row, 768, "gn_bc")
            uin = {"q": S("uinq", [128, NJ, TB + 3], F32), "k": S("uink", [128, NJ, TB + 3], F32)}
            acc = [S("acc%d" % i, [128, TB], F32) for i in range(2)]
            XT = {"q": S("qT", [128, NJ, TB], BF16), "k": S("kT", [128, NJ, TB], BF16)}
            vin = [S("vin%d" % i, [128, 768], F32) for i in range(2)]
            oin = [S("oin%d" % i, [128, 768], F32) for i in range(2)]
            gin = [S("gin%d" % i, [128, 8], F32) for i in range(2)]
            vext_l = [S("vext%d" % i, [128, H, DH + 1], BF16) for i in range(2)]
            go_l = [S("go%d" % i, [128, 768], F32) for i in range(2)]
            gt_l = [S("gt%d" % i, [128, 8], F32) for i in range(2)]
            lf_l = [S("lf%d" % i, [128, 4], F32) for i in range(2)]
            bb_l = [S("bb%d" % i, [128, 4], F32) for i in range(2)]
            ee_l = [S("ee%d" % i, [128, 4], F32) for i in range(2)]
            emb_l = [S("emb%d" % i, [128, 4], F32) for i in range(2)]
            dec_l = [S("dec%d" % i, [128, 4], F32) for i in range(2)]
            Ktm_l = [S("Ktm%d" % i, [128, 768], BF16) for i in range(2)]
            PT4 = [S("PT4_%d" % i, [128, H, 128], BF16) for i in range(2)]
            decT_l = [S("decT%d" % i, [128, NJ], F32) for i in range(2)]
            dC2 = [S("dC2_%d" % i, [128, 2, DH + 1], F32) for i in range(3)]
            Cst = S("Cst", [128, NJ, DH + 1], F32)
            Cb = S("Cb", [128, NJ, DH + 1], BF16)
            dC_l = [S("dC%d" % i, [128, DH + 1], F32) for i in range(4)]
            junk_l = [S("junk%d" % i, [128, DH], F32) for i in range(2)]
            ssq_l = [S("ssq%d" % i, [128, 4], F32) for i in range(2)]
            dm_l = [S("dm%d" % i, [128, 4], F32) for i in range(2)]
            t1_l = [S("t1%d" % i, [128, 4], F32) for i in range(2)]
            sc_l = [S("sc%d" % i, [128, 4], F32) for i in range(2)]
            yo = [S("yo%d" % i, [128, 768], BF16) for i in range(2)]
            epsc = S("epsc", [128, 1], F32)
            kb.op("gpsimd", lambda e: e.memset(epsc[:], RMS_EPS), writes=["epsc"])
            kb.op("gpsimd", lambda e: e.memset(Cst[:], 0.0), writes=[("Cst", 0), ("Cst", 1), ("Cst", 2)])
            kb.op("gpsimd", lambda e: e.memset(Cb[:], 0.0), writes=[("Cb", 0), ("Cb", 1), ("Cb", 2)])
            kb.op("gpsimd", lambda e: e.memset(vext_l[0][:], 1.0), writes=["vext0"])
            kb.op("gpsimd", lambda e: e.memset(vext_l[1][:], 1.0), writes=["vext1"])
            kb.op("gpsimd", lambda e: e.memset(uin["q"][:, :, 0:3], 0.0), writes=["uinq"])
            kb.op("gpsimd", lambda e: e.memset(uin["k"][:, :, 0:3], 0.0), writes=["uink"])
            srcs = {"q": q_fm.rearrange("(c p) t -> p c t", p=128), "k": k_fm.rearrange("(c p) t -> p c t", p=128)}
            ai = 0
            pi = 0
            dci = 0
            for tb in range(T // TB):
                t0 = tb * TB
                for nm in ("q", "k"):
                    if tb == 0:
                        kb.dma("sync", uin[nm][:, :, 3:3 + TB], srcs[nm][:, :, 0:TB], writes=["uin" + nm])
                    else:
                        kb.dma("sync", uin[nm][:, :, :], srcs[nm][:, :, t0 - 3:t0 + TB], writes=["uin" + nm])
                    for j in range(NJ):
                        a = acc[ai % 2]
                        ak = "acc%d" % (ai % 2)
                        ai += 1
                        kb.op("vector", lambda e, nm=nm, j=j, a=a: e.tensor_scalar(
                            out=a[:], in0=uin[nm][:, j, 3:3 + TB], scalar1=cw[nm][:, 3, j:j + 1], scalar2=None, op0=ALU.mult),
                            reads=["uin" + nm] + [("cw" + nm, tp) for tp in range(4)], writes=[ak])
                        for tap in (2, 1, 0):
                            kb.op("vector", lambda e, nm=nm, j=j, a=a, tap=tap: e.scalar_tensor_tensor(
                                out=a[:], in0=uin[nm][:, j, tap:tap + TB], scalar=cw[nm][:, tap, j:j + 1], in1=a[:],
                                op0=ALU.mult, op1=ALU.add), reads=["uin" + nm, ak], writes=[ak])
                        kb.op("scalar", lambda e, nm=nm, j=j, a=a: e.activation(out=XT[nm][:, j, :], in_=a[:], func=AF.Silu),
                              reads=[ak], writes=[(nm + "T", j)])
                def _front(c):
                    nonlocal pi, dci
                    r0 = t0 + c * 128
                    cs = slice(c * 128, (c + 1) * 128)
                    ib = (tb * NC + c) % 2
                    vext, go, gt, lf, bb, ee, emb, dec, Ktm, ssq, dm, t1, sc = (vext_l[ib], go_l[ib], gt_l[ib], lf_l[ib], bb_l[ib], ee_l[ib], emb_l[ib], dec_l[ib], Ktm_l[ib], ssq_l[ib], dm_l[ib], t1_l[ib], sc_l[ib])
                    kb.dma("sync", vin[ib][:], v_tm[r0:r0 + 128, :], writes=["vin%d" % ib])
                    kb.dma("sync", oin[ib][:], o_tm[r0:r0 + 128, :], writes=["oin%d" % ib])
                    kb.dma("sync", gin[ib][:], if_tm[r0:r0 + 128, :], writes=["gin%d" % ib])
                    kb.op("vector", lambda e, ib=ib: e.tensor_copy(out=vext[:, :, 0:DH], in_=vin[ib][:].rearrange("p (h v) -> p h v", v=DH)),
                          reads=["vin%d" % ib], writes=["vext%d" % ib])
                    kb.op("scalar", lambda e, ib=ib: e.activation(out=go[:], in_=oin[ib][:], func=AF.Sigmoid), reads=["oin%d" % ib], writes=["go%d" % ib])
                    kb.op("gpsimd", lambda e: e.tensor_tensor(out=go[:], in0=go[:], in1=gn_bc[:], op=ALU.mult), reads=["go%d" % ib, "gn_bc"], writes=["go%d" % ib])
                    kb.op("vector", lambda e, ib=ib: e.tensor_tensor(out=gt[:], in0=gin[ib][:], in1=gb_bc[:], op=ALU.add),
                          reads=["gin%d" % ib, "gbi", "gbf"], writes=["gt%d" % ib])
                    kb.op("scalar", lambda e: e.activation(out=lf[:], in_=gt[:, 4:8], func=AF.Sigmoid), reads=["gt%d" % ib], writes=["lf%d" % ib])
                    kb.op("scalar", lambda e: e.activation(out=lf[:], in_=lf[:], func=AF.Ln), reads=["lf%d" % ib], writes=["lf%d" % ib])
                    psg, pkg = self.bank(0)
                    kb.op("tensor", lambda e, psg=psg: e.matmul(out=psg[:, 0:4], lhsT=self.m_ge[:], rhs=lf[:], start=True, stop=True),
                          reads=["m_ge", "lf%d" % ib], writes=[pkg])
                    kb.op("tensor", lambda e, psg=psg: e.matmul(out=psg[:, 4:8], lhsT=self.ones_f[:], rhs=lf[:], start=True, stop=True),
                          reads=["ones_f", "lf%d" % ib], writes=[pkg])
                    kb.op("vector", lambda e, psg=psg: e.tensor_copy(out=bb[:], in_=psg[:, 0:4]), reads=[pkg], writes=["bb%d" % ib])
                    kb.op("scalar", lambda e, psg=psg: e.activation(out=dec[:], in_=psg[:, 4:8], func=AF.Exp), reads=[pkg], writes=["dec%d" % ib])
                    decT = decT_l[ib]
                    for (jj, q0, q1, hh) in ((0, 0, 128, 0), (1, 0, 64, 0), (1, 64, 128, 1), (2, 0, 128, 1), (3, 0, 128, 2), (4, 0, 64, 2), (4, 64, 128, 3), (5, 0, 128, 3)):
                        kb.op("gpsimd", lambda e, jj=jj, q0=q0, q1=q1, hh=hh, decT=decT: e.tensor_copy(out=decT[q0:q1, jj:jj + 1], in_=dec[q0:q1, hh:hh + 1]),
                              reads=["dec%d" % ib], writes=["decT%d" % ib])
                    kb.op("scalar", lambda e: e.activation(out=emb[:], in_=bb[:], func=AF.Exp, scale=-1.0), reads=["bb%d" % ib], writes=["emb%d" % ib])
                    kb.op("vector", lambda e: e.scalar_tensor_tensor(out=ee[:], in0=gt[:, 0:4], scalar=-0.5 * float(np.log(DH)), in1=bb[:],
                                                                     op0=ALU.add, op1=ALU.subtract), reads=["gt%d" % ib, "bb%d" % ib], writes=["ee%d" % ib])
                    kb.op("scalar", lambda e: e.activation(out=ee[:], in_=ee[:], func=AF.Exp), reads=["ee%d" % ib], writes=["ee%d" % ib])
                    psk, pkk = self.next_psb()
                    for j in range(NJ):
                        kb.op("tensor", lambda e, j=j, cs=cs, psk=psk: e.transpose(out=psk[:, j * 128:(j + 1) * 128], in_=XT["k"][:, j, cs], identity=self.ident[:]),
                              reads=[("kT", j), "ident"], writes=[pkk])
                    for h in range(H):
                        hs = slice(h * DH, (h + 1) * DH)
                        kb.op("vector", lambda e, h=h, hs=hs, psk=psk: e.tensor_scalar(out=Ktm[:, hs], in0=psk[:, hs], scalar1=ee[:, h:h + 1], scalar2=None, op0=ALU.mult),
                              reads=[pkk, "ee%d" % ib], writes=[("Ktm", ib, h)])
                def _back(c):
                    nonlocal pi, dci
                    r0 = t0 + c * 128
                    cs = slice(c * 128, (c + 1) * 128)
                    ib = (tb * NC + c) % 2
                    vext, go, gt, lf, bb, ee, emb, dec, Ktm, ssq, dm, t1, sc = (vext_l[ib], go_l[ib], gt_l[ib], lf_l[ib], bb_l[ib], ee_l[ib], emb_l[ib], dec_l[ib], Ktm_l[ib], ssq_l[ib], dm_l[ib], t1_l[ib], sc_l[ib])
                    psS, pkS = self.bank(0)
                    psn = [self.bank(1), self.bank(2)]
                    for h in range(H):
                        (ja, a0, a1, oa), (jb, b0, b1, ob_) = pieces[h]
                        kb.op("tensor", lambda e, h=h, cs=cs, ja=ja, a0=a0, a1=a1, psS=psS: e.matmul(
                            out=psS[:, h * 128:(h + 1) * 128], lhsT=XT["k"][a0:a1, ja, cs], rhs=XT["q"][a0:a1, ja, cs], start=True, stop=False),
                            reads=[("kT", ja), ("qT", ja)], writes=[pkS])
                        kb.op("tensor", lambda e, h=h, cs=cs, jb=jb, b0=b0, b1=b1, psS=psS: e.matmul(
                            out=psS[:, h * 128:(h + 1) * 128], lhsT=XT["k"][b0:b1, jb, cs], rhs=XT["q"][b0:b1, jb, cs], start=False, stop=True),
                            reads=[("kT", jb), ("qT", jb)], writes=[pkS])
                    for h in range(H):
                        kb.op("vector", lambda e, h=h, psS=psS, ib=ib: e.scalar_tensor_tensor(
                            out=PT4[ib][:, h, :], in0=psS[:, h * 128:(h + 1) * 128], scalar=ee[:, h:h + 1], in1=self.m_ge[:], op0=ALU.mult, op1=ALU.mult),
                            reads=[pkS, "ee%d" % ib, "m_ge"], writes=[("PT4", ib, h)])
                    for h in range(H):
                        (ja, a0, a1, oa), (jb, b0, b1, ob_) = pieces[h]
                        pn, pkn = psn[h // 2]
                        ncol = slice((h % 2) * 256, (h % 2) * 256 + DH + 1)
                        kb.op("tensor", lambda e, h=h, pn=pn, ncol=ncol, ib=ib: e.matmul(out=pn[:, ncol], lhsT=PT4[ib][:, h, :], rhs=vext[:, h, :], start=True, stop=False),
                              reads=[("PT4", ib, h), "vext%d" % ib], writes=[pkn])
                        kb.op("tensor", lambda e, cs=cs, ja=ja, a0=a0, a1=a1, pn=pn, ncol=ncol: e.matmul(
                            out=pn[:, ncol], lhsT=XT["q"][a0:a1, ja, cs], rhs=Cb[a0:a1, ja, :], start=False, stop=False),
                            reads=[("qT", ja), ("Cb", ja // 2)], writes=[pkn])
                        kb.op("tensor", lambda e, cs=cs, jb=jb, b0=b0, b1=b1, pn=pn, ncol=ncol: e.matmul(
                            out=pn[:, ncol], lhsT=XT["q"][b0:b1, jb, cs], rhs=Cb[b0:b1, jb, :], start=False, stop=True),
                            reads=[("qT", jb), ("Cb", jb // 2)], writes=[pkn])
                    for h in range(H):
                        (ja, a0, a1, oa), (jb, b0, b1, ob_) = pieces[h]
                        for (jj, q0, q1, off) in ((ja, a0, a1, oa), (jb, b0, b1, ob_)):
                            pc, pkc = self.bank(3 + jj // 2)
                            cc0 = (jj % 2) * 256
                            kb.op("tensor", lambda e, h=h, q0=q0, q1=q1, off=off, pc=pc, cc0=cc0: e.matmul(
                                out=pc[q0:q1, cc0:cc0 + DH + 1], lhsT=Ktm[:, h * DH + off:h * DH + off + (q1 - q0)], rhs=vext[:, h, :], start=True, stop=True),
                                reads=[("Ktm", ib, h), "vext%d" % ib], writes=[pkc])
                    for k2 in range(3):
                        pc, pkc = self.bank(3 + k2)
                        dcb = self.bc_last(decT_l[ib][:, 2 * k2:2 * k2 + 2], DH + 1)
                        kb.op("vector", lambda e, k2=k2, pc=pc, dcb=dcb: e.tensor_tensor(
                            out=dC2[k2][:], in0=pc[:, :].rearrange("p (a b) -> p a b", b=256)[:, :, 0:DH + 1], in1=dcb, op=ALU.mult),
                            reads=[pkc, "decT%d" % ib], writes=[("dC2", k2)])
                        kb.op("vector", lambda e, k2=k2, dcb=dcb: e.tensor_tensor(
                            out=Cst[:, 2 * k2:2 * k2 + 2, :], in0=Cst[:, 2 * k2:2 * k2 + 2, :], in1=dcb, op=ALU.mult),
                            reads=[("Cst", k2), "decT%d" % ib, ("Cb", k2)], writes=[("Cst", k2)])
                        kb.op("gpsimd", lambda e, k2=k2: e.tensor_tensor(
                            out=Cst[:, 2 * k2:2 * k2 + 2, :], in0=Cst[:, 2 * k2:2 * k2 + 2, :], in1=dC2[k2][:], op=ALU.add),
                            reads=[("Cst", k2), ("dC2", k2)], writes=[("Cst", k2)])
                        kb.op("scalar", lambda e, k2=k2: e.copy(out=Cb[:, 2 * k2:2 * k2 + 2, :], in_=Cst[:, 2 * k2:2 * k2 + 2, :]),
                              reads=[("Cst", k2)], writes=[("Cb", k2)])
                    for h in range(H):
                        pn, pkn = psn[h // 2]
                        c0 = (h % 2) * 256
                        kb.op("vector", lambda e, h=h, pn=pn, c0=c0: e.tensor_copy(out=dm[:, h:h + 1], in_=pn[:, c0 + DH:c0 + DH + 1]),
                              reads=[pkn], writes=["dm%d" % ib])
                        kb.op("scalar", lambda e, h=h, pn=pn, c0=c0: e.activation(out=junk_l[h % 2][:], in_=pn[:, c0:c0 + DH], func=AF.Square, accum_out=ssq[:, h:h + 1]),
                              reads=[pkn], writes=["junk%d" % (h % 2), "ssq%d" % ib])
                    kb.op("vector", lambda e: e.tensor_scalar(out=t1[:], in0=dm[:], scalar1=-1.0, scalar2=None, op0=ALU.mult), reads=["dm%d" % ib], writes=["t1%d" % ib])
                    kb.op("vector", lambda e: e.tensor_tensor(out=dm[:], in0=dm[:], in1=t1[:], op=ALU.max), reads=["dm%d" % ib, "t1%d" % ib], writes=["dm%d" % ib])
                    kb.op("vector", lambda e: e.tensor_tensor(out=dm[:], in0=dm[:], in1=emb[:], op=ALU.max), reads=["dm%d" % ib, "emb%d" % ib], writes=["dm%d" % ib])
                    kb.op("vector", lambda e: e.reciprocal(out=dm[:], in_=dm[:]), reads=["dm%d" % ib], writes=["dm%d" % ib])
                    kb.op("vector", lambda e: e.tensor_tensor(out=t1[:], in0=dm[:], in1=dm[:], op=ALU.mult), reads=["dm%d" % ib], writes=["t1%d" % ib])
                    kb.op("vector", lambda e: e.tensor_tensor(out=t1[:], in0=t1[:], in1=ssq[:], op=ALU.mult), reads=["t1%d" % ib, "ssq%d" % ib], writes=["t1%d" % ib])
                    kb.op("scalar", lambda e: e.activation(out=t1[:], in_=t1[:], func=AF.Sqrt, bias=epsc[:], scale=1.0 / DH), reads=["t1%d" % ib, "epsc"], writes=["t1%d" % ib])
                    kb.op("vector", lambda e: e.reciprocal(out=t1[:], in_=t1[:]), reads=["t1%d" % ib], writes=["t1%d" % ib])
                    kb.op("vector", lambda e: e.tensor_tensor(out=sc[:], in0=t1[:], in1=dm[:], op=ALU.mult), reads=["t1%d" % ib, "dm%d" % ib], writes=["sc%d" % ib])
                    for h in range(H):
                        pn, pkn = psn[h // 2]
                        c0 = (h % 2) * 256
                        hs = slice(h * DH, (h + 1) * DH)
                        kb.op("vector", lambda e, h=h, pn=pn, c0=c0, hs=hs, ib=ib: e.scalar_tensor_tensor(
                            out=yo[ib][:, hs], in0=pn[:, c0:c0 + DH], scalar=sc[:, h:h + 1], in1=go[:, hs], op0=ALU.mult, op1=ALU.mult),
                            reads=[pkn, "sc%d" % ib, "go%d" % ib], writes=["yo%d" % ib])
                    kb.dma("gpsimd", y_tm[r0:r0 + 128, :], yo[ib][:], reads=["yo%d" % ib])
                _front(0)
                for c in range(NC):
                    if c + 1 < NC:
                        _front(c + 1)
                    _back(c)
            kb.flush()

    def phase_s5(self, u_fm, lre_d, lim_d, ls_d, bre_d, bim_d, cre_d, cim_d, dsk_d, gw_d, gb_d, y_fm):
        nc, kb, T = self.nc, self.kb, self.T
        L = min(512, T)
        TWO_PI = 2.0 * float(np.pi)
        with ExitStack() as st:
            S = lambda name, shape, dt: sb(st, nc, name, shape, dt)
            V = lambda fn, r, w: kb.op("vector", fn, reads=r, writes=w)
            lre = S("lre", [128, 8], F32)
            lim = S("lim", [128, 8], F32)
            stp = S("stp", [128, 8], F32)
            kb.dma("sync", lre[:], bass.AP(lre_d.tensor, lre_d.offset, [[1, 128], [128, 8]]), writes=["lre"], allow_slow_non_contiguous=True)
            kb.dma("sync", lim[:], bass.AP(lim_d.tensor, lim_d.offset, [[1, 128], [128, 8]]), writes=["lim"], allow_slow_non_contiguous=True)
            for two in range(2):
                kb.dma("sync", stp[two * 64:(two + 1) * 64, :], bass.AP(ls_d.tensor, ls_d.offset + two, [[0, 64], [2, 8]]),
                       writes=[("stp", two)], allow_slow_non_contiguous=True)
            are = S("are", [128, 8], F32)
            th = S("th", [128, 8], F32)
            rho = S("rho", [128, 8], F32)
            cs1 = S("cs1", [128, 8], F32)
            sn1 = S("sn1", [128, 8], F32)
            w1 = S("w1", [128, 8], F32)
            w2 = S("w2", [128, 8], F32)
            wi = S("wi", [128, 8], mybir.dt.int32)
            cfr = S("cfr", [128, 8], F32)
            cfi = S("cfi", [128, 8], F32)
            kb.op("scalar", lambda e: e.activation(out=stp[:], in_=stp[:], func=AF.Exp), reads=[("stp", 0), ("stp", 1)], writes=["stp"])
            V(lambda e: e.tensor_tensor(out=are[:], in0=lre[:], in1=stp[:], op=ALU.mult), ["lre", "stp"], ["are"])
            V(lambda e: e.tensor_tensor(out=th[:], in0=lim[:], in1=stp[:], op=ALU.mult), ["lim", "stp"], ["th"])
            kb.op("scalar", lambda e: e.activation(out=rho[:], in_=are[:], func=AF.Exp), reads=["are"], writes=["rho"])

            def sin_of(dst, shift):
                V(lambda e: e.tensor_scalar(out=w1[:], in0=th[:], scalar1=shift, scalar2=1.0 / TWO_PI, op0=ALU.add, op1=ALU.mult), ["th"], ["w1"])
                V(lambda e: e.tensor_copy(out=wi[:], in_=w1[:]), ["w1"], ["wi"])
                V(lambda e: e.tensor_copy(out=w2[:], in_=wi[:]), ["wi"], ["w2"])
                V(lambda e: e.tensor_tensor(out=w1[:], in0=w1[:], in1=w2[:], op=ALU.subtract), ["w1", "w2"], ["w1"])
                V(lambda e: e.tensor_scalar(out=w2[:], in0=w1[:], scalar1=0.5, scalar2=None, op0=ALU.is_gt), ["w1"], ["w2"])
                V(lambda e: e.tensor_tensor(out=w1[:], in0=w1[:], in1=w2[:], op=ALU.subtract), ["w1", "w2"], ["w1"])
                V(lambda e: e.tensor_scalar(out=w2[:], in0=w1[:], scalar1=-0.5, scalar2=None, op0=ALU.is_lt), ["w1"], ["w2"])
                V(lambda e: e.tensor_tensor(out=w1[:], in0=w1[:], in1=w2[:], op=ALU.add), ["w1", "w2"], ["w1"])
                kb.op("scalar", lambda e: e.activation(out=dst[:], in_=w1[:], func=AF.Sin, scale=TWO_PI), reads=["w1"], writes=[dst.name])
            sin_of(sn1, 0.0)
            sin_of(cs1, 0.5 * float(np.pi))
            lbr = S("lbr", [128, 8], F32)
            lbi = S("lbi", [128, 8], F32)
            den = S("den", [128, 8], F32)
            V(lambda e: e.tensor_tensor(out=lbr[:], in0=rho[:], in1=cs1[:], op=ALU.mult), ["rho", cs1.name], ["lbr"])
            V(lambda e: e.tensor_tensor(out=lbi[:], in0=rho[:], in1=sn1[:], op=ALU.mult), ["rho", sn1.name], ["lbi"])
            V(lambda e: e.tensor_scalar(out=lbr[:], in0=lbr[:], scalar1=-1.0, scalar2=None, op0=ALU.add), ["lbr"], ["lbr"])
            V(lambda e: e.tensor_tensor(out=den[:], in0=lre[:], in1=lre[:], op=ALU.mult), ["lre"], ["den"])
            V(lambda e: e.tensor_tensor(out=w1[:], in0=lim[:], in1=lim[:], op=ALU.mult), ["lim"], ["w1"])
            V(lambda e: e.tensor_tensor(out=den[:], in0=den[:], in1=w1[:], op=ALU.add), ["den", "w1"], ["den"])
            V(lambda e: e.reciprocal(out=den[:], in_=den[:]), ["den"], ["den"])
            V(lambda e: e.tensor_tensor(out=w1[:], in0=lbr[:], in1=lre[:], op=ALU.mult), ["lbr", "lre"], ["w1"])
            V(lambda e: e.tensor_tensor(out=w2[:], in0=lbi[:], in1=lim[:], op=ALU.mult), ["lbi", "lim"], ["w2"])
            V(lambda e: e.tensor_tensor(out=w1[:], in0=w1[:], in1=w2[:], op=ALU.add), ["w1", "w2"], ["w1"])
            V(lambda e: e.tensor_tensor(out=cfr[:], in0=w1[:], in1=den[:], op=ALU.mult), ["w1", "den"], ["cfr"])
            V(lambda e: e.tensor_tensor(out=w1[:], in0=lbi[:], in1=lre[:], op=ALU.mult), ["lbi", "lre"], ["w1"])
            V(lambda e: e.tensor_tensor(out=w2[:], in0=lbr[:], in1=lim[:], op=ALU.mult), ["lbr", "lim"], ["w2"])
            V(lambda e: e.tensor_tensor(out=w1[:], in0=w1[:], in1=w2[:], op=ALU.subtract), ["w1", "w2"], ["w1"])
            V(lambda e: e.tensor_tensor(out=cfi[:], in0=w1[:], in1=den[:], op=ALU.mult), ["w1", "den"], ["cfi"])
            Ct = S("Ct", [128, 8, L], F32)
            Sn = S("Sn", [128, 8, L], F32)
            ta = S("ta", [128, 8, L // 2], F32)
            tb_ = S("tb_", [128, 8, L // 2], F32)
            V(lambda e: e.tensor_copy(out=Ct[:, :, 0], in_=cs1[:]), [cs1.name], ["Ct"])
            V(lambda e: e.tensor_copy(out=Sn[:, :, 0], in_=sn1[:]), [sn1.name], ["Sn"])
            n = 1
            while n < L:
                cn = self.bc_last(Ct[:, :, n - 1], n)
                sn = self.bc_last(Sn[:, :, n - 1], n)
                V(lambda e, n=n, cn=cn: e.tensor_tensor(out=ta[:, :, 0:n], in0=Ct[:, :, 0:n], in1=cn, op=ALU.mult), ["Ct"], ["ta"])
                V(lambda e, n=n, sn=sn: e.tensor_tensor(out=tb_[:, :, 0:n], in0=Sn[:, :, 0:n], in1=sn, op=ALU.mult), ["Sn"], ["tb_"])
                V(lambda e, n=n: e.tensor_tensor(out=ta[:, :, 0:n], in0=ta[:, :, 0:n], in1=tb_[:, :, 0:n], op=ALU.subtract), ["ta", "tb_"], ["ta"])
                V(lambda e, n=n, sn=sn: e.tensor_tensor(out=tb_[:, :, 0:n], in0=Ct[:, :, 0:n], in1=sn, op=ALU.mult), ["Ct", "Sn"], ["tb_"])
                V(lambda e, n=n: e.tensor_copy(out=Ct[:, :, n:2 * n], in_=ta[:, :, 0:n]), ["ta"], ["Ct"])
                V(lambda e, n=n, cn=cn: e.tensor_tensor(out=ta[:, :, 0:n], in0=Sn[:, :, 0:n], in1=cn, op=ALU.mult), ["Sn", "Ct"], ["ta"])
                V(lambda e, n=n: e.tensor_tensor(out=Sn[:, :, n:2 * n], in0=ta[:, :, 0:n], in1=tb_[:, :, 0:n], op=ALU.add), ["ta", "tb_"], ["Sn"])
                n *= 2
            BTf = S("BTf", [128, 8, 2, 128], F32)
            CTf = S("CTf", [128, 8, 2, 128], F32)
            BT = S("BT", [128, 8, 2, 128], BF16)
            CT = S("CT", [128, 8, 2, 128], BF16)
            kb.op("gpsimd", lambda e: e.memset(BTf[:], 0.0), writes=["BTf"])
            kb.op("gpsimd", lambda e: e.memset(CTf[:], 0.0), writes=["CTf"])
            for g in range(16):
                j, two, gl = g // 2, g % 2, g % 8
                for ri, (bd, cd) in enumerate(((bre_d, cre_d), (bim_d, cim_d))):
                    kb.dma("sync", BTf[gl * 16:(gl + 1) * 16, j, ri, two * 64:(two + 1) * 64], bd[g].rearrange("p c -> c p"),
                           reads=["BTf"], writes=[("BTf", g, ri)], allow_slow_non_contiguous=True)
                    kb.dma("sync", CTf[two * 64:(two + 1) * 64, j, ri, gl * 16:(gl + 1) * 16], cd[g].rearrange("c p -> p c"),
                           reads=["CTf"], writes=[("CTf", g, ri)], allow_slow_non_contiguous=True)
            bkeys = [("BTf", g, ri) for g in range(16) for ri in range(2)]
            ckeys = [("CTf", g, ri) for g in range(16) for ri in range(2)]
            V(lambda e: e.tensor_copy(out=BT[:], in_=BTf[:]), bkeys, ["BT"])
            c1 = S("c1", [128, 8, 128], F32)
            c2 = S("c2", [128, 8, 128], F32)
            cr_b = self.bc_last(cfr[:, :], 128)
            ci_b = self.bc_last(cfi[:, :], 128)
            V(lambda e: e.tensor_tensor(out=c1[:], in0=CTf[:, :, 0, :], in1=cr_b, op=ALU.mult), ckeys + ["cfr"], ["c1"])
            V(lambda e: e.tensor_tensor(out=c2[:], in0=CTf[:, :, 1, :], in1=ci_b, op=ALU.mult), ckeys + ["cfi"], ["c2"])
            V(lambda e: e.tensor_tensor(out=CT[:, :, 0, :], in0=c1[:], in1=c2[:], op=ALU.subtract), ["c1", "c2"], [("CT", 0)])
            V(lambda e: e.tensor_tensor(out=c1[:], in0=CTf[:, :, 0, :], in1=ci_b, op=ALU.mult), ckeys + ["cfi"], ["c1"])
            V(lambda e: e.tensor_tensor(out=c2[:], in0=CTf[:, :, 1, :], in1=cr_b, op=ALU.mult), ckeys + ["cfr"], ["c2"])
            V(lambda e: e.scalar_tensor_tensor(out=CT[:, :, 1, :], in0=c1[:], scalar=-1.0, in1=c2[:], op0=ALU.mult, op1=ALU.subtract),
              ["c1", "c2"], [("CT", 1)])
            Gw = S("Gw", [128, 2, 256], BF16)
            kb.dma("gpsimd", Gw[:], gw_d.rearrange("(m p) n -> p m n", p=128), writes=["Gw"])
            dsk = S("dsk", [128, 2], F32)
            gbi = S("gbi", [128, 2], F32)
            kb.dma("sync", dsk[:], dsk_d.rearrange("(m p) -> p m", p=128), writes=["dsk"], allow_slow_non_contiguous=True)
            kb.dma("sync", gbi[:], gb_d.rearrange("(m p) -> p m", p=128), writes=["gbi"], allow_slow_non_contiguous=True)
            h0r = S("h0r", [128, 8], F32)
            h0i = S("h0i", [128, 8], F32)
            kb.op("gpsimd", lambda e: e.memset(h0r[:], 0.0), writes=["h0r"])
            kb.op("gpsimd", lambda e: e.memset(h0i[:], 0.0), writes=["h0i"])
            uin = [S("uin%d" % i, [128, 2, L], F32) for i in range(2)]
            ub = S("ub", [128, 2, L], BF16)
            xr_ = [S("xr%d" % i, [128, L], F32) for i in range(2)]
            xi_ = [S("xi%d" % i, [128, L], F32) for i in range(2)]
            p1_ = [S("p1%d" % i, [128, L], F32) for i in range(4)]
            p2_ = [S("p2%d" % i, [128, L], F32) for i in range(4)]
            zr_ = [S("zr%d" % i, [128, L], F32) for i in range(2)]
            zi_ = [S("zi%d" % i, [128, L], F32) for i in range(2)]
            gr_ = [S("gr%d" % i, [128, L], F32) for i in range(2)]
            gi_ = [S("gi%d" % i, [128, L], F32) for i in range(2)]
            hr = [S("hr%d" % i, [128, L], BF16) for i in range(2)]
            hi = [S("hi%d" % i, [128, L], BF16) for i in range(2)]
            yy = S("yy", [128, 2, L], F32)
            y2 = S("y2", [128, L], F32)
            zb = S("zb", [128, 2, L], BF16)
            zf = S("zf", [128, 2, L], F32)
            sgl = S("sgl", [128, L], F32)
            yo = [S("yo%d" % i, [128, 2, L], BF16) for i in range(2)]
            uv = u_fm.rearrange("(m p) t -> p m t", p=128)
            yv = y_fm.rearrange("(m p) t -> p m t", p=128)
            hb_i = 0
            for blk in range(T // L):
                t0 = blk * L
                ib = blk % 2
                kb.dma("sync", uin[ib][:], uv[:, :, t0:t0 + L], writes=["uin%d" % ib])
                V(lambda e, ib=ib: e.tensor_copy(out=ub[:], in_=uin[ib][:]), ["uin%d" % ib], ["ub"])
                psy = [self.bank(0), self.bank(1)]
                def _tile(j):
                    nonlocal hb_i
                    m = j // 4
                    jb = j % 2
                    xr, xi, zr, zi, gr, gi = xr_[jb], xi_[jb], zr_[jb], zi_[jb], gr_[jb], gi_[jb]
                    kxr_, kxi_, kzr, kzi, kgr, kgi = ['%s%d' % (n_, jb) for n_ in ('xr', 'xi', 'zr', 'zi', 'gr', 'gi')]
                    pxr, kxr = self.bank(2 + (j % 2) * 2)
                    pxi, kxi = self.bank(3 + (j % 2) * 2)
                    kb.op("tensor", lambda e, j=j, m=m, pxr=pxr: e.matmul(out=pxr[:, 0:L], lhsT=BT[:, j, 0, :], rhs=ub[:, m, :], start=True, stop=True),
                          reads=["BT", "ub"], writes=[kxr])
                    kb.op("tensor", lambda e, j=j, m=m, pxi=pxi: e.matmul(out=pxi[:, 0:L], lhsT=BT[:, j, 1, :], rhs=ub[:, m, :], start=True, stop=True),
                          reads=["BT", "ub"], writes=[kxi])
                    kb.op("scalar", lambda e, pxr=pxr: e.copy(out=xr[:], in_=pxr[:, 0:L]), reads=[kxr], writes=[kxr_])
                    kb.op("scalar", lambda e, pxi=pxi: e.copy(out=xi[:], in_=pxi[:, 0:L]), reads=[kxi], writes=[kxi_])
                    V(lambda e, j=j: e.tensor_tensor(out=p1_[0][:], in0=xr[:], in1=Ct[:, j, :], op=ALU.mult), [kxr_, "Ct"], ["p1_0"])
                    kb.op("gpsimd", lambda e, j=j: e.tensor_tensor(out=p2_[0][:], in0=xi[:], in1=Sn[:, j, :], op=ALU.mult), reads=[kxi_, "Sn"], writes=["p2_0"])
                    V(lambda e: e.tensor_tensor(out=zr[:], in0=p1_[0][:], in1=p2_[0][:], op=ALU.add), ["p1_0", "p2_0"], [kzr])
                    V(lambda e, j=j: e.tensor_tensor(out=p1_[1][:], in0=xi[:], in1=Ct[:, j, :], op=ALU.mult), [kxi_, "Ct"], ["p1_1"])
                    kb.op("gpsimd", lambda e, j=j: e.tensor_tensor(out=p2_[1][:], in0=xr[:], in1=Sn[:, j, :], op=ALU.mult), reads=[kxr_, "Sn"], writes=["p2_1"])
                    V(lambda e: e.tensor_tensor(out=zi[:], in0=p1_[1][:], in1=p2_[1][:], op=ALU.subtract), ["p1_1", "p2_1"], [kzi])
                    rj = rho[:, j:j + 1]
                    rb = bass.AP(rj.tensor, rj.offset, [list(rj.ap[0]), [0, L]])
                    V(lambda e, j=j, rb=rb: e.tensor_tensor_scan(out=gr[:], data0=rb, data1=zr[:], initial=h0r[:, j:j + 1], op0=ALU.mult, op1=ALU.add),
                      ["rho", kzr, ("h0r", j)], [kgr])
                    V(lambda e, j=j, rb=rb: e.tensor_tensor_scan(out=gi[:], data0=rb, data1=zi[:], initial=h0i[:, j:j + 1], op0=ALU.mult, op1=ALU.add),
                      ["rho", kzi, ("h0i", j)], [kgi])
                    hb = hb_i % 2
                    hb_i += 1
                    V(lambda e, j=j: e.tensor_tensor(out=p1_[2][:], in0=gr[:], in1=Ct[:, j, :], op=ALU.mult), [kgr, "Ct"], ["p1_2"])
                    kb.op("gpsimd", lambda e, j=j: e.tensor_tensor(out=p2_[2][:], in0=gi[:], in1=Sn[:, j, :], op=ALU.mult), reads=[kgi, "Sn"], writes=["p2_2"])
                    V(lambda e, hb=hb: e.tensor_tensor(out=hr[hb][:], in0=p1_[2][:], in1=p2_[2][:], op=ALU.subtract), ["p1_2", "p2_2"], ["hr%d" % hb])
                    V(lambda e, j=j: e.tensor_tensor(out=h0r[:, j:j + 1], in0=p1_[2][:, L - 1:L], in1=p2_[2][:, L - 1:L], op=ALU.subtract), ["p1_2", "p2_2"], [("h0r", j)])
                    V(lambda e, j=j: e.tensor_tensor(out=p1_[3][:], in0=gi[:], in1=Ct[:, j, :], op=ALU.mult), [kgi, "Ct"], ["p1_3"])
                    kb.op("gpsimd", lambda e, j=j: e.tensor_tensor(out=p2_[3][:], in0=gr[:], in1=Sn[:, j, :], op=ALU.mult), reads=[kgr, "Sn"], writes=["p2_3"])
                    V(lambda e, hb=hb: e.tensor_tensor(out=hi[hb][:], in0=p1_[3][:], in1=p2_[3][:], op=ALU.add), ["p1_3", "p2_3"], ["hi%d" % hb])
                    V(lambda e, j=j: e.tensor_tensor(out=h0i[:, j:j + 1], in0=p1_[3][:, L - 1:L], in1=p2_[3][:, L - 1:L], op=ALU.add), ["p1_3", "p2_3"], [("h0i", j)])
                    py, ky = psy[m]
                    kb.op("tensor", lambda e, j=j, hb=hb, py=py: e.matmul(out=py[:, 0:L], lhsT=CT[:, j, 0, :], rhs=hr[hb][:], start=(j % 4 == 0), stop=False),
                          reads=[("CT", 0), "hr%d" % hb], writes=[ky])
                    kb.op("tensor", lambda e, j=j, hb=hb, py=py: e.matmul(out=py[:, 0:L], lhsT=CT[:, j, 1, :], rhs=hi[hb][:], start=False, stop=(j % 4 == 3)),
                          reads=[("CT", 1), "hi%d" % hb], writes=[ky])
                for j in range(8):
                    _tile(j)
                for m in range(2):
                    py, ky = psy[m]
                    V(lambda e, m=m, py=py, ib=ib: e.scalar_tensor_tensor(out=yy[:, m, :], in0=uin[ib][:, m, :], scalar=dsk[:, m:m + 1], in1=py[:, 0:L],
                                                                    op0=ALU.mult, op1=ALU.add), [ky, "dsk", "uin%d" % ib], [("yy", m)])
                    V(lambda e, m=m: e.tensor_tensor(out=y2[:], in0=yy[:, m, :], in1=yy[:, m, :], op=ALU.mult), [("yy", m)], ["y2"])
                    V(lambda e: e.tensor_scalar(out=y2[:], in0=y2[:], scalar1=0.044715, scalar2=1.0, op0=ALU.mult, op1=ALU.add), ["y2"], ["y2"])
                    V(lambda e, m=m: e.tensor_tensor(out=y2[:], in0=y2[:], in1=yy[:, m, :], op=ALU.mult), ["y2", ("yy", m)], ["y2"])
                    kb.op("scalar", lambda e: e.activation(out=y2[:], in_=y2[:], func=AF.Sigmoid, scale=1.5957691216057308), reads=["y2"], writes=["y2"])
                    V(lambda e, m=m: e.tensor_tensor(out=zf[:, m, :], in0=y2[:], in1=yy[:, m, :], op=ALU.mult), ["y2", ("yy", m)], [("zf", m)])
                    kb.op("gpsimd", lambda e, m=m: e.tensor_copy(out=zb[:, m, :], in_=zf[:, m, :]), reads=[("zf", m)], writes=[("zb", m)])
                for m2 in range(2):
                    pg, kg = self.bank(2 + m2)
                    for m in range(2):
                        kb.op("tensor", lambda e, m=m, m2=m2, pg=pg: e.matmul(out=pg[:, 0:L], lhsT=Gw[:, m, m2 * 128:(m2 + 1) * 128], rhs=zb[:, m, :],
                                                                           start=(m == 0), stop=(m == 1)), reads=["Gw", ("zb", 0), ("zb", 1)], writes=[kg])
                    kb.op("scalar", lambda e, m2=m2, pg=pg: e.activation(out=sgl[:], in_=pg[:, 0:L], func=AF.Sigmoid, bias=gbi[:, m2:m2 + 1]),
                          reads=[kg, "gbi"], writes=["sgl"])
                    V(lambda e, m2=m2, ib=ib: e.tensor_tensor(out=yo[ib][:, m2, :], in0=sgl[:], in1=zf[:, m2, :], op=ALU.mult), ["sgl", ("zf", m2)], [("yo", ib, m2)])
                kb.dma("gpsimd", yv[:, :, t0:t0 + L], yo[ib][:], reads=[("yo", ib, 0), ("yo", ib, 1)])
            kb.flush()

    def phase_rwkv7(self, u_fm, mu_d, w0_d, w2_d, a0_d, a2_d, g2_d, kk_d, ka_d, rk_d, lnw_d, lnb_d, y_tm):
        nc, kb, T = self.nc, self.kb, self.T
        TB = min(512, T)
        NCH = TB // 64
        NP = TB // 128
        with ExitStack() as st:
            S = lambda name, shape, dt: sb(st, nc, name, shape, dt)
            V = lambda fn, r, w: kb.op("vector", fn, reads=r, writes=w)
            G = lambda fn, r, w: kb.op("gpsimd", fn, reads=r, writes=w)
            A = lambda fn, r, w: kb.op("scalar", fn, reads=r, writes=w)
            PE = lambda fn, r, w: kb.op("tensor", fn, reads=r, writes=w)
            mu = S("mu", [128, 14], F32)
            kb.dma("sync", mu[:, 0:13], mu_d[0:1664].rearrange("(c p) -> p c", p=128), writes=[("mu", 0)], allow_slow_non_contiguous=True)
            kb.dma("sync", mu[0:32, 13:14], mu_d[1664:1696].rearrange("(c p) -> p c", p=32), writes=[("mu", 1)], allow_slow_non_contiguous=True)
            mukeys = [("mu", 0), ("mu", 1)]
            pcs = {}
            for nm, dd in (("w0", w0_d), ("a0", a0_d), ("kk", kk_d), ("ka", ka_d), ("rk", rk_d)):
                t = S("pc_" + nm, [128, 4], F32)
                kb.dma("sync", t[:], dd.rearrange("(c p) -> p c", p=128), writes=["pc_" + nm], allow_slow_non_contiguous=True)
                pcs[nm] = t
            omka = S("omka", [128, 4], F32)
            V(lambda e: e.tensor_scalar(out=omka[:], in0=pcs["ka"][:], scalar1=-1.0, scalar2=1.0, op0=ALU.mult, op1=ALU.add), ["pc_ka"], ["omka"])
            LW = S("LW", [128, 4, 512], BF16)
            G(lambda e: e.memset(LW[:], 0.0), [], ["LW"])
            kb.dma("gpsimd", LW[0:32, 0, :], w2_d, reads=["LW"], writes=[("LW", 0)])
            kb.dma("gpsimd", LW[32:64, 1, :], a2_d, reads=["LW"], writes=[("LW", 1)])
            kb.dma("gpsimd", LW[64:128, 2, :], g2_d[0:64, :], reads=["LW"], writes=[("LW", 2)])
            kb.dma("gpsimd", LW[0:32, 3, :], g2_d[64:96, :], reads=["LW"], writes=[("LW", 3)])
            lnw_bc = S("lnw_bc", [128, 512], F32)
            lnb_bc = S("lnb_bc", [128, 512], F32)
            self.load_bcast(lnw_bc[:], lnw_d, 512, "lnw_bc")
            self.load_bcast(lnb_bc[:], lnb_d, 512, "lnb_bc")
            blk2 = S("blk2", [128, 128], F32)
            IND = S("IND", [128, 2], BF16)
            G(lambda e: e.memset(blk2[:], 0.0), [], ["blk2"])
            G(lambda e: e.memset(blk2[0:64, 0:64], 1.0), [], ["blk2"])
            G(lambda e: e.memset(blk2[64:128, 64:128], 1.0), [], ["blk2"])
            G(lambda e: e.memset(IND[:], 0.0), [], ["IND"])
            G(lambda e: e.memset(IND[0:64, 0:1], 1.0), [], ["IND"])
            G(lambda e: e.memset(IND[64:128, 1:2], 1.0), [], ["IND"])
            MG = S("MG", [128, 128], F32)
            for rh in range(2):
                for ch in range(2):
                    G(lambda e, rh=rh, ch=ch: e.affine_select(
                        out=MG[rh * 64:(rh + 1) * 64, ch * 64:(ch + 1) * 64], in_=self.ones_f[rh * 64:(rh + 1) * 64, 0:64], pattern=[[1, 64]],
                        compare_op=ALU.is_ge, fill=0.0, base=(-1 if ch == 0 else 0), channel_multiplier=-1), ["ones_f"], ["MG"])
            ML = S("ML", [64, 64], F32)
            G(lambda e: e.affine_select(out=ML[:], in_=self.ones_f[0:64, 0:64], pattern=[[-1, 64]], compare_op=ALU.is_ge, fill=0.0,
                                        base=-1, channel_multiplier=1), ["ones_f"], ["ML"])
            rmask = S("rmask", [128, TB], F32)
            G(lambda e: e.memset(rmask[:], 1.0), [], ["rmask"])
            G(lambda e: e.memset(rmask[:].rearrange("p (c l) -> p c l", l=64)[:, :, 0:1], 0.0), [], ["rmask"])
            tiny = S("tiny", [128, 1], F32)
            gneps = S("gneps", [128, 1], F32)
            G(lambda e: e.memset(tiny[:], 1e-12), [], ["tiny"])
            G(lambda e: e.memset(gneps[:], GN_EPS), [], ["gneps"])
            uin = S("uin", [128, 14, TB + 1], F32)
            dtmp = S("dtmp", [128, TB], F32)
            lo = S("lo", [128, 2, TB], BF16)
            g_sb = S("g_sb", [128, NP, 512], F32)
            lw = S("lw", [128, TB], F32)
            av = S("av", [128, TB], F32)
            kq = S("kq", [128, TB], F32)
            sq = S("sq", [128, TB], F32)
            rn = S("rn", [128, TB], F32)
            kkv = S("kkv", [128, TB], F32)
            kp = S("kp", [128, TB], F32)
            cc = S("cc", [128, TB], F32)
            w1 = S("w1", [128, TB], F32)
            w2t = S("w2t", [128, TB], F32)
            er = S("er", [128, TB], F32)
            AR = S("AR", [128, 4, 2, NCH, 2, 64], BF16)
            Zv = [S("Zv%d" % i, [128, 8, 64], BF16) for i in range(2)]
            BK = S("BK", [128, 4, NCH, 2, 64], BF16)
            WL = S("WL", [128, 4, NCH], F32)
            xvb = S("xvb", [128, 4, 64 + TB], BF16)
            rkb = S("rkb", [128, 4, TB], BF16)
            Z = [S("Z%d" % i, [128, 8, 64], BF16) for i in range(2)]
            Gm = [S("Gm%d" % i, [128, 8, 128], BF16) for i in range(2)]
            BKtm = [S("BKtm%d" % i, [128, 8, 64], BF16) for i in range(2)]
            An = [[S("An%d_%d" % (q, i), [64, 8, 64], BF16) for i in range(2)] for q in range(2)]
            Bn = [[S("Bn%d_%d" % (q, i), [64, 8, 64], BF16) for i in range(2)] for q in range(2)]
            MT = [S("MT%d" % i, [64, 8, 64], BF16) for i in range(2)]
            Xb = [S("Xb%d" % i, [64, 8, 64], BF16) for i in range(2)]
            ST = S("ST", [128, 4, 64], F32)
            STb = S("STb", [128, 4, 64], BF16)
            stmp = S("stmp", [128, 4, 64], F32)
            ysb = S("ysb", [128, 512], F32)
            ysq = S("ysq", [128, 512], F32)
            s1 = S("s1", [128, 8], F32)
            s2 = S("s2", [128, 8], F32)
            mean = S("mean", [128, 8], F32)
            var = S("var", [128, 8], F32)
            bsum = S("bsum", [128, 8], F32)
            yt1 = S("yt1", [128, 512], F32)
            yt2 = S("yt2", [128, 512], F32)
            yo = [S("yo%d" % i, [128, 512], BF16) for i in range(2)]
            G(lambda e: e.memset(ST[:], 0.0), [], ["ST"])
            G(lambda e: e.memset(STb[:], 0.0), [], ["STb"])
            G(lambda e: e.memset(xvb[:], 0.0), [], ["xvb"])
            G(lambda e: e.memset(uin[:, :, 0:1], 0.0), [], ["uin"])
            G(lambda e: e.memset(AR[:], 0.0), [], ["AR"])
            G(lambda e: e.memset(Zv[0][:], 0.0), [], ["Zv"])
            G(lambda e: e.memset(Zv[1][:], 0.0), [], ["Zv"])
            G(lambda e: e.memset(lo[:], 0.0), [], ["lo"])
            identb64 = self.ident[0:64, 0:64]
            uv = u_fm[0:1664, :].rearrange("(c p) t -> p c t", p=128)
            mgb = bass.AP(MG[:].tensor, MG[:].offset, [list(MG[:].ap[0]), [0, 4], [1, 128]])
            mlb = bass.AP(ML[:].tensor, ML[:].offset, [list(ML[:].ap[0]), [0, 8], [1, 64]])
            idb = bass.AP(self.identf[0:64, 0:64].tensor, self.identf[0:64, 0:64].offset, [list(self.identf[0:64, 0:64].ap[0]), [0, 8], [1, 64]])
            oi = 0
            for tb in range(T // TB):
                t0 = tb * TB
                if tb == 0:
                    kb.dma("sync", uin[:, 0:13, 1:TB + 1], uv[:, :, 0:TB], reads=["uin"], writes=[("uin", 0)] + ["x%d" % j_ for j_ in range(13)])
                    kb.dma("sync", uin[0:32, 13, 1:TB + 1], u_fm[1664:1696, 0:TB], reads=["uin"], writes=[("uin", 1), "x13"])
                else:
                    kb.dma("sync", uin[:, 0:13, :], uv[:, :, t0 - 1:t0 + TB], reads=["uin"], writes=[("uin", 0)] + ["x%d" % j_ for j_ in range(13)])
                    kb.dma("sync", uin[0:32, 13, :], u_fm[1664:1696, t0 - 1:t0 + TB], reads=["uin"], writes=[("uin", 1), "x13"])
                ukeys = [("uin", 0), ("uin", 1)]
                for j in range(14):
                    pp = 128 if j < 13 else 32
                    V(lambda e, j=j, pp=pp: e.tensor_tensor(out=dtmp[0:pp, :], in0=uin[0:pp, j, 0:TB], in1=uin[0:pp, j, 1:TB + 1], op=ALU.subtract),
                      ukeys + ["x%d" % (j - 1)], ["dtmp"])
                    V(lambda e, j=j, pp=pp: e.scalar_tensor_tensor(out=uin[0:pp, j, 1:TB + 1], in0=dtmp[0:pp, :], scalar=mu[0:pp, j:j + 1],
                                                                   in1=uin[0:pp, j, 1:TB + 1], op0=ALU.mult, op1=ALU.add),
                      ukeys + ["dtmp"] + mukeys, ["x%d" % j])
                X = lambda j: uin[:, j, 1:TB + 1]
                A(lambda e: e.activation(out=lo[0:32, 0, :], in_=uin[0:32, 12, 1:TB + 1], func=AF.Tanh), ["x12", "lo"], [("lo", 0)])
                A(lambda e: e.copy(out=lo[32:64, 0, :], in_=uin[32:64, 12, 1:TB + 1]), ["x12"], [("lo", 1)])
                A(lambda e: e.activation(out=lo[64:128, 0, :], in_=uin[64:128, 12, 1:TB + 1], func=AF.Sigmoid), ["x12"], [("lo", 2)])
                A(lambda e: e.activation(out=lo[0:32, 1, :], in_=uin[0:32, 13, 1:TB + 1], func=AF.Sigmoid), ["x13"], [("lo", 3)])
                lokeys = [("lo", i) for i in range(4)]
                for tt in range(NP):
                    pg, kg = self.bank(tt % 2)
                    PE(lambda e, tt=tt, pg=pg: e.matmul(out=pg[:, :], lhsT=lo[:, 0, tt * 128:(tt + 1) * 128], rhs=LW[:, 2, :], start=True, stop=False),
                       lokeys + [("LW", 2)], [kg])
                    PE(lambda e, tt=tt, pg=pg: e.matmul(out=pg[:, :], lhsT=lo[:, 1, tt * 128:(tt + 1) * 128], rhs=LW[:, 3, :], start=False, stop=True),
                       lokeys + [("LW", 3)], [kg])
                    A(lambda e, tt=tt, pg=pg: e.copy(out=g_sb[:, tt, :], in_=pg[:, :]), [kg], [("g_sb", tt)])
                for ti in range(4):
                    G(lambda e, ti=ti: e.tensor_copy(out=xvb[:, ti, 64:64 + TB], in_=uin[:, 8 + ti, 1:TB + 1]), ["x%d" % (8 + ti)], [("xvb", ti)])
                for ti in range(4):
                    cs4 = slice(ti * 128, (ti + 1) * 128)
                    pw, kw_ = self.bank(2)
                    pa, ka_ = self.bank(3)
                    PE(lambda e, pw=pw, cs4=cs4: e.matmul(out=pw[:, 0:TB], lhsT=LW[:, 0, cs4], rhs=lo[:, 0, :], start=True, stop=True),
                       lokeys + [("LW", 0)], [kw_])
                    PE(lambda e, pa=pa, cs4=cs4: e.matmul(out=pa[:, 0:TB], lhsT=LW[:, 1, cs4], rhs=lo[:, 0, :], start=True, stop=True),
                       lokeys + [("LW", 1)], [ka_])
                    A(lambda e, pw=pw, ti=ti: e.activation(out=lw[:], in_=pw[:, 0:TB], func=AF.Sigmoid, bias=pcs["w0"][:, ti:ti + 1]), [kw_, "pc_w0"], ["lw"])
                    A(lambda e, pa=pa, ti=ti: e.activation(out=av[:], in_=pa[:, 0:TB], func=AF.Sigmoid, bias=pcs["a0"][:, ti:ti + 1]), [ka_, "pc_a0"], ["av"])
                    V(lambda e: e.tensor_scalar(out=lw[:], in0=lw[:], scalar1=-0.6065306597126334, scalar2=None, op0=ALU.mult), ["lw"], ["lw"])
                    V(lambda e, ti=ti: e.tensor_scalar(out=kq[:], in0=X(4 + ti), scalar1=pcs["kk"][:, ti:ti + 1], scalar2=None, op0=ALU.mult),
                      ["x%d" % (4 + ti), "pc_kk"], ["kq"])
                    G(lambda e: e.tensor_tensor(out=sq[:], in0=kq[:], in1=kq[:], op=ALU.mult), ["kq"], ["sq"])
                    pn, kn = self.bank(4)
                    PE(lambda e, pn=pn: e.matmul(out=pn[:, 0:TB], lhsT=blk2[:], rhs=sq[:], start=True, stop=True), ["blk2", "sq"], [kn])
                    A(lambda e, pn=pn: e.activation(out=rn[:], in_=pn[:, 0:TB], func=AF.Ln, bias=tiny[:]), [kn, "tiny"], ["rn"])
                    A(lambda e: e.activation(out=rn[:], in_=rn[:], func=AF.Exp, scale=-0.5), ["rn"], ["rn"])
                    V(lambda e: e.tensor_tensor(out=kkv[:], in0=kq[:], in1=rn[:], op=ALU.mult), ["kq", "rn"], ["kkv"])
                    V(lambda e, ti=ti: e.tensor_scalar(out=kp[:], in0=av[:], scalar1=pcs["ka"][:, ti:ti + 1], scalar2=omka[:, ti:ti + 1], op0=ALU.mult, op1=ALU.add),
                      ["av", "pc_ka", "omka"], ["kp"])
                    V(lambda e, ti=ti: e.tensor_tensor(out=kp[:], in0=kp[:], in1=X(4 + ti), op=ALU.mult), ["kp", "x%d" % (4 + ti)], ["kp"])
                    V(lambda e, ti=ti: e.scalar_tensor_tensor(out=rkb[:, ti, :], in0=kp[:], scalar=pcs["rk"][:, ti:ti + 1], in1=X(ti), op0=ALU.mult, op1=ALU.mult),
                      ["kp", "pc_rk", "x%d" % ti], [("rkb", ti)])
                    V(lambda e: e.tensor_tensor_scan(out=cc[:], data0=rmask[:], data1=lw[:], initial=0.0, op0=ALU.mult, op1=ALU.add), ["rmask", "lw"], ["cc"])
                    A(lambda e: e.activation(out=er[:], in_=cc[:], func=AF.Exp), ["cc"], ["er"])
                    V(lambda e, ti=ti: e.tensor_copy(out=WL[:, ti, :], in_=er[:].rearrange("p (c l) -> p c l", l=64)[:, :, 63]), ["er"], [("WL", ti)])
                    c3 = lambda t_: t_[:].rearrange("p (c l) -> p c l", l=64)
                    for pr in range(2):
                        hp = slice(pr * 64, pr * 64 + 64)
                        V(lambda e, ti=ti, pr=pr, hp=hp: e.tensor_tensor(out=AR[hp, ti, pr, :, 1, :], in0=er[hp, :].rearrange("p (c l) -> p c l", l=64),
                                                                       in1=uin[hp, ti, 1:TB + 1].rearrange("p (c l) -> p c l", l=64), op=ALU.mult),
                          ["er", "x%d" % ti, "AR"], [("AR", ti, pr, 1)])
                    V(lambda e: e.tensor_tensor(out=w1[:], in0=cc[:], in1=lw[:], op=ALU.subtract), ["cc", "lw"], ["w1"])
                    A(lambda e: e.activation(out=w1[:], in_=w1[:], func=AF.Exp), ["w1"], ["w1"])
                    for pr in range(2):
                        hp = slice(pr * 64, pr * 64 + 64)
                        V(lambda e, ti=ti, pr=pr, hp=hp: e.scalar_tensor_tensor(out=AR[hp, ti, pr, :, 0, :], in0=kkv[hp, :].rearrange("p (c l) -> p c l", l=64), scalar=-1.0,
                                                                              in1=w1[hp, :].rearrange("p (c l) -> p c l", l=64), op0=ALU.mult, op1=ALU.mult),
                          ["kkv", "w1", "AR"], [("AR", ti, pr, 0)])
                    A(lambda e: e.activation(out=w2t[:], in_=cc[:], func=AF.Exp, scale=-1.0), ["cc"], ["w2t"])
                    G(lambda e: e.tensor_tensor(out=kkv[:], in0=kkv[:], in1=av[:], op=ALU.mult), ["kkv", "av"], ["kkv"])
                    V(lambda e, ti=ti: e.tensor_tensor(out=BK[:, ti, :, 0, :], in0=c3(kkv), in1=c3(w2t), op=ALU.mult), ["kkv", "w2t"], [("BK", ti)])
                    V(lambda e, ti=ti: e.tensor_tensor(out=BK[:, ti, :, 1, :], in0=c3(kp), in1=c3(w2t), op=ALU.mult), ["kp", "w2t"], [("BK", ti)])
                arkeys = [("AR", ti, pr, x) for ti in range(4) for pr in range(2) for x in range(2)]
                bkkeys = [("BK", ti) for ti in range(4)]
                for cp in range(NCH // 2):
                    cks = (2 * cp, 2 * cp + 1)
                    bA = lambda q: self.bank(3 * q)
                    bB = lambda q: self.bank(3 * q + 1)
                    bM = lambda q: self.bank(3 * q + 2)
                    for q, c in enumerate(cks):
                        pv, kv = self.psb[q], "psb%d" % q
                        for ti in range(4):
                            PE(lambda e, ti=ti, c=c, pv=pv: e.transpose(out=pv[:, ti * 128:(ti + 1) * 128], in_=xvb[:, ti, c * 64:c * 64 + 128], identity=self.ident[:]),
                               [("xvb", ti), "ident"], [kv])
                        for ti in range(4):
                            PE(lambda e, ti=ti, c=c, pv=pv: e.transpose(out=pv[:, 512 + ti * 128:512 + (ti + 1) * 128], in_=BK[:, ti, c, :, :], identity=self.ident[:]),
                               bkkeys + ["ident"], [kv])
                        (pa, ka), (pb, kbk), (pm, km) = bA(q), bB(q), bM(q)
                        for h in range(8):
                            ti = h // 2
                            pg, kg = (pa, ka) if h < 4 else (pb, kbk)
                            PE(lambda e, h=h, ti=ti, c=c, pg=pg: e.matmul(out=pg[:, (h % 4) * 128:(h % 4 + 1) * 128], lhsT=BK[:, ti, c, :, :], rhs=AR[:, ti, h % 2, c, :, :],
                                                                      start=True, stop=True), arkeys + bkkeys, [kg])
                        for h in range(8):
                            ti = h // 2
                            PE(lambda e, h=h, ti=ti, c=c, pm=pm: e.matmul(out=pm[0:64, h * 64:(h + 1) * 64], lhsT=AR[:, ti, h % 2, c, 0, :], rhs=BK[:, ti, c, 0, :],
                                                                      start=True, stop=True), arkeys + bkkeys, [km])
                    for q, c in enumerate(cks):
                        pv, kv = self.psb[q], "psb%d" % q
                        (pa, ka), (pb, kbk), (pm, km) = bA(q), bB(q), bM(q)
                        A(lambda e, pv=pv, q=q: e.copy(out=Z[q][64:128, :, :], in_=pv[64:128, 0:512].rearrange("p (h v) -> p h v", v=64)), [kv], [("Z1", q)])
                        A(lambda e, pv=pv, q=q: e.copy(out=Zv[q][64:128, :, :], in_=pv[64:128, 0:512].rearrange("p (h v) -> p h v", v=64)), [kv, "Zv"], [("Zv1", q)])
                        A(lambda e, pv=pv, q=q: e.copy(out=BKtm[q][:], in_=pv[:, 512:1024].rearrange("p (h k) -> p h k", k=64)), [kv], [("BKtm", q)])
                        V(lambda e, pa=pa, q=q: e.tensor_tensor(out=Gm[q][:, 0:4, :], in0=pa[:, :].rearrange("p (h t) -> p h t", t=128), in1=mgb, op=ALU.mult),
                          [ka, "MG"], [("Gm", q, 0)])
                        V(lambda e, pb=pb, q=q: e.tensor_tensor(out=Gm[q][:, 4:8, :], in0=pb[:, :].rearrange("p (h t) -> p h t", t=128), in1=mgb, op=ALU.mult),
                          [kbk, "MG"], [("Gm", q, 1)])
                        V(lambda e, pm=pm, q=q: e.tensor_tensor(out=Bn[q][0][:], in0=pm[0:64, :].rearrange("p (h t) -> p h t", t=64), in1=mlb, op=ALU.mult),
                          [km, "ML"], [("Bn", q, 0)])
                        A(lambda e, q=q: e.copy(out=An[q][0][:], in_=Gm[q][0:64, :, 0:64]), [("Gm", q, 0), ("Gm", q, 1)], [("An", q, 0)])
                        V(lambda e, q=q: e.tensor_tensor(out=MT[q][:], in0=An[q][0][:], in1=idb, op=ALU.add), [("An", q, 0), "identf"], [("MT", q)])
                    for r in range(1, 7):
                        o, n_ = (r - 1) % 2, r % 2
                        for q in range(2):
                            (pa, ka), (pb, kbk), (pm, km) = bA(q), bB(q), bM(q)
                            a_o, b_o = An[q][o], Bn[q][o]
                            if r <= 4:
                                for h in range(8):
                                    PE(lambda e, h=h, pa=pa, a_o=a_o, b_o=b_o: e.matmul(out=pa[0:64, h * 64:(h + 1) * 64], lhsT=b_o[:, h, :], rhs=a_o[:, h, :], start=True, stop=True),
                                       [("An", q, o), ("Bn", q, o)], [ka])
                            if r <= 5:
                                for h in range(8):
                                    PE(lambda e, h=h, pb=pb, a_o=a_o, b_o=b_o: e.matmul(out=pb[0:64, h * 64:(h + 1) * 64], lhsT=a_o[:, h, :], rhs=b_o[:, h, :], start=True, stop=True),
                                       [("An", q, o), ("Bn", q, o)], [kbk])
                            if r >= 2:
                                for h in range(8):
                                    PE(lambda e, h=h, pm=pm, b_o=b_o, q=q: e.matmul(out=pm[0:64, h * 64:(h + 1) * 64], lhsT=b_o[:, h, :], rhs=MT[q][:, h, :], start=True, stop=True),
                                       [("Bn", q, o), ("MT", q)], [km])
                        for q in range(2):
                            (pa, ka), (pb, kbk), (pm, km) = bA(q), bB(q), bM(q)
                            if r <= 4:
                                A(lambda e, pa=pa, q=q, n_=n_: e.copy(out=An[q][n_][:], in_=pa[0:64, :].rearrange("p (h t) -> p h t", t=64)), [ka], [("An", q, n_)])
                            if r <= 5:
                                V(lambda e, pb=pb, q=q, n_=n_: e.tensor_copy(out=Bn[q][n_][:], in_=pb[0:64, :].rearrange("p (h t) -> p h t", t=64)), [kbk], [("Bn", q, n_)])
                            if r >= 2:
                                V(lambda e, pm=pm, q=q: e.tensor_tensor(out=MT[q][:], in0=MT[q][:], in1=pm[0:64, :].rearrange("p (h t) -> p h t", t=64), op=ALU.add),
                                  [("MT", q), km], [("MT", q)])
                    for q, c in enumerate(cks):
                        gmk = [("Gm", q, 0), ("Gm", q, 1)]
                        px, kx = bA(q)
                        for h in range(8):
                            ti = h // 2
                            PE(lambda e, h=h, ti=ti, c=c, px=px: e.matmul(out=px[0:64, h * 64:(h + 1) * 64], lhsT=AR[:, ti, h % 2, c, 0, :], rhs=STb[:, ti, :], start=True, stop=False),
                               arkeys + ["STb"], [kx])
                            PE(lambda e, h=h, px=px, q=q: e.matmul(out=px[0:64, h * 64:(h + 1) * 64], lhsT=Gm[q][:, h, 0:64], rhs=Zv[q][:, h, :], start=False, stop=True),
                               gmk + [("Zv1", q), "Zv"], [kx])
                        A(lambda e, px=px, q=q: e.copy(out=Xb[q][:], in_=px[0:64, :].rearrange("p (h v) -> p h v", v=64)), [kx], [("Xb", q)])
                        pu, ku = bB(q)
                        for h in range(8):
                            PE(lambda e, h=h, pu=pu, q=q: e.matmul(out=pu[0:64, h * 64:(h + 1) * 64], lhsT=MT[q][:, h, :], rhs=Xb[q][:, h, :], start=True, stop=True),
                               [("MT", q), ("Xb", q)], [ku])
                        A(lambda e, pu=pu, q=q: e.copy(out=Z[q][0:64, :, :], in_=pu[0:64, :].rearrange("p (h v) -> p h v", v=64)), [ku], [("Z0", q)])
                        zk = [("Z0", q), ("Z1", q)]
                        ps_, ks_ = bA(q)
                        for h in range(8):
                            ti, pr = h // 2, h % 2
                            PE(lambda e, h=h, ti=ti, pr=pr, ps_=ps_, q=q: e.matmul(out=ps_[pr * 64:(pr + 1) * 64, ti * 64:(ti + 1) * 64], lhsT=BKtm[q][:, h, :], rhs=Z[q][:, h, :], start=True, stop=True),
                               [("BKtm", q)] + zk, [ks_])
                        py, ky = bM(q)
                        ro = q * 64
                        for h in range(8):
                            ti = h // 2
                            PE(lambda e, h=h, ti=ti, c=c, py=py, ro=ro: e.matmul(out=py[ro:ro + 64, h * 64:(h + 1) * 64], lhsT=AR[:, ti, h % 2, c, 1, :], rhs=STb[:, ti, :],
                                                                             start=True, stop=False), arkeys + ["STb"], [ky])
                            PE(lambda e, h=h, py=py, ro=ro, q=q: e.matmul(out=py[ro:ro + 64, h * 64:(h + 1) * 64], lhsT=Gm[q][:, h, 64:128], rhs=Z[q][:, h, :], start=False, stop=True),
                               gmk + zk, [ky])
                        V(lambda e, ps_=ps_: e.tensor_tensor(out=stmp[:], in0=ST[:], in1=ps_[:, 0:256].rearrange("p (a v) -> p a v", v=64), op=ALU.add), ["ST", ks_], ["stmp"])
                        V(lambda e, c=c: e.tensor_tensor(out=ST[:], in0=stmp[:], in1=self.bc_last(WL[:, :, c], 64), op=ALU.mult), ["stmp"] + [("WL", ti) for ti in range(4)], ["ST"])
                        A(lambda e: e.copy(out=STb[:], in_=ST[:]), ["ST"], ["STb"])
                        A(lambda e, py=py, ro=ro: e.copy(out=ysb[ro:ro + 64, :], in_=py[ro:ro + 64, :]), [ky], [("ysb", q)])
                    tt = cp
                    r0 = t0 + tt * 128
                    ob = oi % 2
                    oi += 1
                    ysk = [("ysb", 0), ("ysb", 1)]
                    A(lambda e: e.activation(out=ysq[:], in_=ysb[:], func=AF.Square), ysk, ["ysq"])
                    V(lambda e: e.tensor_reduce(out=s1[:], in_=ysb[:].rearrange("p (h v) -> p h v", v=64), axis=AX.X, op=ALU.add), ysk, ["s1"])
                    V(lambda e: e.tensor_reduce(out=s2[:], in_=ysq[:].rearrange("p (h v) -> p h v", v=64), axis=AX.X, op=ALU.add), ["ysq"], ["s2"])
                    V(lambda e: e.tensor_scalar(out=mean[:], in0=s1[:], scalar1=1.0 / 64, scalar2=None, op0=ALU.mult), ["s1"], ["mean"])
                    V(lambda e: e.tensor_tensor(out=var[:], in0=mean[:], in1=mean[:], op=ALU.mult), ["mean"], ["var"])
                    V(lambda e: e.scalar_tensor_tensor(out=var[:], in0=s2[:], scalar=1.0 / 64, in1=var[:], op0=ALU.mult, op1=ALU.subtract), ["s2", "var"], ["var"])
                    A(lambda e: e.activation(out=var[:], in_=var[:], func=AF.Sqrt, bias=gneps[:]), ["var", "gneps"], ["var"])
                    V(lambda e: e.reciprocal(out=var[:], in_=var[:]), ["var"], ["var"])
                    y3 = lambda t_: t_[:].rearrange("p (h v) -> p h v", v=64)
                    V(lambda e: e.tensor_tensor(out=y3(yt1), in0=y3(ysb), in1=self.bc_last(mean[:, :], 64), op=ALU.subtract), ysk + ["mean"], ["yt1"])
                    V(lambda e: e.tensor_tensor(out=y3(yt1), in0=y3(yt1), in1=self.bc_last(var[:, :], 64), op=ALU.mult), ["yt1", "var"], ["yt1"])
                    G(lambda e: e.tensor_tensor(out=yt1[:], in0=yt1[:], in1=lnw_bc[:], op=ALU.mult), ["yt1", "lnw_bc"], ["yt1"])
                    G(lambda e: e.tensor_tensor(out=yt1[:], in0=yt1[:], in1=lnb_bc[:], op=ALU.add), ["yt1", "lnb_bc"], ["yt1"])
                    pb2, kbn = self.bank(1)
                    for ti in range(4):
                        PE(lambda e, ti=ti, tt=tt, pb2=pb2: e.matmul(out=pb2[:, ti * 2:ti * 2 + 2], lhsT=rkb[:, ti, tt * 128:(tt + 1) * 128], rhs=IND[:], start=True, stop=True),
                           [("rkb", ti), "IND"], [kbn])
                    V(lambda e, pb2=pb2: e.tensor_copy(out=bsum[:], in_=pb2[:, 0:8]), [kbn], ["bsum"])
                    pv2, kv2 = self.psb[0], "psb0"
                    for ti in range(4):
                        PE(lambda e, ti=ti, tt=tt, pv2=pv2: e.transpose(out=pv2[:, ti * 128:(ti + 1) * 128], in_=xvb[:, ti, 64 + tt * 128:64 + (tt + 1) * 128], identity=self.ident[:]),
                           [("xvb", ti), "ident"], [kv2])
                    V(lambda e, pv2=pv2: e.tensor_tensor(out=y3(yt2), in0=pv2[:, 0:512].rearrange("p (h v) -> p h v", v=64), in1=self.bc_last(bsum[:, :], 64), op=ALU.mult),
                      [kv2, "bsum"], ["yt2"])
                    V(lambda e: e.tensor_tensor(out=yt2[:], in0=yt2[:], in1=yt1[:], op=ALU.add), ["yt2", "yt1"], ["yt2"])
                    V(lambda e, tt=tt, ob=ob: e.tensor_tensor(out=yo[ob][:], in0=yt2[:], in1=g_sb[:, tt, :], op=ALU.mult), ["yt2", ("g_sb", tt)], ["yo%d" % ob])
                    kb.dma("gpsimd", y_tm[r0:r0 + 128, :], yo[ob][:], reads=["yo%d" % ob])
            kb.flush()


PARAM_NAMES = ["norm_mix", "norm_ffn", "norm_final", "w_in_even", "w_out_even", "lb_table", "a_norm",
               "b_mu", "b_w0", "b_w2", "b_a0", "b_a2", "b_g2", "b_kk", "b_ka", "b_rk", "b_ln_w", "b_ln_b",
               "w_in_odd", "w_out_odd", "c_lam_re", "c_lam_im", "c_log_step", "c_b_re", "c_b_im",
               "c_c_re", "c_c_im", "c_d", "c_glu_w", "c_glu_b", "d_conv_q", "d_conv_k", "d_i_bias",
               "d_f_bias", "d_norm", "ffn_gate", "ffn_up", "ffn_down"]


def build(T, shapes, dbg=False):
    nc = bass.Bass("TRN2", target_bir_lowering=False)
    x_d = nc.dram_tensor("x", [T, D], F32, kind="ExternalInput").ap()
    p = {n: nc.dram_tensor(n, list(shapes[n]), F32, kind="ExternalInput").ap() for n in PARAM_NAMES}
    out_d = nc.dram_tensor("out", [T, D], F32, kind="ExternalOutput").ap()
    kind = "ExternalOutput" if dbg else "Internal"
    scr = lambda name, shape, dt: nc.dram_tensor(name, shape, dt, kind=kind).ap()
    u0_fm = scr("u0_fm", [1024 + 1696, T], F32)
    u0_tm = scr("u0_tm", [T, 1024], F32)
    y0_tm = scr("y0_tm", [T, 1024], BF16)
    xm0 = scr("xm0", [T, D], F32)
    xf0 = scr("xf0", [T, D], F32)
    u1_fm = scr("u1_fm", [256 + 1536, T], F32)
    u1_tm = scr("u1_tm", [T, 1544], F32)
    yc_fm = scr("yc_fm", [256, T], BF16)
    yd_tm = scr("yd_tm", [T, 768], BF16)
    xm1 = scr("xm1", [T, D], F32)
    with ExitStack() as st:
        P = Prog(nc, st, T)
        P.make_masks(st)
        P.phase_proj(x_d, p["norm_mix"][0], p["w_in_even"][0],
                     [(0, 1024, "fm", u0_fm[0:1024, :]), (1024, 1024, "tm", u0_tm), (2048, 1696, "fm", u0_fm[1024:, :])])
        P.phase_hgrn2(u0_fm[0:512, :], u0_fm[512:1024, :], u0_tm[:, 0:512], u0_tm[:, 512:1024],
                      p["lb_table"], p["a_norm"][0], y0_tm[:, 0:512])
        P.phase_rwkv7(u0_fm[1024:, :], p["b_mu"][0], p["b_w0"][0], p["b_w2"][0], p["b_a0"][0], p["b_a2"][0], p["b_g2"][0],
                      p["b_kk"][0], p["b_ka"][0], p["b_rk"][0], p["b_ln_w"][0], p["b_ln_b"][0], y0_tm[:, 512:1024])
        P.phase_outproj(x_d, [("tm", y0_tm, 0, 8)], p["w_out_even"][0], xm0)
        P.phase_ffn(xm0, p["norm_ffn"][0], p["ffn_gate"][0], p["ffn_up"][0], p["ffn_down"][0], xf0)
        P.phase_proj(xf0, p["norm_mix"][1], p["w_in_odd"][0],
                     [(0, 256 + 1536, "fm", u1_fm), (256 + 1536, 1544, "tm", u1_tm)])
        P.phase_s5(u1_fm[0:256, :], p["c_lam_re"][0], p["c_lam_im"][0], p["c_log_step"][0], p["c_b_re"][0], p["c_b_im"][0],
                   p["c_c_re"][0], p["c_c_im"][0], p["c_d"][0], p["c_glu_w"][0], p["c_glu_b"][0], yc_fm)
        P.phase_mlstm(u1_fm[256:1024, :], u1_fm[1024:1792, :], u1_tm[:, 0:768], u1_tm[:, 768:1536], u1_tm[:, 1536:1544],
                      p["d_conv_q"][0], p["d_conv_k"][0], p["d_i_bias"][0], p["d_f_bias"][0], p["d_norm"][0], yd_tm)
        P.phase_outproj(xf0, [("fm", yc_fm, 0, 2), ("tm", yd_tm, 2, 6)], p["w_out_odd"][0], xm1)
        P.phase_ffn(xm1, p["norm_ffn"][1], p["ffn_gate"][1], p["ffn_up"][1], p["ffn_down"][1], out_d, gfin_row=p["norm_final"])
    return nc


def kernel(**inputs):
    x = np.asarray(inputs["x"], dtype=np.float32)
    B, T, _ = x.shape
    params = {n: np.ascontiguousarray(np.asarray(inputs[n], dtype=np.float32)) for n in PARAM_NAMES}
    shapes = {n: params[n].shape for n in PARAM_NAMES}
    nc = build(T, shapes)
    in_maps = []
    for b in range(B):
        m = {"x": np.ascontiguousarray(x[b])}
        m.update(params)
        in_maps.append(m)
    res = run_bass_kernel_spmd(nc, in_maps, core_ids=list(range(B)))
    return np.stack([np.asarray(r["out"], dtype=np.float32) for r in res.results], axis=0)
```

```python
from contextlib import ExitStack
import numpy as np
import concourse.bass as bass
import concourse.mybir as mybir
from concourse.bass_utils import run_bass_kernel_spmd

F32 = mybir.dt.float32
BF16 = mybir.dt.bfloat16
ALU = mybir.AluOpType
AF = mybir.ActivationFunctionType
AX = mybir.AxisListType

D = 1024
EVEN_IN = 3744
ODD_IN = 3336
FFN_H = 2816
RMS_EPS = 1e-6
GN_EPS = 64e-5

ENGS = ["tensor", "vector", "scalar", "gpsimd", "sync"]
SAME_ENGINE_SYNC = True
DMA_RING = 12


class KB:
    def __init__(self, nc, st):
        self.nc = nc
        self.sem = {}
        self.semh = []
        for e in ENGS:
            h = st.enter_context(nc.semaphore("s_" + e))
            self.sem[e] = len(self.semh)
            self.semh.append(h)
        self.ring = {}
        self.ring_val = {}
        self.ring_pos = {}
        for q in ["sync", "gpsimd", "scalar"]:
            ids = []
            for j in range(DMA_RING):
                h = st.enter_context(nc.semaphore("d_%s%d" % (q, j)))
                ids.append(len(self.semh))
                self.semh.append(h)
            self.ring[q] = ids
            self.ring_val[q] = [0] * DMA_RING
            self.ring_pos[q] = 0
        self.cnt = {e: 0 for e in ENGS}
        self.ops = {e: [] for e in ENGS}
        self.waited = {e: {} for e in ENGS}
        self.res = {}
        self.nops = 0

    def _wait(self, eng, s, v):
        if s == self.sem[eng]:
            if eng == "tensor" or not SAME_ENGINE_SYNC:
                return
        if self.waited[eng].get(s, 0) >= v:
            return
        self.waited[eng][s] = v
        h = self.semh[s]
        self.ops[eng].append(lambda e, h=h, v=v: e.wait_ge(h, v))

    def _deps(self, eng, reads, writes):
        for k in reads:
            r = self.res.get(k)
            if r:
                for s, v in r[0].items():
                    self._wait(eng, s, v)
        for k in writes:
            r = self.res.get(k)
            if r:
                for s, v in r[0].items():
                    self._wait(eng, s, v)
                for s, v in r[1].items():
                    self._wait(eng, s, v)

    def _mark(self, s, v, reads, writes):
        for k in reads:
            r = self.res.setdefault(k, ({}, {}))
            if r[1].get(s, 0) < v:
                r[1][s] = v
        for k in writes:
            self.res[k] = ({s: v}, {})

    @staticmethod
    def _excl(reads, writes):
        ps = [k for k in reads if isinstance(k, str) and k.startswith("ps")]
        if ps:
            reads = [k for k in reads if k not in ps]
            writes = list(writes) + ps
        return reads, writes

    def op(self, eng, fn, reads=(), writes=()):
        reads, writes = self._excl(reads, writes)
        self._deps(eng, reads, writes)
        self.cnt[eng] += 1
        s = self.sem[eng]
        h = self.semh[s]
        self.ops[eng].append(lambda e, fn=fn, h=h: fn(e).then_inc(h, 1))
        self._mark(s, self.cnt[eng], reads, writes)
        self.nops += 1

    def dma(self, q, out, in_, reads=(), writes=(), **kw):
        self._deps(q, reads, writes)
        j = self.ring_pos[q] % DMA_RING
        self.ring_pos[q] += 1
        s = self.ring[q][j]
        prev = self.ring_val[q][j]
        if prev:
            self._wait(q, s, prev)
        v = prev + 16
        self.ring_val[q][j] = v
        h = self.semh[s]
        self.ops[q].append(
            lambda e, h=h, out=out, in_=in_, kw=kw: e.dma_start(out=out, in_=in_, **kw).then_inc(h, 16))
        self._mark(s, v, reads, writes)
        self.nops += 1

    def flush(self):
        for e in ENGS:
            if e != "sync" and self.cnt[e]:
                self._wait("sync", self.sem[e], self.cnt[e])
        for q in self.ring:
            for j in range(DMA_RING):
                if self.ring_val[q][j]:
                    self._wait("sync", self.ring[q][j], self.ring_val[q][j])
        with self.nc.Block() as block:
            for e in ENGS:
                ops = self.ops[e]
                if not ops:
                    continue

                def body(eng, ops=ops):
                    for f in ops:
                        f(eng)
                getattr(block, e)(body)
        self.ops = {e: [] for e in ENGS}
        self.res = {}


_UID = [0]


def sb(st, nc, name, shape, dt):
    _UID[0] += 1
    return st.enter_context(nc.sbuf_tensor("%s_%d" % (name, _UID[0]), list(shape), dt))


class Prog:
    def __init__(self, nc, st, T):
        self.nc = nc
        self.T = T
        self.kb = KB(nc, st)
        self.uid = 0
        self.psf = [st.enter_context(nc.psum_tensor("psf%d" % i, [128, 512], F32)) for i in range(6)]
        self.psb = [st.enter_context(nc.psum_tensor("psb%d" % i, [128, 1024], BF16)) for i in range(2)]
        self.psf_i = 0
        self.psb_i = 0
        self.evac_i = 0
        self.ident = sb(st, nc, "ident", [128, 128], BF16)
        self.identf = sb(st, nc, "identf", [128, 128], F32)
        self.ones_f = sb(st, nc, "ones_f", [128, 128], F32)
        self.ones_b = sb(st, nc, "ones_b", [128, 128], BF16)
        kb = self.kb
        kb.op("gpsimd", lambda e: e.memset(self.ones_f[:], 1.0), writes=["ones_f"])
        kb.op("gpsimd", lambda e: e.memset(self.ones_b[:], 1.0), writes=["ones_b"])
        kb.op("gpsimd", lambda e: e.affine_select(
            out=self.identf[:], in_=self.ones_f[:], pattern=[[-1, 128]], compare_op=ALU.is_equal,
            fill=0.0, base=0, channel_multiplier=1), reads=["ones_f"], writes=["identf"])
        kb.op("gpsimd", lambda e: e.tensor_copy(out=self.ident[:], in_=self.identf[:]),
              reads=["identf"], writes=["ident"])

    def key(self, name):
        self.uid += 1
        return "%s#%d" % (name, self.uid)

    def next_psf(self):
        i = self.psf_i % len(self.psf)
        self.psf_i += 1
        return self.psf[i], "psf%d" % i

    def bank(self, i):
        return self.psf[i], "psf%d" % i

    def next_psb(self):
        i = self.psb_i % len(self.psb)
        self.psb_i += 1
        return self.psb[i], "psb%d" % i

    def evac_eng(self):
        self.evac_i += 1
        return "scalar" if self.evac_i % 2 else "vector"

    def copy(self, eng, out, in_, reads, writes):
        if eng == "scalar":
            self.kb.op("scalar", lambda e: e.copy(out=out, in_=in_), reads=reads, writes=writes)
        else:
            self.kb.op(eng, lambda e: e.tensor_copy(out=out, in_=in_), reads=reads, writes=writes)

    def norm_tile_T(self, xt, xk, g_bc, tmp, dstT, dst_keys, eps=RMS_EPS):
        kb = self.kb
        junk, ms, rstd, hb = tmp["junk"], tmp["ms"], tmp["rstd"], tmp["hb"]
        kb.op("vector", lambda e: e.scalar_tensor_tensor(
            out=junk[:], in0=xt, scalar=1.0, in1=xt, op0=ALU.mult, op1=ALU.mult, accum_out=ms[:]),
            reads=[xk], writes=["junk", "ms"])
        kb.op("scalar", lambda e: e.activation(out=rstd[:], in_=ms[:], func=AF.Sqrt, bias=tmp["epsc"][:], scale=1.0 / D),
              reads=["ms"], writes=["rstd"])
        kb.op("vector", lambda e: e.reciprocal(out=rstd[:], in_=rstd[:]), reads=["rstd"], writes=["rstd"])
        kb.op("vector", lambda e: e.scalar_tensor_tensor(
            out=hb[:], in0=xt, scalar=rstd[:], in1=g_bc[:], op0=ALU.mult, op1=ALU.mult),
            reads=[xk, "rstd", "g_bc"], writes=["hb"])
        self.transpose_tile(hb, "hb", 8, dstT, dst_keys)

    def transpose_tile(self, src, sk, nchunks, dstT, dst_keys):
        kb = self.kb
        ps, pk = self.next_psb()
        for kc in range(nchunks):
            kb.op("tensor", lambda e, kc=kc: e.transpose(
                out=ps[:, kc * 128:(kc + 1) * 128], in_=src[:, kc * 128:(kc + 1) * 128], identity=self.ident[:]),
                reads=[sk, "ident"], writes=[pk])
        eng = self.evac_eng()
        self.copy(eng, dstT, ps[:, 0:nchunks * 128].rearrange("p (a b) -> p a b", b=128), [pk], dst_keys)

    def norm_tmp(self, st, pfx=""):
        nc = self.nc
        tmp = {
            "junk": sb(st, nc, pfx + "junk", [128, D], BF16),
            "ms": sb(st, nc, pfx + "ms", [128, 1], F32),
            "rstd": sb(st, nc, pfx + "rstd", [128, 1], F32),
            "hb": sb(st, nc, pfx + "hb", [128, D], BF16),
            "epsc": sb(st, nc, pfx + "epsc", [128, 1], F32),
        }
        self.kb.op("gpsimd", lambda e: e.memset(tmp["epsc"][:], RMS_EPS), writes=["epsc"])
        return tmp

    def load_bcast(self, dst, row_ap, n, key):
        self.kb.dma("sync", dst, row_ap.partition_broadcast(128), writes=[key])

    def phase_proj(self, x_d, g_row, W_d, outs):
        nc, kb, T = self.nc, self.kb, self.T
        NT = T // 128
        TB = min(512, T)
        with ExitStack() as st:
            hT = sb(st, nc, "hT", [128, 8, T], BF16)
            xts = [sb(st, nc, "xt%d" % i, [128, D], F32) for i in range(2)]
            g_bc = sb(st, nc, "g_bc", [128, D], F32)
            tmp = self.norm_tmp(st)
            self.load_bcast(g_bc[:], g_row, D, "g_bc")
            kb.dma("sync", xts[0][:], x_d[0:128, :], writes=["xt0"])
            for i in range(NT):
                if i + 1 < NT:
                    kb.dma("sync", xts[(i + 1) % 2][:], x_d[(i + 1) * 128:(i + 2) * 128, :], writes=["xt%d" % ((i + 1) % 2)])
                self.norm_tile_T(xts[i % 2][:], "xt%d" % (i % 2), g_bc, tmp,
                                 hT[:, :, i * 128:(i + 1) * 128], [("hT", i)])
            hkeys = [("hT", i) for i in range(NT)]
            Wv = W_d.rearrange("(kc p) n -> p kc n", p=128)
            wts = [sb(st, nc, "wt%d" % i, [128, 8, 512], BF16) for i in range(2)]
            stg = [sb(st, nc, "stg%d" % i, [128, 512], F32) for i in range(4)]
            wi = 0
            si = 0
            for (c0, ncols, kind, dst) in outs:
                for g0 in range(0, ncols, 512):
                    gw = min(512, ncols - g0)
                    wt = wts[wi % 2]
                    wk = "wt%d" % (wi % 2)
                    wi += 1
                    kb.dma("gpsimd", wt[:, :, 0:gw], Wv[:, :, c0 + g0:c0 + g0 + gw], writes=[wk])
                    if kind == "fm":
                        for b0 in range(0, gw, 128):
                            M = min(128, gw - b0)
                            for tb in range(T // TB):
                                ps, pk = self.next_psf()
                                for kc in range(8):
                                    kb.op("tensor", lambda e, kc=kc, ps=ps, wt=wt, b0=b0, M=M, tb=tb: e.matmul(
                                        out=ps[0:M, 0:TB], lhsT=wt[:, kc, b0:b0 + M], rhs=hT[:, kc, tb * TB:(tb + 1) * TB],
                                        start=(kc == 0), stop=(kc == 7)),
                                        reads=[wk] + hkeys[tb * TB // 128:(tb + 1) * TB // 128], writes=[pk])
                                sg = stg[si % 4]
                                sk = "stg%d" % (si % 4)
                                si += 1
                                self.copy(self.evac_eng(), sg[0:M, 0:TB], ps[0:M, 0:TB], [pk], [sk])
                                kb.dma("sync", dst[g0 + b0:g0 + b0 + M, tb * TB:(tb + 1) * TB], sg[0:M, 0:TB], reads=[sk])
                    else:
                        for i in range(NT):
                            ps, pk = self.next_psf()
                            for kc in range(8):
                                kb.op("tensor", lambda e, kc=kc, ps=ps, wt=wt, i=i, gw=gw: e.matmul(
                                    out=ps[:, 0:gw], lhsT=hT[:, kc, i * 128:(i + 1) * 128], rhs=wt[:, kc, 0:gw],
                                    start=(kc == 0), stop=(kc == 7)),
                                    reads=[wk, hkeys[i]], writes=[pk])
                            sg = stg[si % 4]
                            sk = "stg%d" % (si % 4)
                            si += 1
                            self.copy(self.evac_eng(), sg[:, 0:gw], ps[:, 0:gw], [pk], [sk])
                            kb.dma("sync", dst[i * 128:(i + 1) * 128, g0:g0 + gw], sg[:, 0:gw], reads=[sk])
            kb.flush()

    def phase_outproj(self, x_d, srcs, W_d, xo_d):
        nc, kb, T = self.nc, self.kb, self.T
        NT = T // 128
        with ExitStack() as st:
            Wt = sb(st, nc, "Wo", [128, 8, D], BF16)
            Wv = W_d.rearrange("(kc p) n -> p kc n", p=128)
            kb.dma("gpsimd", Wt[:, :, 0:512], Wv[:, :, 0:512], writes=["Wo0"])
            kb.dma("gpsimd", Wt[:, :, 512:1024], Wv[:, :, 512:1024], writes=["Wo1"])
            xts = [sb(st, nc, "xt%d" % i, [128, D], F32) for i in range(2)]
            yts = [sb(st, nc, "yt%d" % i, [128, D], BF16) for i in range(2)]
            yTs = [sb(st, nc, "yT%d" % i, [128, 8, 128], BF16) for i in range(2)]
            stg = [sb(st, nc, "stg%d" % i, [128, D], F32) for i in range(2)]

            def load(i):
                b = i % 2
                kb.dma("sync", xts[b][:], x_d[i * 128:(i + 1) * 128, :], writes=["xt%d" % b])
                for (kind, ap, kc0, nkc) in srcs:
                    if kind == "tm":
                        kb.dma("sync", yts[b][:, kc0 * 128:(kc0 + nkc) * 128], ap[i * 128:(i + 1) * 128, :],
                               writes=[("yt", b, kc0)])
                    else:
                        kb.dma("sync", yTs[b][:, kc0:kc0 + nkc, :],
                               ap.rearrange("(c p) t -> p c t", p=128)[:, :, i * 128:(i + 1) * 128],
                               writes=[("yT", b, kc0)])
            load(0)
            for i in range(NT):
                b = i % 2
                if i + 1 < NT:
                    load(i + 1)
                ykeys = []
                for (kind, ap, kc0, nkc) in srcs:
                    if kind == "tm":
                        self.transpose_tile(yts[b][:, kc0 * 128:(kc0 + nkc) * 128], ("yt", b, kc0), nkc,
                                            yTs[b][:, kc0:kc0 + nkc, :], [("yT", b, kc0)])
                    ykeys.append(("yT", b, kc0))
                for c in range(2):
                    ps, pk = self.next_psf()
                    for kc in range(8):
                        kb.op("tensor", lambda e, kc=kc, ps=ps, b=b, c=c: e.matmul(
                            out=ps[:, :], lhsT=yTs[b][:, kc, :], rhs=Wt[:, kc, c * 512:(c + 1) * 512],
                            start=(kc == 0), stop=(kc == 7)), reads=ykeys + ["Wo%d" % c], writes=[pk])
                    kb.op("vector", lambda e, ps=ps, b=b, c=c: e.tensor_tensor(
                        out=stg[b][:, c * 512:(c + 1) * 512], in0=ps[:, :], in1=xts[b][:, c * 512:(c + 1) * 512], op=ALU.add),
                        reads=[pk, "xt%d" % b], writes=[("stg", b, c)])
                kb.dma("sync", xo_d[i * 128:(i + 1) * 128, :], stg[b][:], reads=[("stg", b, 0), ("stg", b, 1)])
            kb.flush()

    def phase_ffn(self, x_d, g_row, Wg_d, Wu_d, Wd_d, xo_d, gfin_row=None):
        nc, kb, T = self.nc, self.kb, self.T
        TB = min(512, T)
        NTB = TB // 128
        NJ = FFN_H // 128
        with ExitStack() as st:
            Wd = sb(st, nc, "Wd", [128, NJ, D], BF16)
            Wdv = Wd_d.rearrange("(j p) n -> p j n", p=128)
            def load_wd():
                for j0 in range(0, NJ, 4):
                    j1 = min(NJ, j0 + 4)
                    kb.dma("gpsimd", Wd[:, j0:j1, :], Wdv[:, j0:j1, :], writes=[("Wd", j0 // 4)])
            wdkeys = [("Wd", j) for j in range((NJ + 3) // 4)]
            Wgv = Wg_d.rearrange("(kc p) n -> p kc n", p=128)
            Wuv = Wu_d.rearrange("(kc p) n -> p kc n", p=128)
            wgs = [sb(st, nc, "wg%d" % i, [128, 8, 512], BF16) for i in range(3)]
            wus = [sb(st, nc, "wu%d" % i, [128, 8, 512], BF16) for i in range(3)]
            xb = sb(st, nc, "xb", [128, NTB, D], F32)
            hT = sb(st, nc, "hT", [128, 8, TB], BF16)
            aT = sb(st, nc, "aT", [128, NJ, TB], BF16)
            sg = [sb(st, nc, "sg%d" % i, [128, TB], F32) for i in range(2)]
            stg = [sb(st, nc, "stg%d" % i, [128, D], F32) for i in range(2)]
            g_bc = sb(st, nc, "g_bc", [128, D], F32)
            tmp = self.norm_tmp(st)
            self.load_bcast(g_bc[:], g_row, D, "g_bc")
            if gfin_row is not None:
                gf_bc = sb(st, nc, "gf_bc", [128, D], F32)
                self.load_bcast(gf_bc[:], gfin_row, D, "gf_bc")
            wi = 0
            si = 0
            for tb in range(T // TB):
                for i in range(NTB):
                    r0 = tb * TB + i * 128
                    kb.dma("sync", xb[:, i, :], x_d[r0:r0 + 128, :], writes=[("xb", i)])
                for i in range(NTB):
                    self.norm_tile_T(xb[:, i, :], ("xb", i), g_bc, tmp, hT[:, :, i * 128:(i + 1) * 128], [("hT", i)])
                hkeys = [("hT", i) for i in range(NTB)]
                for g0 in range(0, FFN_H, 512):
                    gw = min(512, FFN_H - g0)
                    b = wi % 3
                    wi += 1
                    kb.dma("gpsimd", wgs[b][:, :, 0:gw], Wgv[:, :, g0:g0 + gw], writes=["wg%d" % b])
                    kb.dma("gpsimd", wus[b][:, :, 0:gw], Wuv[:, :, g0:g0 + gw], writes=["wu%d" % b])
                    if tb == 0 and g0 == 512:
                        load_wd()
                    for b0 in range(0, gw, 128):
                        j = (g0 + b0) // 128
                        psg, pkg = self.next_psf()
                        for kc in range(8):
                            kb.op("tensor", lambda e, kc=kc, ps=psg, b=b, b0=b0: e.matmul(
                                out=ps[:, 0:TB], lhsT=wgs[b][:, kc, b0:b0 + 128], rhs=hT[:, kc, :],
                                start=(kc == 0), stop=(kc == 7)), reads=["wg%d" % b] + hkeys, writes=[pkg])
                        psu, pku = self.next_psf()
                        for kc in range(8):
                            kb.op("tensor", lambda e, kc=kc, ps=psu, b=b, b0=b0: e.matmul(
                                out=ps[:, 0:TB], lhsT=wus[b][:, kc, b0:b0 + 128], rhs=hT[:, kc, :],
                                start=(kc == 0), stop=(kc == 7)), reads=["wu%d" % b] + hkeys, writes=[pku])
                        s = sg[si % 2]
                        sk = "sg%d" % (si % 2)
                        si += 1
                        kb.op("scalar", lambda e, s=s, ps=psg: e.activation(out=s[:, 0:TB], in_=ps[:, 0:TB], func=AF.Silu),
                              reads=[pkg], writes=[sk])
                        kb.op("vector", lambda e, s=s, ps=psu, j=j: e.tensor_tensor(
                            out=aT[:, j, :], in0=ps[:, 0:TB], in1=s[:, 0:TB], op=ALU.mult),
                            reads=[pku, sk], writes=[("aT", j)])
                akeys = [("aT", j) for j in range(NJ)]
                for i in range(NTB):
                    r0 = tb * TB + i * 128
                    sb_ = (tb * NTB + i) % 2
                    for c in range(2):
                        ps, pk = self.next_psf()
                        for j in range(NJ):
                            kb.op("tensor", lambda e, j=j, ps=ps, i=i, c=c: e.matmul(
                                out=ps[:, :], lhsT=aT[:, j, i * 128:(i + 1) * 128], rhs=Wd[:, j, c * 512:(c + 1) * 512],
                                start=(j == 0), stop=(j == NJ - 1)), reads=akeys + wdkeys, writes=[pk])
                        kb.op("vector", lambda e, ps=ps, i=i, c=c, sb_=sb_: e.tensor_tensor(
                            out=stg[sb_][:, c * 512:(c + 1) * 512], in0=ps[:, :], in1=xb[:, i, c * 512:(c + 1) * 512], op=ALU.add),
                            reads=[pk, ("xb", i)], writes=[("stg", sb_, c)])
                    skeys = [("stg", sb_, 0), ("stg", sb_, 1)]
                    if gfin_row is not None:
                        junk, ms, rstd = tmp["junk"], tmp["ms"], tmp["rstd"]
                        so = stg[sb_]
                        kb.op("vector", lambda e, so=so: e.scalar_tensor_tensor(
                            out=junk[:], in0=so[:], scalar=1.0, in1=so[:], op0=ALU.mult, op1=ALU.mult, accum_out=ms[:]),
                            reads=skeys, writes=["junk", "ms"])
                        kb.op("scalar", lambda e: e.activation(out=rstd[:], in_=ms[:], func=AF.Sqrt, bias=tmp["epsc"][:], scale=1.0 / D),
                              reads=["ms"], writes=["rstd"])
                        kb.op("vector", lambda e: e.reciprocal(out=rstd[:], in_=rstd[:]), reads=["rstd"], writes=["rstd"])
                        kb.op("vector", lambda e, so=so: e.scalar_tensor_tensor(
                            out=so[:], in0=so[:], scalar=rstd[:], in1=gf_bc[:], op0=ALU.mult, op1=ALU.mult),
                            reads=skeys + ["rstd", "gf_bc"], writes=skeys)
                    kb.dma("sync", xo_d[r0:r0 + 128, :], stg[sb_][:], reads=skeys)
            kb.flush()

    def bc_last(self, ap, n):
        return bass.AP(ap.tensor, ap.offset, [list(p) for p in ap.ap] + [[0, n]])

    def make_masks(self, st):
        nc, kb = self.nc, self.kb
        self.m_ge = sb(st, nc, "m_ge", [128, 128], F32)
        kb.op("gpsimd", lambda e: e.affine_select(
            out=self.m_ge[:], in_=self.ones_f[:], pattern=[[1, 128]], compare_op=ALU.is_ge,
            fill=0.0, base=0, channel_multiplier=-1), reads=["ones_f"], writes=["m_ge"])

    def phase_hgrn2(self, q_fm, f_fm, i_tm, g_tm, lbt_d, gain_row, y_tm):
        nc, kb, T = self.nc, self.kb, self.T
        TB = min(512, T)
        NC = TB // 128
        H = 4
        with ExitStack() as st:
            S = lambda name, shape, dt: sb(st, nc, name, shape, dt)
            lbt = S("lbt", [128, 3, 4], F32)
            lbe = S("lbe", [128, 3, 4], F32)
            lbs = S("lbs", [128, 4], F32)
            lb = S("lb", [128, 4], F32)
            oml = S("oml", [128, 4], F32)
            kb.dma("sync", lbt[:], lbt_d.rearrange("r (c p) -> p r c", p=128), writes=["lbt"], allow_slow_non_contiguous=True)
            kb.op("scalar", lambda e: e.activation(out=lbe[:], in_=lbt[:], func=AF.Exp), reads=["lbt"], writes=["lbe"])
            kb.op("vector", lambda e: e.tensor_tensor(out=lbs[:], in0=lbe[:, 0, :], in1=lbe[:, 1, :], op=ALU.add), reads=["lbe"], writes=["lbs"])
            kb.op("vector", lambda e: e.tensor_tensor(out=lbs[:], in0=lbs[:], in1=lbe[:, 2, :], op=ALU.add), reads=["lbe", "lbs"], writes=["lbs"])
            kb.op("vector", lambda e: e.reciprocal(out=lbs[:], in_=lbs[:]), reads=["lbs"], writes=["lbs"])
            kb.op("vector", lambda e: e.tensor_tensor(out=lb[:], in0=lbe[:, 0, :], in1=lbs[:], op=ALU.mult), reads=["lbe", "lbs"], writes=["lb"])
            kb.op("vector", lambda e: e.tensor_scalar(out=oml[:], in0=lb[:], scalar1=-1.0, scalar2=1.0, op0=ALU.mult, op1=ALU.add),
                  reads=["lb"], writes=["oml"])
            gn_bc = S("gn_bc", [128, 512], F32)
            self.load_bcast(gn_bc[:], gain_row, 512, "gn_bc")
            rmask = S("rmask", [128, TB], F32)
            kb.op("gpsimd", lambda e: e.memset(rmask[:], 1.0), writes=["rmask"])
            kb.op("gpsimd", lambda e: e.memset(rmask[:].rearrange("p (c l) -> p c l", l=128)[:, :, 0:1], 0.0), writes=["rmask"])
            qin = S("qin", [128, H, TB], F32)
            fin = S("fin", [128, H, TB], F32)
            t_f_l = [S("t_f%d" % i, [128, TB], F32) for i in range(H)]
            t_lf_l = [S("t_lf%d" % i, [128, TB], F32) for i in range(H)]
            t_b_l = [S("t_b%d" % i, [128, TB], F32) for i in range(H)]
            t_d_l = [S("t_d%d" % i, [128, TB], F32) for i in range(H)]
            t_e_l = [S("t_e%d" % i, [128, TB], F32) for i in range(H)]
            t_e2_l = [S("t_e2%d" % i, [128, TB], F32) for i in range(H)]
            t_q_l = [S("t_q%d" % i, [128, TB], F32) for i in range(H)]
            QT = S("QT", [128, H, TB], BF16)
            KT = S("KT", [128, H, TB], BF16)
            bm = S("bm", [128, H, NC], F32)
            dl = S("dl", [128, H, NC], F32)
            emid = S("emid", [128, H, NC], F32)
            e1 = S("e1", [128, H, NC], F32)
            e2 = S("e2", [128, H, NC], F32)
            vin = [S("vin%d" % i, [128, 512], F32) for i in range(2)]
            gin = [S("gin%d" % i, [128, 512], F32) for i in range(2)]
            vb_l = [S("vb%d" % i, [128, 512], BF16) for i in range(2)]
            gg_l = [S("gg%d" % i, [128, 512], F32) for i in range(2)]
            St = S("St", [128, H, 128], F32)
            Sb = S("Sb", [128, H, 128], BF16)
            PT4 = [S("PT4_%d" % i, [128, H, 128], BF16) for i in range(2)]
            Ktm4 = [S("Ktm4_%d" % i, [128, H, 128], BF16) for i in range(2)]
            dS4 = S("dS4", [128, H, 128], F32)
            mge4 = bass.AP(self.m_ge[:].tensor, self.m_ge[:].offset, [list(self.m_ge[:].ap[0]), [0, H], [1, 128]])
            dS_l = [S("dS%d" % i, [128, 128], F32) for i in range(2)]
            osq = S("osq", [128, 512], F32)
            ssq = S("ssq", [128, 4], F32)
            rs = S("rs", [128, 4], F32)
            yo = [S("yo%d" % i, [128, 512], BF16) for i in range(2)]
            epsc = S("epsc", [128, 1], F32)
            kb.op("gpsimd", lambda e: e.memset(epsc[:], RMS_EPS), writes=["epsc"])
            kb.op("gpsimd", lambda e: e.memset(St[:], 0.0), writes=["St"])
            kb.op("gpsimd", lambda e: e.memset(Sb[:], 0.0), writes=["Sb"])
            pi = 0
            for tb in range(T // TB):
                t0 = tb * TB
                kb.dma("sync", qin[:], q_fm.rearrange("(h p) t -> p h t", p=128)[:, :, t0:t0 + TB], writes=["qin"])
                kb.dma("sync", fin[:], f_fm.rearrange("(h p) t -> p h t", p=128)[:, :, t0:t0 + TB], writes=["fin"])
                def _head(h):
                    t_f, t_lf, t_b, t_d, t_e, t_q, t_e2 = t_f_l[h], t_lf_l[h], t_b_l[h], t_d_l[h], t_e_l[h], t_q_l[h], t_e2_l[h]
                    kb.op("scalar", lambda e, h=h: e.activation(out=t_f[:], in_=fin[:, h, :], func=AF.Sigmoid), reads=["fin"], writes=["t_f%d" % h])
                    kb.op("vector", lambda e, h=h: e.tensor_scalar(out=t_f[:], in0=t_f[:], scalar1=oml[:, h:h + 1], scalar2=lb[:, h:h + 1],
                                                                 op0=ALU.mult, op1=ALU.add), reads=["t_f%d" % h, "oml", "lb"], writes=["t_f%d" % h])
                    kb.op("scalar", lambda e: e.activation(out=t_lf[:], in_=t_f[:], func=AF.Ln), reads=["t_f%d" % h], writes=["t_lf%d" % h])
                    kb.op("vector", lambda e: e.tensor_tensor_scan(out=t_b[:], data0=rmask[:], data1=t_lf[:], initial=0.0,
                                                                   op0=ALU.mult, op1=ALU.add), reads=["rmask", "t_lf%d" % h], writes=["t_b%d" % h])
                    b3 = t_b[:].rearrange("p (c l) -> p c l", l=128)
                    kb.op("vector", lambda e, h=h, b3=b3: e.tensor_copy(out=bm[:, h, :], in_=b3[:, :, 63]), reads=["t_b%d" % h], writes=["bm"])
                    kb.op("vector", lambda e, h=h, b3=b3: e.tensor_tensor(out=dl[:, h, :], in0=b3[:, :, 127], in1=b3[:, :, 63], op=ALU.subtract),
                          reads=["t_b%d" % h], writes=["dl"])
                    kb.op("vector", lambda e, h=h, b3=b3: e.tensor_tensor(
                        out=t_d[:].rearrange("p (c l) -> p c l", l=128), in0=b3, in1=self.bc_last(bm[:, h, :], 128), op=ALU.subtract),
                        reads=["t_b%d" % h, "bm"], writes=["t_d%d" % h])
                    kb.op("scalar", lambda e: e.activation(out=t_e[:], in_=t_d[:], func=AF.Exp), reads=["t_d%d" % h], writes=["t_e%d" % h])
                    kb.op("scalar", lambda e, h=h: e.activation(out=t_q[:], in_=qin[:, h, :], func=AF.Silu), reads=["qin"], writes=["t_q%d" % h])
                    kb.op("vector", lambda e, h=h: e.tensor_tensor(out=QT[:, h, :], in0=t_q[:], in1=t_e[:], op=ALU.mult),
                          reads=["t_q%d" % h, "t_e%d" % h], writes=[("QT", h)])
                    kb.op("scalar", lambda e: e.activation(out=t_e2[:], in_=t_d[:], func=AF.Exp, scale=-1.0), reads=["t_d%d" % h], writes=["t_e2%d" % h])
                    kb.op("vector", lambda e: e.tensor_scalar(out=t_f[:], in0=t_f[:], scalar1=-1.0, scalar2=1.0, op0=ALU.mult, op1=ALU.add),
                          reads=["t_f%d" % h], writes=["t_f%d" % h])
                    kb.op("vector", lambda e, h=h: e.tensor_tensor(out=KT[:, h, :], in0=t_f[:], in1=t_e2[:], op=ALU.mult),
                          reads=["t_f%d" % h, "t_e2%d" % h], writes=[("KT", h)])
                    kb.op("scalar", lambda e, h=h: e.activation(out=emid[:, h, :], in_=bm[:, h, :], func=AF.Exp), reads=["bm"], writes=["emid"])
                    kb.op("scalar", lambda e, h=h: e.activation(out=e2[:, h, :], in_=dl[:, h, :], func=AF.Exp), reads=["dl"], writes=["e2"])
                    kb.op("vector", lambda e, h=h: e.tensor_tensor(out=e1[:, h, :], in0=e2[:, h, :], in1=emid[:, h, :], op=ALU.mult),
                          reads=["e2", "emid"], writes=["e1"])
                for h in range(H):
                    _head(h)
                def _front(c):
                    r0 = t0 + c * 128
                    ib = (tb * NC + c) % 2
                    vb, gg = vb_l[ib], gg_l[ib]
                    kvb, kgg = 'vb%d' % ib, 'gg%d' % ib
                    cs = slice(c * 128, (c + 1) * 128)
                    kb.dma("sync", vin[ib][:], i_tm[r0:r0 + 128, :], writes=["vin%d" % ib])
                    kb.dma("sync", gin[ib][:], g_tm[r0:r0 + 128, :], writes=["gin%d" % ib])
                    kb.op("vector", lambda e, ib=ib: e.tensor_copy(out=vb[:], in_=vin[ib][:]), reads=["vin%d" % ib], writes=[kvb])
                    kb.op("scalar", lambda e, ib=ib: e.activation(out=gg[:], in_=gin[ib][:], func=AF.Silu), reads=["gin%d" % ib], writes=[kgg])
                    kb.op("gpsimd", lambda e: e.tensor_tensor(out=gg[:], in0=gg[:], in1=gn_bc[:], op=ALU.mult), reads=[kgg, "gn_bc"], writes=[kgg])
                    psa, pka = self.bank(2 + ib)
                    for h in range(H):
                        kb.op("tensor", lambda e, h=h, cs=cs, psa=psa: e.matmul(out=psa[:, h * 128:(h + 1) * 128], lhsT=KT[:, h, cs], rhs=QT[:, h, cs], start=True, stop=True),
                              reads=[("KT", h), ("QT", h)], writes=[pka])
                    kb.op("vector", lambda e, psa=psa, ib=ib: e.tensor_tensor(out=PT4[ib][:], in0=psa[:, :].rearrange("p (h t) -> p h t", t=128), in1=mge4, op=ALU.mult),
                          reads=[pka, "m_ge"], writes=[("PT4", ib)])
                    psk, pkk = self.next_psb()
                    for h in range(H):
                        kb.op("tensor", lambda e, h=h, cs=cs, psk=psk: e.transpose(out=psk[:, h * 128:(h + 1) * 128], in_=KT[:, h, cs], identity=self.ident[:]),
                              reads=[("KT", h), "ident"], writes=[pkk])
                    kb.op("scalar", lambda e, psk=psk, ib=ib: e.copy(out=Ktm4[ib][:], in_=psk[:, 0:512].rearrange("p (h k) -> p h k", k=128)), reads=[pkk], writes=[("Ktm4", ib)])

                def _back(c):
                    r0 = t0 + c * 128
                    ib = (tb * NC + c) % 2
                    vb, gg = vb_l[ib], gg_l[ib]
                    kvb, kgg = 'vb%d' % ib, 'gg%d' % ib
                    cs = slice(c * 128, (c + 1) * 128)
                    pso, pko = self.bank(ib)
                    kb.op("vector", lambda e, c=c: e.tensor_tensor(out=Sb[:], in0=St[:], in1=self.bc_last(emid[:, :, c], 128), op=ALU.mult),
                          reads=["St", "emid"], writes=["Sb"])
                    for h in range(H):
                        hs = slice(h * 128, (h + 1) * 128)
                        kb.op("tensor", lambda e, h=h, hs=hs, pso=pso, ib=ib: e.matmul(out=pso[:, hs], lhsT=PT4[ib][:, h, :], rhs=vb[:, hs], start=True, stop=False),
                              reads=[("PT4", ib), kvb], writes=[pko])
                        kb.op("tensor", lambda e, h=h, cs=cs, hs=hs, pso=pso: e.matmul(out=pso[:, hs], lhsT=QT[:, h, cs], rhs=Sb[:, h, :], start=False, stop=True),
                              reads=[("QT", h), "Sb"], writes=[pko])
                    pss, pks = self.bank(4 + ib)
                    for h in range(H):
                        hs = slice(h * 128, (h + 1) * 128)
                        kb.op("tensor", lambda e, h=h, hs=hs, pss=pss, ib=ib: e.matmul(out=pss[:, hs], lhsT=Ktm4[ib][:, h, :], rhs=vb[:, hs], start=True, stop=True),
                              reads=[("Ktm4", ib), kvb], writes=[pks])
                    kb.op("vector", lambda e, c=c, pss=pss: e.tensor_tensor(out=dS4[:], in0=pss[:, :].rearrange("p (h v) -> p h v", v=128), in1=self.bc_last(e2[:, :, c], 128), op=ALU.mult),
                          reads=[pks, "e2"], writes=["dS4"])
                    kb.op("vector", lambda e, c=c: e.tensor_tensor(out=St[:], in0=St[:], in1=self.bc_last(e1[:, :, c], 128), op=ALU.mult),
                          reads=["St", "e1", "Sb"], writes=["St"])
                    kb.op("gpsimd", lambda e: e.tensor_tensor(out=St[:], in0=St[:], in1=dS4[:], op=ALU.add), reads=["St", "dS4"], writes=["St"])
                    kb.op("scalar", lambda e, pso=pso: e.activation(out=osq[:], in_=pso[:, :], func=AF.Square), reads=[pko], writes=["osq"])
                    kb.op("vector", lambda e: e.tensor_reduce(out=ssq[:], in_=osq[:].rearrange("p (h v) -> p h v", v=128), axis=AX.X, op=ALU.add),
                          reads=["osq"], writes=["ssq"])
                    kb.op("scalar", lambda e: e.activation(out=rs[:], in_=ssq[:], func=AF.Sqrt, bias=epsc[:], scale=1.0 / 128), reads=["ssq", "epsc"], writes=["rs"])
                    kb.op("vector", lambda e: e.reciprocal(out=rs[:], in_=rs[:]), reads=["rs"], writes=["rs"])
                    for h in range(H):
                        hs = slice(h * 128, (h + 1) * 128)
                        kb.op("vector", lambda e, h=h, hs=hs, pso=pso, ib=ib: e.scalar_tensor_tensor(
                            out=yo[ib][:, hs], in0=pso[:, hs], scalar=rs[:, h:h + 1], in1=gg[:, hs], op0=ALU.mult, op1=ALU.mult),
                            reads=[pko, "rs", kgg], writes=["yo%d" % ib])
                    kb.dma("gpsimd", y_tm[r0:r0 + 128, :], yo[ib][:], reads=["yo%d" % ib])
                _front(0)
                for c in range(NC):
                    if c + 1 < NC:
                        _front(c + 1)
                    _back(c)
            kb.flush()

    def phase_mlstm(self, q_fm, k_fm, v_tm, o_tm, if_tm, cq_d, ck_d, ib_row, fb_row, gain_row, y_tm):
        nc, kb, T = self.nc, self.kb, self.T
        TB = min(512, T)
        NC = TB // 128
        H, DH, NJ = 4, 192, 6
        pieces = {0: [(0, 0, 128, 0), (1, 0, 64, 128)], 1: [(2, 0, 128, 64), (1, 64, 128, 0)],
                  2: [(3, 0, 128, 0), (4, 0, 64, 128)], 3: [(5, 0, 128, 64), (4, 64, 128, 0)]}
        with ExitStack() as st:
            S = lambda name, shape, dt: sb(st, nc, name, shape, dt)
            cw = {"q": S("cwq", [128, 4, NJ], F32), "k": S("cwk", [128, 4, NJ], F32)}
            for tap in range(4):
                kb.dma("sync", cw["q"][:, tap, :], cq_d[tap].rearrange("(c p) -> p c", p=128), writes=[("cwq", tap)], allow_slow_non_contiguous=True)
                kb.dma("sync", cw["k"][:, tap, :], ck_d[tap].rearrange("(c p) -> p c", p=128), writes=[("cwk", tap)], allow_slow_non_contiguous=True)
            gb_bc = S("gb_bc", [128, 8], F32)
            kb.dma("sync", gb_bc[:, 0:4], ib_row.partition_broadcast(128), writes=["gbi"])
            kb.dma("sync", gb_bc[:, 4:8], fb_row.partition_broadcast(128), writes=["gbf"])
            gn_bc = S("gn_bc", [128, 768], F32)
            self.load_bcast(gn_bc[:], gain_row, 768, "gn_bc")
            uin = {"q": S("uinq", [128, NJ, TB + 3], F32), "k": S("uink", [128, NJ, TB + 3], F32)}
            acc = [S("acc%d" % i, [128, TB], F32) for i in range(2)]
            XT = {"q": S("qT", [128, NJ, TB], BF16), "k": S("kT", [128, NJ, TB], BF16)}
            vin = [S("vin%d" % i, [128, 768], F32) for i in range(2)]
            oin = [S("oin%d" % i, [128, 768], F32) for i in range(2)]
            gin = [S("gin%d" % i, [128, 8], F32) for i in range(2)]
            vext_l = [S("vext%d" % i, [128, H, DH + 1], BF16) for i in range(2)]
            go_l = [S("go%d" % i, [128, 768], F32) for i in range(2)]
            gt_l = [S("gt%d" % i, [128, 8], F32) for i in range(2)]
            lf_l = [S("lf%d" % i, [128, 4], F32) for i in range(2)]
            bb_l = [S("bb%d" % i, [128, 4], F32) for i in range(2)]
            ee_l = [S("ee%d" % i, [128, 4], F32) for i in range(2)]
            emb_l = [S("emb%d" % i, [128, 4], F32) for i in range(2)]
            dec_l = [S("dec%d" % i, [128, 4], F32) for i in range(2)]
            Ktm_l = [S("Ktm%d" % i, [128, 768], BF16) for i in range(2)]
            PT4 = [S("PT4_%d" % i, [128, H, 128], BF16) for i in range(2)]
            decT_l = [S("decT%d" % i, [128, NJ], F32) for i in range(2)]
            dC2 = [S("dC2_%d" % i, [128, 2, DH + 1], F32) for i in range(3)]
            Cst = S("Cst", [128, NJ, DH + 1], F32)
            Cb = S("Cb", [128, NJ, DH + 1], BF16)
            dC_l = [S("dC%d" % i, [128, DH + 1], F32) for i in range(4)]
            junk_l = [S("junk%d" % i, [128, DH], F32) for i in range(2)]
            ssq_l = [S("ssq%d" % i, [128, 4], F32) for i in range(2)]
            dm_l = [S("dm%d" % i, [128, 4], F32) for i in range(2)]
            t1_l = [S("t1%d" % i, [128, 4], F32) for i in range(2)]
            sc_l = [S("sc%d" % i, [128, 4], F32) for i in range(2)]
            yo = [S("yo%d" % i, [128, 768], BF16) for i in range(2)]
            epsc = S("epsc", [128, 1], F32)
            kb.op("gpsimd", lambda e: e.memset(epsc[:], RMS_EPS), writes=["epsc"])
            kb.op("gpsimd", lambda e: e.memset(Cst[:], 0.0), writes=[("Cst", 0), ("Cst", 1), ("Cst", 2)])
            kb.op("gpsimd", lambda e: e.memset(Cb[:], 0.0), writes=[("Cb", 0), ("Cb", 1), ("Cb", 2)])
            kb.op("gpsimd", lambda e: e.memset(vext_l[0][:], 1.0), writes=["vext0"])
            kb.op("gpsimd", lambda e: e.memset(vext_l[1][:], 1.0), writes=["vext1"])
            kb.op("gpsimd", lambda e: e.memset(uin["q"][:, :, 0:3], 0.0), writes=["uinq"])
            kb.op("gpsimd", lambda e: e.memset(uin["k"][:, :, 0:3], 0.0), writes=["uink"])
            srcs = {"q": q_fm.rearrange("(c p) t -> p c t", p=128), "k": k_fm.rearrange("(c p) t -> p c t", p=128)}
            ai = 0
            pi = 0
            dci = 0
            for tb in range(T // TB):
                t0 = tb * TB
                for nm in ("q", "k"):
                    if tb == 0:
                        kb.dma("sync", uin[nm][:, :, 3:3 + TB], srcs[nm][:, :, 0:TB], writes=["uin" + nm])
                    else:
                        kb.dma("sync", uin[nm][:, :, :], srcs[nm][:, :, t0 - 3:t0 + TB], writes=["uin" + nm])
                    for j in range(NJ):
                        a = acc[ai % 2]
                        ak = "acc%d" % (ai % 2)
                        ai += 1
                        kb.op("vector", lambda e, nm=nm, j=j, a=a: e.tensor_scalar(
                            out=a[:], in0=uin[nm][:, j, 3:3 + TB], scalar1=cw[nm][:, 3, j:j + 1], scalar2=None, op0=ALU.mult),
                            reads=["uin" + nm] + [("cw" + nm, tp) for tp in range(4)], writes=[ak])
                        for tap in (2, 1, 0):
                            kb.op("vector", lambda e, nm=nm, j=j, a=a, tap=tap: e.scalar_tensor_tensor(
                                out=a[:], in0=uin[nm][:, j, tap:tap + TB], scalar=cw[nm][:, tap, j:j + 1], in1=a[:],
                                op0=ALU.mult, op1=ALU.add), reads=["uin" + nm, ak], writes=[ak])
                        kb.op("scalar", lambda e, nm=nm, j=j, a=a: e.activation(out=XT[nm][:, j, :], in_=a[:], func=AF.Silu),
                              reads=[ak], writes=[(nm + "T", j)])
                def _front(c):
                    nonlocal pi, dci
                    r0 = t0 + c * 128
                    cs = slice(c * 128, (c + 1) * 128)
                    ib = (tb * NC + c) % 2
                    vext, go, gt, lf, bb, ee, emb, dec, Ktm, ssq, dm, t1, sc = (vext_l[ib], go_l[ib], gt_l[ib], lf_l[ib], bb_l[ib], ee_l[ib], emb_l[ib], dec_l[ib], Ktm_l[ib], ssq_l[ib], dm_l[ib], t1_l[ib], sc_l[ib])
                    kb.dma("sync", vin[ib][:], v_tm[r0:r0 + 128, :], writes=["vin%d" % ib])
                    kb.dma("sync", oin[ib][:], o_tm[r0:r0 + 128, :], writes=["oin%d" % ib])
                    kb.dma("sync", gin[ib][:], if_tm[r0:r0 + 128, :], writes=["gin%d" % ib])
                    kb.op("vector", lambda e, ib=ib: e.tensor_copy(out=vext[:, :, 0:DH], in_=vin[ib][:].rearrange("p (h v) -> p h v", v=DH)),
                          reads=["vin%d" % ib], writes=["vext%d" % ib])
                    kb.op("scalar", lambda e, ib=ib: e.activation(out=go[:], in_=oin[ib][:], func=AF.Sigmoid), reads=["oin%d" % ib], writes=["go%d" % ib])
                    kb.op("gpsimd", lambda e: e.tensor_tensor(out=go[:], in0=go[:], in1=gn_bc[:], op=ALU.mult), reads=["go%d" % ib, "gn_bc"], writes=["go%d" % ib])
                    kb.op("vector", lambda e, ib=ib: e.tensor_tensor(out=gt[:], in0=gin[ib][:], in1=gb_bc[:], op=ALU.add),
                          reads=["gin%d" % ib, "gbi", "gbf"], writes=["gt%d" % ib])
                    kb.op("scalar", lambda e: e.activation(out=lf[:], in_=gt[:, 4:8], func=AF.Sigmoid), reads=["gt%d" % ib], writes=["lf%d" % ib])
                    kb.op("scalar", lambda e: e.activation(out=lf[:], in_=lf[:], func=AF.Ln), reads=["lf%d" % ib], writes=["lf%d" % ib])
                    psg, pkg = self.bank(0)
                    kb.op("tensor", lambda e, psg=psg: e.matmul(out=psg[:, 0:4], lhsT=self.m_ge[:], rhs=lf[:], start=True, stop=True),
                          reads=["m_ge", "lf%d" % ib], writes=[pkg])
                    kb.op("tensor", lambda e, psg=psg: e.matmul(out=psg[:, 4:8], lhsT=self.ones_f[:], rhs=lf[:], start=True, stop=True),
                          reads=["ones_f", "lf%d" % ib], writes=[pkg])
                    kb.op("vector", lambda e, psg=psg: e.tensor_copy(out=bb[:], in_=psg[:, 0:4]), reads=[pkg], writes=["bb%d" % ib])
                    kb.op("scalar", lambda e, psg=psg: e.activation(out=dec[:], in_=psg[:, 4:8], func=AF.Exp), reads=[pkg], writes=["dec%d" % ib])
                    decT = decT_l[ib]
                    for (jj, q0, q1, hh) in ((0, 0, 128, 0), (1, 0, 64, 0), (1, 64, 128, 1), (2, 0, 128, 1), (3, 0, 128, 2), (4, 0, 64, 2), (4, 64, 128, 3), (5, 0, 128, 3)):
                        kb.op("gpsimd", lambda e, jj=jj, q0=q0, q1=q1, hh=hh, decT=decT: e.tensor_copy(out=decT[q0:q1, jj:jj + 1], in_=dec[q0:q1, hh:hh + 1]),
                              reads=["dec%d" % ib], writes=["decT%d" % ib])
                    kb.op("scalar", lambda e: e.activation(out=emb[:], in_=bb[:], func=AF.Exp, scale=-1.0), reads=["bb%d" % ib], writes=["emb%d" % ib])
                    kb.op("vector", lambda e: e.scalar_tensor_tensor(out=ee[:], in0=gt[:, 0:4], scalar=-0.5 * float(np.log(DH)), in1=bb[:],
                                                                     op0=ALU.add, op1=ALU.subtract), reads=["gt%d" % ib, "bb%d" % ib], writes=["ee%d" % ib])
                    kb.op("scalar", lambda e: e.activation(out=ee[:], in_=ee[:], func=AF.Exp), reads=["ee%d" % ib], writes=["ee%d" % ib])
                    psk, pkk = self.next_psb()
                    for j in range(NJ):
                        kb.op("tensor", lambda e, j=j, cs=cs, psk=psk: e.transpose(out=psk[:, j * 128:(j + 1) * 128], in_=XT["k"][:, j, cs], identity=self.ident[:]),
                              reads=[("kT", j), "ident"], writes=[pkk])
                    for h in range(H):
                        hs = slice(h * DH, (h + 1) * DH)
                        kb.op("vector", lambda e, h=h, hs=hs, psk=psk: e.tensor_scalar(out=Ktm[:, hs], in0=psk[:, hs], scalar1=ee[:, h:h + 1], scalar2=None, op0=ALU.mult),
                              reads=[pkk, "ee%d" % ib], writes=[("Ktm", ib, h)])
                def _back(c):
                    nonlocal pi, dci
                    r0 = t0 + c * 128
                    cs = slice(c * 128, (c + 1) * 128)
                    ib = (tb * NC + c) % 2
                    vext, go, gt, lf, bb, ee, emb, dec, Ktm, ssq, dm, t1, sc = (vext_l[ib], go_l[ib], gt_l[ib], lf_l[ib], bb_l[ib], ee_l[ib], emb_l[ib], dec_l[ib], Ktm_l[ib], ssq_l[ib], dm_l[ib], t1_l[ib], sc_l[ib])
                    psS, pkS = self.bank(0)
                    psn = [self.bank(1), self.bank(2)]
                    for h in range(H):
                        (ja, a0, a1, oa), (jb, b0, b1, ob_) = pieces[h]
                        kb.op("tensor", lambda e, h=h, cs=cs, ja=ja, a0=a0, a1=a1, psS=psS: e.matmul(
                            out=psS[:, h * 128:(h + 1) * 128], lhsT=XT["k"][a0:a1, ja, cs], rhs=XT["q"][a0:a1, ja, cs], start=True, stop=False),
                            reads=[("kT", ja), ("qT", ja)], writes=[pkS])
                        kb.op("tensor", lambda e, h=h, cs=cs, jb=jb, b0=b0, b1=b1, psS=psS: e.matmul(
                            out=psS[:, h * 128:(h + 1) * 128], lhsT=XT["k"][b0:b1, jb, cs], rhs=XT["q"][b0:b1, jb, cs], start=False, stop=True),
                            reads=[("kT", jb), ("qT", jb)], writes=[pkS])
                    for h in range(H):
                        kb.op("vector", lambda e, h=h, psS=psS, ib=ib: e.scalar_tensor_tensor(
                            out=PT4[ib][:, h, :], in0=psS[:, h * 128:(h + 1) * 128], scalar=ee[:, h:h + 1], in1=self.m_ge[:], op0=ALU.mult, op1=ALU.mult),
                            reads=[pkS, "ee%d" % ib, "m_ge"], writes=[("PT4", ib, h)])
                    for h in range(H):
                        (ja, a0, a1, oa), (jb, b0, b1, ob_) = pieces[h]
                        pn, pkn = psn[h // 2]
                        ncol = slice((h % 2) * 256, (h % 2) * 256 + DH + 1)
                        kb.op("tensor", lambda e, h=h, pn=pn, ncol=ncol, ib=ib: e.matmul(out=pn[:, ncol], lhsT=PT4[ib][:, h, :], rhs=vext[:, h, :], start=True, stop=False),
                              reads=[("PT4", ib, h), "vext%d" % ib], writes=[pkn])
                        kb.op("tensor", lambda e, cs=cs, ja=ja, a0=a0, a1=a1, pn=pn, ncol=ncol: e.matmul(
                            out=pn[:, ncol], lhsT=XT["q"][a0:a1, ja, cs], rhs=Cb[a0:a1, ja, :], start=False, stop=False),
                            reads=[("qT", ja), ("Cb", ja // 2)], writes=[pkn])
                        kb.op("tensor", lambda e, cs=cs, jb=jb, b0=b0, b1=b1, pn=pn, ncol=ncol: e.matmul(
                            out=pn[:, ncol], lhsT=XT["q"][b0:b1, jb, cs], rhs=Cb[b0:b1, jb, :], start=False, stop=True),
                            reads=[("qT", jb), ("Cb", jb // 2)], writes=[pkn])
                    for h in range(H):
                        (ja, a0, a1, oa), (jb, b0, b1, ob_) = pieces[h]
                        for (jj, q0, q1, off) in ((ja, a0, a1, oa), (jb, b0, b1, ob_)):
                            pc, pkc = self.bank(3 + jj // 2)
                            cc0 = (jj % 2) * 256
                            kb.op("tensor", lambda e, h=h, q0=q0, q1=q1, off=off, pc=pc, cc0=cc0: e.matmul(
                                out=pc[q0:q1, cc0:cc0 + DH + 1], lhsT=Ktm[:, h * DH + off:h * DH + off + (q1 - q0)], rhs=vext[:, h, :], start=True, stop=True),
                                reads=[("Ktm", ib, h), "vext%d" % ib], writes=[pkc])
                    for k2 in range(3):
                        pc, pkc = self.bank(3 + k2)
                        dcb = self.bc_last(decT_l[ib][:, 2 * k2:2 * k2 + 2], DH + 1)
                        kb.op("vector", lambda e, k2=k2, pc=pc, dcb=dcb: e.tensor_tensor(
                            out=dC2[k2][:], in0=pc[:, :].rearrange("p (a b) -> p a b", b=256)[:, :, 0:DH + 1], in1=dcb, op=ALU.mult),
                            reads=[pkc, "decT%d" % ib], writes=[("dC2", k2)])
                        kb.op("vector", lambda e, k2=k2, dcb=dcb: e.tensor_tensor(
                            out=Cst[:, 2 * k2:2 * k2 + 2, :], in0=Cst[:, 2 * k2:2 * k2 + 2, :], in1=dcb, op=ALU.mult),
                            reads=[("Cst", k2), "decT%d" % ib, ("Cb", k2)], writes=[("Cst", k2)])
                        kb.op("gpsimd", lambda e, k2=k2: e.tensor_tensor(
                            out=Cst[:, 2 * k2:2 * k2 + 2, :], in0=Cst[:, 2 * k2:2 * k2 + 2, :], in1=dC2[k2][:], op=ALU.add),
                            reads=[("Cst", k2), ("dC2", k2)], writes=[("Cst", k2)])
                        kb.op("scalar", lambda e, k2=k2: e.copy(out=Cb[:, 2 * k2:2 * k2 + 2, :], in_=Cst[:, 2 * k2:2 * k2 + 2, :]),
                              reads=[("Cst", k2)], writes=[("Cb", k2)])
                    for h in range(H):
                        pn, pkn = psn[h // 2]
                        c0 = (h % 2) * 256
                        kb.op("vector", lambda e, h=h, pn=pn, c0=c0: e.tensor_copy(out=dm[:, h:h + 1], in_=pn[:, c0 + DH:c0 + DH + 1]),
                              reads=[pkn], writes=["dm%d" % ib])
                        kb.op("scalar", lambda e, h=h, pn=pn, c0=c0: e.activation(out=junk_l[h % 2][:], in_=pn[:, c0:c0 + DH], func=AF.Square, accum_out=ssq[:, h:h + 1]),
                              reads=[pkn], writes=["junk%d" % (h % 2), "ssq%d" % ib])
                    kb.op("vector", lambda e: e.tensor_scalar(out=t1[:], in0=dm[:], scalar1=-1.0, scalar2=None, op0=ALU.mult), reads=["dm%d" % ib], writes=["t1%d" % ib])
                    kb.op("vector", lambda e: e.tensor_tensor(out=dm[:], in0=dm[:], in1=t1[:], op=ALU.max), reads=["dm%d" % ib, "t1%d" % ib], writes=["dm%d" % ib])
                    kb.op("vector", lambda e: e.tensor_tensor(out=dm[:], in0=dm[:], in1=emb[:], op=ALU.max), reads=["dm%d" % ib, "emb%d" % ib], writes=["dm%d" % ib])
                    kb.op("vector", lambda e: e.reciprocal(out=dm[:], in_=dm[:]), reads=["dm%d" % ib], writes=["dm%d" % ib])
                    kb.op("vector", lambda e: e.tensor_tensor(out=t1[:], in0=dm[:], in1=dm[:], op=ALU.mult), reads=["dm%d" % ib], writes=["t1%d" % ib])
                    kb.op("vector", lambda e: e.tensor_tensor(out=t1[:], in0=t1[:], in1=ssq[:], op=ALU.mult), reads=["t1%d" % ib, "ssq%d" % ib], writes=["t1%d" % ib])
                    kb.op("scalar", lambda e: e.activation(out=t1[:], in_=t1[:], func=AF.Sqrt, bias=epsc[:], scale=1.0 / DH), reads=["t1%d" % ib, "epsc"], writes=["t1%d" % ib])
                    kb.op("vector", lambda e: e.reciprocal(out=t1[:], in_=t1[:]), reads=["t1%d" % ib], writes=["t1%d" % ib])
                    kb.op("vector", lambda e: e.tensor_tensor(out=sc[:], in0=t1[:], in1=dm[:], op=ALU.mult), reads=["t1%d" % ib, "dm%d" % ib], writes=["sc%d" % ib])
                    for h in range(H):
                        pn, pkn = psn[h // 2]
                        c0 = (h % 2) * 256
                        hs = slice(h * DH, (h + 1) * DH)
                        kb.op("vector", lambda e, h=h, pn=pn, c0=c0, hs=hs, ib=ib: e.scalar_tensor_tensor(
                            out=yo[ib][:, hs], in0=pn[:, c0:c0 + DH], scalar=sc[:, h:h + 1], in1=go[:, hs], op0=ALU.mult, op1=ALU.mult),
                            reads=[pkn, "sc%d" % ib, "go%d" % ib], writes=["yo%d" % ib])
                    kb.dma("gpsimd", y_tm[r0:r0 + 128, :], yo[ib][:], reads=["yo%d" % ib])
                _front(0)
                for c in range(NC):
                    if c + 1 < NC:
                        _front(c + 1)
                    _back(c)
            kb.flush()

    def phase_s5(self, u_fm, lre_d, lim_d, ls_d, bre_d, bim_d, cre_d, cim_d, dsk_d, gw_d, gb_d, y_fm):
        nc, kb, T = self.nc, self.kb, self.T
        L = min(512, T)
        TWO_PI = 2.0 * float(np.pi)
        with ExitStack() as st:
            S = lambda name, shape, dt: sb(st, nc, name, shape, dt)
            V = lambda fn, r, w: kb.op("vector", fn, reads=r, writes=w)
            lre = S("lre", [128, 8], F32)
            lim = S("lim", [128, 8], F32)
            stp = S("stp", [128, 8], F32)
            kb.dma("sync", lre[:], bass.AP(lre_d.tensor, lre_d.offset, [[1, 128], [128, 8]]), writes=["lre"], allow_slow_non_contiguous=True)
            kb.dma("sync", lim[:], bass.AP(lim_d.tensor, lim_d.offset, [[1, 128], [128, 8]]), writes=["lim"], allow_slow_non_contiguous=True)
            for two in range(2):
                kb.dma("sync", stp[two * 64:(two + 1) * 64, :], bass.AP(ls_d.tensor, ls_d.offset + two, [[0, 64], [2, 8]]),
                       writes=[("stp", two)], allow_slow_non_contiguous=True)
            are = S("are", [128, 8], F32)
            th = S("th", [128, 8], F32)
            rho = S("rho", [128, 8], F32)
            cs1 = S("cs1", [128, 8], F32)
            sn1 = S("sn1", [128, 8], F32)
            w1 = S("w1", [128, 8], F32)
            w2 = S("w2", [128, 8], F32)
            wi = S("wi", [128, 8], mybir.dt.int32)
            cfr = S("cfr", [128, 8], F32)
            cfi = S("cfi", [128, 8], F32)
            kb.op("scalar", lambda e: e.activation(out=stp[:], in_=stp[:], func=AF.Exp), reads=[("stp", 0), ("stp", 1)], writes=["stp"])
            V(lambda e: e.tensor_tensor(out=are[:], in0=lre[:], in1=stp[:], op=ALU.mult), ["lre", "stp"], ["are"])
            V(lambda e: e.tensor_tensor(out=th[:], in0=lim[:], in1=stp[:], op=ALU.mult), ["lim", "stp"], ["th"])
            kb.op("scalar", lambda e: e.activation(out=rho[:], in_=are[:], func=AF.Exp), reads=["are"], writes=["rho"])

            def sin_of(dst, shift):
                V(lambda e: e.tensor_scalar(out=w1[:], in0=th[:], scalar1=shift, scalar2=1.0 / TWO_PI, op0=ALU.add, op1=ALU.mult), ["th"], ["w1"])
                V(lambda e: e.tensor_copy(out=wi[:], in_=w1[:]), ["w1"], ["wi"])
                V(lambda e: e.tensor_copy(out=w2[:], in_=wi[:]), ["wi"], ["w2"])
                V(lambda e: e.tensor_tensor(out=w1[:], in0=w1[:], in1=w2[:], op=ALU.subtract), ["w1", "w2"], ["w1"])
                V(lambda e: e.tensor_scalar(out=w2[:], in0=w1[:], scalar1=0.5, scalar2=None, op0=ALU.is_gt), ["w1"], ["w2"])
                V(lambda e: e.tensor_tensor(out=w1[:], in0=w1[:], in1=w2[:], op=ALU.subtract), ["w1", "w2"], ["w1"])
                V(lambda e: e.tensor_scalar(out=w2[:], in0=w1[:], scalar1=-0.5, scalar2=None, op0=ALU.is_lt), ["w1"], ["w2"])
                V(lambda e: e.tensor_tensor(out=w1[:], in0=w1[:], in1=w2[:], op=ALU.add), ["w1", "w2"], ["w1"])
                kb.op("scalar", lambda e: e.activation(out=dst[:], in_=w1[:], func=AF.Sin, scale=TWO_PI), reads=["w1"], writes=[dst.name])
            sin_of(sn1, 0.0)
            sin_of(cs1, 0.5 * float(np.pi))
            lbr = S("lbr", [128, 8], F32)
            lbi = S("lbi", [128, 8], F32)
            den = S("den", [128, 8], F32)
            V(lambda e: e.tensor_tensor(out=lbr[:], in0=rho[:], in1=cs1[:], op=ALU.mult), ["rho", cs1.name], ["lbr"])
            V(lambda e: e.tensor_tensor(out=lbi[:], in0=rho[:], in1=sn1[:], op=ALU.mult), ["rho", sn1.name], ["lbi"])
            V(lambda e: e.tensor_scalar(out=lbr[:], in0=lbr[:], scalar1=-1.0, scalar2=None, op0=ALU.add), ["lbr"], ["lbr"])
            V(lambda e: e.tensor_tensor(out=den[:], in0=lre[:], in1=lre[:], op=ALU.mult), ["lre"], ["den"])
            V(lambda e: e.tensor_tensor(out=w1[:], in0=lim[:], in1=lim[:], op=ALU.mult), ["lim"], ["w1"])
            V(lambda e: e.tensor_tensor(out=den[:], in0=den[:], in1=w1[:], op=ALU.add), ["den", "w1"], ["den"])
            V(lambda e: e.reciprocal(out=den[:], in_=den[:]), ["den"], ["den"])
            V(lambda e: e.tensor_tensor(out=w1[:], in0=lbr[:], in1=lre[:], op=ALU.mult), ["lbr", "lre"], ["w1"])
            V(lambda e: e.tensor_tensor(out=w2[:], in0=lbi[:], in1=lim[:], op=ALU.mult), ["lbi", "lim"], ["w2"])
            V(lambda e: e.tensor_tensor(out=w1[:], in0=w1[:], in1=w2[:], op=ALU.add), ["w1", "w2"], ["w1"])
            V(lambda e: e.tensor_tensor(out=cfr[:], in0=w1[:], in1=den[:], op=ALU.mult), ["w1", "den"], ["cfr"])
            V(lambda e: e.tensor_tensor(out=w1[:], in0=lbi[:], in1=lre[:], op=ALU.mult), ["lbi", "lre"], ["w1"])
            V(lambda e: e.tensor_tensor(out=w2[:], in0=lbr[:], in1=lim[:], op=ALU.mult), ["lbr", "lim"], ["w2"])
            V(lambda e: e.tensor_tensor(out=w1[:], in0=w1[:], in1=w2[:], op=ALU.subtract), ["w1", "w2"], ["w1"])
            V(lambda e: e.tensor_tensor(out=cfi[:], in0=w1[:], in1=den[:], op=ALU.mult), ["w1", "den"], ["cfi"])
            Ct = S("Ct", [128, 8, L], F32)
            Sn = S("Sn", [128, 8, L], F32)
            ta = S("ta", [128, 8, L // 2], F32)
            tb_ = S("tb_", [128, 8, L // 2], F32)
            V(lambda e: e.tensor_copy(out=Ct[:, :, 0], in_=cs1[:]), [cs1.name], ["Ct"])
            V(lambda e: e.tensor_copy(out=Sn[:, :, 0], in_=sn1[:]), [sn1.name], ["Sn"])
            n = 1
            while n < L:
                cn = self.bc_last(Ct[:, :, n - 1], n)
                sn = self.bc_last(Sn[:, :, n - 1], n)
                V(lambda e, n=n, cn=cn: e.tensor_tensor(out=ta[:, :, 0:n], in0=Ct[:, :, 0:n], in1=cn, op=ALU.mult), ["Ct"], ["ta"])
                V(lambda e, n=n, sn=sn: e.tensor_tensor(out=tb_[:, :, 0:n], in0=Sn[:, :, 0:n], in1=sn, op=ALU.mult), ["Sn"], ["tb_"])
                V(lambda e, n=n: e.tensor_tensor(out=ta[:, :, 0:n], in0=ta[:, :, 0:n], in1=tb_[:, :, 0:n], op=ALU.subtract), ["ta", "tb_"], ["ta"])
                V(lambda e, n=n, sn=sn: e.tensor_tensor(out=tb_[:, :, 0:n], in0=Ct[:, :, 0:n], in1=sn, op=ALU.mult), ["Ct", "Sn"], ["tb_"])
                V(lambda e, n=n: e.tensor_copy(out=Ct[:, :, n:2 * n], in_=ta[:, :, 0:n]), ["ta"], ["Ct"])
                V(lambda e, n=n, cn=cn: e.tensor_tensor(out=ta[:, :, 0:n], in0=Sn[:, :, 0:n], in1=cn, op=ALU.mult), ["Sn", "Ct"], ["ta"])
                V(lambda e, n=n: e.tensor_tensor(out=Sn[:, :, n:2 * n], in0=ta[:, :, 0:n], in1=tb_[:, :, 0:n], op=ALU.add), ["ta", "tb_"], ["Sn"])
                n *= 2
            BTf = S("BTf", [128, 8, 2, 128], F32)
            CTf = S("CTf", [128, 8, 2, 128], F32)
            BT = S("BT", [128, 8, 2, 128], BF16)
            CT = S("CT", [128, 8, 2, 128], BF16)
            kb.op("gpsimd", lambda e: e.memset(BTf[:], 0.0), writes=["BTf"])
            kb.op("gpsimd", lambda e: e.memset(CTf[:], 0.0), writes=["CTf"])
            for g in range(16):
                j, two, gl = g // 2, g % 2, g % 8
                for ri, (bd, cd) in enumerate(((bre_d, cre_d), (bim_d, cim_d))):
                    kb.dma("sync", BTf[gl * 16:(gl + 1) * 16, j, ri, two * 64:(two + 1) * 64], bd[g].rearrange("p c -> c p"),
                           reads=["BTf"], writes=[("BTf", g, ri)], allow_slow_non_contiguous=True)
                    kb.dma("sync", CTf[two * 64:(two + 1) * 64, j, ri, gl * 16:(gl + 1) * 16], cd[g].rearrange("c p -> p c"),
                           reads=["CTf"], writes=[("CTf", g, ri)], allow_slow_non_contiguous=True)
            bkeys = [("BTf", g, ri) for g in range(16) for ri in range(2)]
            ckeys = [("CTf", g, ri) for g in range(16) for ri in range(2)]
            V(lambda e: e.tensor_copy(out=BT[:], in_=BTf[:]), bkeys, ["BT"])
            c1 = S("c1", [128, 8, 128], F32)
            c2 = S("c2", [128, 8, 128], F32)
            cr_b = self.bc_last(cfr[:, :], 128)
            ci_b = self.bc_last(cfi[:, :], 128)
            V(lambda e: e.tensor_tensor(out=c1[:], in0=CTf[:, :, 0, :], in1=cr_b, op=ALU.mult), ckeys + ["cfr"], ["c1"])
            V(lambda e: e.tensor_tensor(out=c2[:], in0=CTf[:, :, 1, :], in1=ci_b, op=ALU.mult), ckeys + ["cfi"], ["c2"])
            V(lambda e: e.tensor_tensor(out=CT[:, :, 0, :], in0=c1[:], in1=c2[:], op=ALU.subtract), ["c1", "c2"], [("CT", 0)])
            V(lambda e: e.tensor_tensor(out=c1[:], in0=CTf[:, :, 0, :], in1=ci_b, op=ALU.mult), ckeys + ["cfi"], ["c1"])
            V(lambda e: e.tensor_tensor(out=c2[:], in0=CTf[:, :, 1, :], in1=cr_b, op=ALU.mult), ckeys + ["cfr"], ["c2"])
            V(lambda e: e.scalar_tensor_tensor(out=CT[:, :, 1, :], in0=c1[:], scalar=-1.0, in1=c2[:], op0=ALU.mult, op1=ALU.subtract),
              ["c1", "c2"], [("CT", 1)])
            Gw = S("Gw", [128, 2, 256], BF16)
            kb.dma("gpsimd", Gw[:], gw_d.rearrange("(m p) n -> p m n", p=128), writes=["Gw"])
            dsk = S("dsk", [128, 2], F32)
            gbi = S("gbi", [128, 2], F32)
            kb.dma("sync", dsk[:], dsk_d.rearrange("(m p) -> p m", p=128), writes=["dsk"], allow_slow_non_contiguous=True)
            kb.dma("sync", gbi[:], gb_d.rearrange("(m p) -> p m", p=128), writes=["gbi"], allow_slow_non_contiguous=True)
            h0r = S("h0r", [128, 8], F32)
            h0i = S("h0i", [128, 8], F32)
            kb.op("gpsimd", lambda e: e.memset(h0r[:], 0.0), writes=["h0r"])
            kb.op("gpsimd", lambda e: e.memset(h0i[:], 0.0), writes=["h0i"])
            uin = [S("uin%d" % i, [128, 2, L], F32) for i in range(2)]
            ub = S("ub", [128, 2, L], BF16)
            xr_ = [S("xr%d" % i, [128, L], F32) for i in range(2)]
            xi_ = [S("xi%d" % i, [128, L], F32) for i in range(2)]
            p1_ = [S("p1%d" % i, [128, L], F32) for i in range(4)]
            p2_ = [S("p2%d" % i, [128, L], F32) for i in range(4)]
            zr_ = [S("zr%d" % i, [128, L], F32) for i in range(2)]
            zi_ = [S("zi%d" % i, [128, L], F32) for i in range(2)]
            gr_ = [S("gr%d" % i, [128, L], F32) for i in range(2)]
            gi_ = [S("gi%d" % i, [128, L], F32) for i in range(2)]
            hr = [S("hr%d" % i, [128, L], BF16) for i in range(2)]
            hi = [S("hi%d" % i, [128, L], BF16) for i in range(2)]
            yy = S("yy", [128, 2, L], F32)
            y2 = S("y2", [128, L], F32)
            zb = S("zb", [128, 2, L], BF16)
            zf = S("zf", [128, 2, L], F32)
            sgl = S("sgl", [128, L], F32)
            yo = [S("yo%d" % i, [128, 2, L], BF16) for i in range(2)]
            uv = u_fm.rearrange("(m p) t -> p m t", p=128)
            yv = y_fm.rearrange("(m p) t -> p m t", p=128)
            hb_i = 0
            for blk in range(T // L):
                t0 = blk * L
                ib = blk % 2
                kb.dma("sync", uin[ib][:], uv[:, :, t0:t0 + L], writes=["uin%d" % ib])
                V(lambda e, ib=ib: e.tensor_copy(out=ub[:], in_=uin[ib][:]), ["uin%d" % ib], ["ub"])
                psy = [self.bank(0), self.bank(1)]
                def _tile(j):
                    nonlocal hb_i
                    m = j // 4
                    jb = j % 2
                    xr, xi, zr, zi, gr, gi = xr_[jb], xi_[jb], zr_[jb], zi_[jb], gr_[jb], gi_[jb]
                    kxr_, kxi_, kzr, kzi, kgr, kgi = ['%s%d' % (n_, jb) for n_ in ('xr', 'xi', 'zr', 'zi', 'gr', 'gi')]
                    pxr, kxr = self.bank(2 + (j % 2) * 2)
                    pxi, kxi = self.bank(3 + (j % 2) * 2)
                    kb.op("tensor", lambda e, j=j, m=m, pxr=pxr: e.matmul(out=pxr[:, 0:L], lhsT=BT[:, j, 0, :], rhs=ub[:, m, :], start=True, stop=True),
                          reads=["BT", "ub"], writes=[kxr])
                    kb.op("tensor", lambda e, j=j, m=m, pxi=pxi: e.matmul(out=pxi[:, 0:L], lhsT=BT[:, j, 1, :], rhs=ub[:, m, :], start=True, stop=True),
                          reads=["BT", "ub"], writes=[kxi])
                    kb.op("scalar", lambda e, pxr=pxr: e.copy(out=xr[:], in_=pxr[:, 0:L]), reads=[kxr], writes=[kxr_])
                    kb.op("scalar", lambda e, pxi=pxi: e.copy(out=xi[:], in_=pxi[:, 0:L]), reads=[kxi], writes=[kxi_])
                    V(lambda e, j=j: e.tensor_tensor(out=p1_[0][:], in0=xr[:], in1=Ct[:, j, :], op=ALU.mult), [kxr_, "Ct"], ["p1_0"])
                    kb.op("gpsimd", lambda e, j=j: e.tensor_tensor(out=p2_[0][:], in0=xi[:], in1=Sn[:, j, :], op=ALU.mult), reads=[kxi_, "Sn"], writes=["p2_0"])
                    V(lambda e: e.tensor_tensor(out=zr[:], in0=p1_[0][:], in1=p2_[0][:], op=ALU.add), ["p1_0", "p2_0"], [kzr])
                    V(lambda e, j=j: e.tensor_tensor(out=p1_[1][:], in0=xi[:], in1=Ct[:, j, :], op=ALU.mult), [kxi_, "Ct"], ["p1_1"])
                    kb.op("gpsimd", lambda e, j=j: e.tensor_tensor(out=p2_[1][:], in0=xr[:], in1=Sn[:, j, :], op=ALU.mult), reads=[kxr_, "Sn"], writes=["p2_1"])
                    V(lambda e: e.tensor_tensor(out=zi[:], in0=p1_[1][:], in1=p2_[1][:], op=ALU.subtract), ["p1_1", "p2_1"], [kzi])
                    rj = rho[:, j:j + 1]
                    rb = bass.AP(rj.tensor, rj.offset, [list(rj.ap[0]), [0, L]])
                    V(lambda e, j=j, rb=rb: e.tensor_tensor_scan(out=gr[:], data0=rb, data1=zr[:], initial=h0r[:, j:j + 1], op0=ALU.mult, op1=ALU.add),
                      ["rho", kzr, ("h0r", j)], [kgr])
                    V(lambda e, j=j, rb=rb: e.tensor_tensor_scan(out=gi[:], data0=rb, data1=zi[:], initial=h0i[:, j:j + 1], op0=ALU.mult, op1=ALU.add),
                      ["rho", kzi, ("h0i", j)], [kgi])
                    hb = hb_i % 2
                    hb_i += 1
                    V(lambda e, j=j: e.tensor_tensor(out=p1_[2][:], in0=gr[:], in1=Ct[:, j, :], op=ALU.mult), [kgr, "Ct"], ["p1_2"])
                    kb.op("gpsimd", lambda e, j=j: e.tensor_tensor(out=p2_[2][:], in0=gi[:], in1=Sn[:, j, :], op=ALU.mult), reads=[kgi, "Sn"], writes=["p2_2"])
                    V(lambda e, hb=hb: e.tensor_tensor(out=hr[hb][:], in0=p1_[2][:], in1=p2_[2][:], op=ALU.subtract), ["p1_2", "p2_2"], ["hr%d" % hb])
                    V(lambda e, j=j: e.tensor_tensor(out=h0r[:, j:j + 1], in0=p1_[2][:, L - 1:L], in1=p2_[2][:, L - 1:L], op=ALU.subtract), ["p1_2", "p2_2"], [("h0r", j)])
                    V(lambda e, j=j: e.tensor_tensor(out=p1_[3][:], in0=gi[:], in1=Ct[:, j, :], op=ALU.mult), [kgi, "Ct"], ["p1_3"])
                    kb.op("gpsimd", lambda e, j=j: e.tensor_tensor(out=p2_[3][:], in0=gr[:], in1=Sn[:, j, :], op=ALU.mult), reads=[kgr, "Sn"], writes=["p2_3"])
                    V(lambda e, hb=hb: e.tensor_tensor(out=hi[hb][:], in0=p1_[3][:], in1=p2_[3][:], op=ALU.add), ["p1_3", "p2_3"], ["hi%d" % hb])
                    V(lambda e, j=j: e.tensor_tensor(out=h0i[:, j:j + 1], in0=p1_[3][:, L - 1:L], in1=p2_[3][:, L - 1:L], op=ALU.add), ["p1_3", "p2_3"], [("h0i", j)])
                    py, ky = psy[m]
                    kb.op("tensor", lambda e, j=j, hb=hb, py=py: e.matmul(out=py[:, 0:L], lhsT=CT[:, j, 0, :], rhs=hr[hb][:], start=(j % 4 == 0), stop=False),
                          reads=[("CT", 0), "hr%d" % hb], writes=[ky])
                    kb.op("tensor", lambda e, j=j, hb=hb, py=py: e.matmul(out=py[:, 0:L], lhsT=CT[:, j, 1, :], rhs=hi[hb][:], start=False, stop=(j % 4 == 3)),
                          reads=[("CT", 1), "hi%d" % hb], writes=[ky])
                for j in range(8):
                    _tile(j)
                for m in range(2):
                    py, ky = psy[m]
                    V(lambda e, m=m, py=py, ib=ib: e.scalar_tensor_tensor(out=yy[:, m, :], in0=uin[ib][:, m, :], scalar=dsk[:, m:m + 1], in1=py[:, 0:L],
                                                                    op0=ALU.mult, op1=ALU.add), [ky, "dsk", "uin%d" % ib], [("yy", m)])
                    V(lambda e, m=m: e.tensor_tensor(out=y2[:], in0=yy[:, m, :], in1=yy[:, m, :], op=ALU.mult), [("yy", m)], ["y2"])
                    V(lambda e: e.tensor_scalar(out=y2[:], in0=y2[:], scalar1=0.044715, scalar2=1.0, op0=ALU.mult, op1=ALU.add), ["y2"], ["y2"])
                    V(lambda e, m=m: e.tensor_tensor(out=y2[:], in0=y2[:], in1=yy[:, m, :], op=ALU.mult), ["y2", ("yy", m)], ["y2"])
                    kb.op("scalar", lambda e: e.activation(out=y2[:], in_=y2[:], func=AF.Sigmoid, scale=1.5957691216057308), reads=["y2"], writes=["y2"])
                    V(lambda e, m=m: e.tensor_tensor(out=zf[:, m, :], in0=y2[:], in1=yy[:, m, :], op=ALU.mult), ["y2", ("yy", m)], [("zf", m)])
                    kb.op("gpsimd", lambda e, m=m: e.tensor_copy(out=zb[:, m, :], in_=zf[:, m, :]), reads=[("zf", m)], writes=[("zb", m)])
                for m2 in range(2):
                    pg, kg = self.bank(2 + m2)
                    for m in range(2):
                        kb.op("tensor", lambda e, m=m, m2=m2, pg=pg: e.matmul(out=pg[:, 0:L], lhsT=Gw[:, m, m2 * 128:(m2 + 1) * 128], rhs=zb[:, m, :],
                                                                           start=(m == 0), stop=(m == 1)), reads=["Gw", ("zb", 0), ("zb", 1)], writes=[kg])
                    kb.op("scalar", lambda e, m2=m2, pg=pg: e.activation(out=sgl[:], in_=pg[:, 0:L], func=AF.Sigmoid, bias=gbi[:, m2:m2 + 1]),
                          reads=[kg, "gbi"], writes=["sgl"])
                    V(lambda e, m2=m2, ib=ib: e.tensor_tensor(out=yo[ib][:, m2, :], in0=sgl[:], in1=zf[:, m2, :], op=ALU.mult), ["sgl", ("zf", m2)], [("yo", ib, m2)])
                kb.dma("gpsimd", yv[:, :, t0:t0 + L], yo[ib][:], reads=[("yo", ib, 0), ("yo", ib, 1)])
            kb.flush()

    def phase_rwkv7(self, u_fm, mu_d, w0_d, w2_d, a0_d, a2_d, g2_d, kk_d, ka_d, rk_d, lnw_d, lnb_d, y_tm):
        nc, kb, T = self.nc, self.kb, self.T
        TB = min(512, T)
        NCH = TB // 64
        NP = TB // 128
        with ExitStack() as st:
            S = lambda name, shape, dt: sb(st, nc, name, shape, dt)
            V = lambda fn, r, w: kb.op("vector", fn, reads=r, writes=w)
            G = lambda fn, r, w: kb.op("gpsimd", fn, reads=r, writes=w)
            A = lambda fn, r, w: kb.op("scalar", fn, reads=r, writes=w)
            PE = lambda fn, r, w: kb.op("tensor", fn, reads=r, writes=w)
            mu = S("mu", [128, 14], F32)
            kb.dma("sync", mu[:, 0:13], mu_d[0:1664].rearrange("(c p) -> p c", p=128), writes=[("mu", 0)], allow_slow_non_contiguous=True)
            kb.dma("sync", mu[0:32, 13:14], mu_d[1664:1696].rearrange("(c p) -> p c", p=32), writes=[("mu", 1)], allow_slow_non_contiguous=True)
            mukeys = [("mu", 0), ("mu", 1)]
            pcs = {}
            for nm, dd in (("w0", w0_d), ("a0", a0_d), ("kk", kk_d), ("ka", ka_d), ("rk", rk_d)):
                t = S("pc_" + nm, [128, 4], F32)
                kb.dma("sync", t[:], dd.rearrange("(c p) -> p c", p=128), writes=["pc_" + nm], allow_slow_non_contiguous=True)
                pcs[nm] = t
            omka = S("omka", [128, 4], F32)
            V(lambda e: e.tensor_scalar(out=omka[:], in0=pcs["ka"][:], scalar1=-1.0, scalar2=1.0, op0=ALU.mult, op1=ALU.add), ["pc_ka"], ["omka"])
            LW = S("LW", [128, 4, 512], BF16)
            G(lambda e: e.memset(LW[:], 0.0), [], ["LW"])
            kb.dma("gpsimd", LW[0:32, 0, :], w2_d, reads=["LW"], writes=[("LW", 0)])
            kb.dma("gpsimd", LW[32:64, 1, :], a2_d, reads=["LW"], writes=[("LW", 1)])
            kb.dma("gpsimd", LW[64:128, 2, :], g2_d[0:64, :], reads=["LW"], writes=[("LW", 2)])
            kb.dma("gpsimd", LW[0:32, 3, :], g2_d[64:96, :], reads=["LW"], writes=[("LW", 3)])
            lnw_bc = S("lnw_bc", [128, 512], F32)
            lnb_bc = S("lnb_bc", [128, 512], F32)
            self.load_bcast(lnw_bc[:], lnw_d, 512, "lnw_bc")
            self.load_bcast(lnb_bc[:], lnb_d, 512, "lnb_bc")
            blk2 = S("blk2", [128, 128], F32)
            IND = S("IND", [128, 2], BF16)
            G(lambda e: e.memset(blk2[:], 0.0), [], ["blk2"])
            G(lambda e: e.memset(blk2[0:64, 0:64], 1.0), [], ["blk2"])
            G(lambda e: e.memset(blk2[64:128, 64:128], 1.0), [], ["blk2"])
            G(lambda e: e.memset(IND[:], 0.0), [], ["IND"])
            G(lambda e: e.memset(IND[0:64, 0:1], 1.0), [], ["IND"])
            G(lambda e: e.memset(IND[64:128, 1:2], 1.0), [], ["IND"])
            MG = S("MG", [128, 128], F32)
            for rh in range(2):
                for ch in range(2):
                    G(lambda e, rh=rh, ch=ch: e.affine_select(
                        out=MG[rh * 64:(rh + 1) * 64, ch * 64:(ch + 1) * 64], in_=self.ones_f[rh * 64:(rh + 1) * 64, 0:64], pattern=[[1, 64]],
                        compare_op=ALU.is_ge, fill=0.0, base=(-1 if ch == 0 else 0), channel_multiplier=-1), ["ones_f"], ["MG"])
            ML = S("ML", [64, 64], F32)
            G(lambda e: e.affine_select(out=ML[:], in_=self.ones_f[0:64, 0:64], pattern=[[-1, 64]], compare_op=ALU.is_ge, fill=0.0,
                                        base=-1, channel_multiplier=1), ["ones_f"], ["ML"])
            rmask = S("rmask", [128, TB], F32)
            G(lambda e: e.memset(rmask[:], 1.0), [], ["rmask"])
            G(lambda e: e.memset(rmask[:].rearrange("p (c l) -> p c l", l=64)[:, :, 0:1], 0.0), [], ["rmask"])
            tiny = S("tiny", [128, 1], F32)
            gneps = S("gneps", [128, 1], F32)
            G(lambda e: e.memset(tiny[:], 1e-12), [], ["tiny"])
            G(lambda e: e.memset(gneps[:], GN_EPS), [], ["gneps"])
            uin = S("uin", [128, 14, TB + 1], F32)
            dtmp = S("dtmp", [128, TB], F32)
            lo = S("lo", [128, 2, TB], BF16)
            g_sb = S("g_sb", [128, NP, 512], F32)
            lw = S("lw", [128, TB], F32)
            av = S("av", [128, TB], F32)
            kq = S("kq", [128, TB], F32)
            sq = S("sq", [128, TB], F32)
            rn = S("rn", [128, TB], F32)
            kkv = S("kkv", [128, TB], F32)
            kp = S("kp", [128, TB], F32)
            cc = S("cc", [128, TB], F32)
            w1 = S("w1", [128, TB], F32)
            w2t = S("w2t", [128, TB], F32)
            er = S("er", [128, TB], F32)
            AR = S("AR", [128, 4, 2, NCH, 2, 64], BF16)
            Zv = [S("Zv%d" % i, [128, 8, 64], BF16) for i in range(2)]
            BK = S("BK", [128, 4, NCH, 2, 64], BF16)
            WL = S("WL", [128, 4, NCH], F32)
            xvb = S("xvb", [128, 4, 64 + TB], BF16)
            rkb = S("rkb", [128, 4, TB], BF16)
            Z = [S("Z%d" % i, [128, 8, 64], BF16) for i in range(2)]
            Gm = [S("Gm%d" % i, [128, 8, 128], BF16) for i in range(2)]
            BKtm = [S("BKtm%d" % i, [128, 8, 64], BF16) for i in range(2)]
            An = [[S("An%d_%d" % (q, i), [64, 8, 64], BF16) for i in range(2)] for q in range(2)]
            Bn = [[S("Bn%d_%d" % (q, i), [64, 8, 64], BF16) for i in range(2)] for q in range(2)]
            MT = [S("MT%d" % i, [64, 8, 64], BF16) for i in range(2)]
            Xb = [S("Xb%d" % i, [64, 8, 64], BF16) for i in range(2)]
            ST = S("ST", [128, 4, 64], F32)
            STb = S("STb", [128, 4, 64], BF16)
            stmp = S("stmp", [128, 4, 64], F32)
            ysb = S("ysb", [128, 512], F32)
            ysq = S("ysq", [128, 512], F32)
            s1 = S("s1", [128, 8], F32)
            s2 = S("s2", [128, 8], F32)
            mean = S("mean", [128, 8], F32)
            var = S("var", [128, 8], F32)
            bsum = S("bsum", [128, 8], F32)
            yt1 = S("yt1", [128, 512], F32)
            yt2 = S("yt2", [128, 512], F32)
            yo = [S("yo%d" % i, [128, 512], BF16) for i in range(2)]
            G(lambda e: e.memset(ST[:], 0.0), [], ["ST"])
            G(lambda e: e.memset(STb[:], 0.0), [], ["STb"])
            G(lambda e: e.memset(xvb[:], 0.0), [], ["xvb"])
            G(lambda e: e.memset(uin[:, :, 0:1], 0.0), [], ["uin"])
            G(lambda e: e.memset(AR[:], 0.0), [], ["AR"])
            G(lambda e: e.memset(Zv[0][:], 0.0), [], ["Zv"])
            G(lambda e: e.memset(Zv[1][:], 0.0), [], ["Zv"])
            G(lambda e: e.memset(lo[:], 0.0), [], ["lo"])
            identb64 = self.ident[0:64, 0:64]
            uv = u_fm[0:1664, :].rearrange("(c p) t -> p c t", p=128)
            mgb = bass.AP(MG[:].tensor, MG[:].offset, [list(MG[:].ap[0]), [0, 4], [1, 128]])
            mlb = bass.AP(ML[:].tensor, ML[:].offset, [list(ML[:].ap[0]), [0, 8], [1, 64]])
            idb = bass.AP(self.identf[0:64, 0:64].tensor, self.identf[0:64, 0:64].offset, [list(self.identf[0:64, 0:64].ap[0]), [0, 8], [1, 64]])
            oi = 0
            for tb in range(T // TB):
                t0 = tb * TB
                if tb == 0:
                    kb.dma("sync", uin[:, 0:13, 1:TB + 1], uv[:, :, 0:TB], reads=["uin"], writes=[("uin", 0)] + ["x%d" % j_ for j_ in range(13)])
                    kb.dma("sync", uin[0:32, 13, 1:TB + 1], u_fm[1664:1696, 0:TB], reads=["uin"], writes=[("uin", 1), "x13"])
                else:
                    kb.dma("sync", uin[:, 0:13, :], uv[:, :, t0 - 1:t0 + TB], reads=["uin"], writes=[("uin", 0)] + ["x%d" % j_ for j_ in range(13)])
                    kb.dma("sync", uin[0:32, 13, :], u_fm[1664:1696, t0 - 1:t0 + TB], reads=["uin"], writes=[("uin", 1), "x13"])
                ukeys = [("uin", 0), ("uin", 1)]
                for j in range(14):
                    pp = 128 if j < 13 else 32
                    V(lambda e, j=j, pp=pp: e.tensor_tensor(out=dtmp[0:pp, :], in0=uin[0:pp, j, 0:TB], in1=uin[0:pp, j, 1:TB + 1], op=ALU.subtract),
                      ukeys + ["x%d" % (j - 1)], ["dtmp"])
                    V(lambda e, j=j, pp=pp: e.scalar_tensor_tensor(out=uin[0:pp, j, 1:TB + 1], in0=dtmp[0:pp, :], scalar=mu[0:pp, j:j + 1],
                                                                   in1=uin[0:pp, j, 1:TB + 1], op0=ALU.mult, op1=ALU.add),
                      ukeys + ["dtmp"] + mukeys, ["x%d" % j])
                X = lambda j: uin[:, j, 1:TB + 1]
                A(lambda e: e.activation(out=lo[0:32, 0, :], in_=uin[0:32, 12, 1:TB + 1], func=AF.Tanh), ["x12", "lo"], [("lo", 0)])
                A(lambda e: e.copy(out=lo[32:64, 0, :], in_=uin[32:64, 12, 1:TB + 1]), ["x12"], [("lo", 1)])
                A(lambda e: e.activation(out=lo[64:128, 0, :], in_=uin[64:128, 12, 1:TB + 1], func=AF.Sigmoid), ["x12"], [("lo", 2)])
                A(lambda e: e.activation(out=lo[0:32, 1, :], in_=uin[0:32, 13, 1:TB + 1], func=AF.Sigmoid), ["x13"], [("lo", 3)])
                lokeys = [("lo", i) for i in range(4)]
                for tt in range(NP):
                    pg, kg = self.bank(tt % 2)
                    PE(lambda e, tt=tt, pg=pg: e.matmul(out=pg[:, :], lhsT=lo[:, 0, tt * 128:(tt + 1) * 128], rhs=LW[:, 2, :], start=True, stop=False),
                       lokeys + [("LW", 2)], [kg])
                    PE(lambda e, tt=tt, pg=pg: e.matmul(out=pg[:, :], lhsT=lo[:, 1, tt * 128:(tt + 1) * 128], rhs=LW[:, 3, :], start=False, stop=True),
                       lokeys + [("LW", 3)], [kg])
                    A(lambda e, tt=tt, pg=pg: e.copy(out=g_sb[:, tt, :], in_=pg[:, :]), [kg], [("g_sb", tt)])
                for ti in range(4):
                    G(lambda e, ti=ti: e.tensor_copy(out=xvb[:, ti, 64:64 + TB], in_=uin[:, 8 + ti, 1:TB + 1]), ["x%d" % (8 + ti)], [("xvb", ti)])
                for ti in range(4):
                    cs4 = slice(ti * 128, (ti + 1) * 128)
                    pw, kw_ = self.bank(2)
                    pa, ka_ = self.bank(3)
                    PE(lambda e, pw=pw, cs4=cs4: e.matmul(out=pw[:, 0:TB], lhsT=LW[:, 0, cs4], rhs=lo[:, 0, :], start=True, stop=True),
                       lokeys + [("LW", 0)], [kw_])
                    PE(lambda e, pa=pa, cs4=cs4: e.matmul(out=pa[:, 0:TB], lhsT=LW[:, 1, cs4], rhs=lo[:, 0, :], start=True, stop=True),
                       lokeys + [("LW", 1)], [ka_])
                    A(lambda e, pw=pw, ti=ti: e.activation(out=lw[:], in_=pw[:, 0:TB], func=AF.Sigmoid, bias=pcs["w0"][:, ti:ti + 1]), [kw_, "pc_w0"], ["lw"])
                    A(lambda e, pa=pa, ti=ti: e.activation(out=av[:], in_=pa[:, 0:TB], func=AF.Sigmoid, bias=pcs["a0"][:, ti:ti + 1]), [ka_, "pc_a0"], ["av"])
                    V(lambda e: e.tensor_scalar(out=lw[:], in0=lw[:], scalar1=-0.6065306597126334, scalar2=None, op0=ALU.mult), ["lw"], ["lw"])
                    V(lambda e, ti=ti: e.tensor_scalar(out=kq[:], in0=X(4 + ti), scalar1=pcs["kk"][:, ti:ti + 1], scalar2=None, op0=ALU.mult),
                      ["x%d" % (4 + ti), "pc_kk"], ["kq"])
                    G(lambda e: e.tensor_tensor(out=sq[:], in0=kq[:], in1=kq[:], op=ALU.mult), ["kq"], ["sq"])
                    pn, kn = self.bank(4)
                    PE(lambda e, pn=pn: e.matmul(out=pn[:, 0:TB], lhsT=blk2[:], rhs=sq[:], start=True, stop=True), ["blk2", "sq"], [kn])
                    A(lambda e, pn=pn: e.activation(out=rn[:], in_=pn[:, 0:TB], func=AF.Ln, bias=tiny[:]), [kn, "tiny"], ["rn"])
                    A(lambda e: e.activation(out=rn[:], in_=rn[:], func=AF.Exp, scale=-0.5), ["rn"], ["rn"])
                    V(lambda e: e.tensor_tensor(out=kkv[:], in0=kq[:], in1=rn[:], op=ALU.mult), ["kq", "rn"], ["kkv"])
                    V(lambda e, ti=ti: e.tensor_scalar(out=kp[:], in0=av[:], scalar1=pcs["ka"][:, ti:ti + 1], scalar2=omka[:, ti:ti + 1], op0=ALU.mult, op1=ALU.add),
                      ["av", "pc_ka", "omka"], ["kp"])
                    V(lambda e, ti=ti: e.tensor_tensor(out=kp[:], in0=kp[:], in1=X(4 + ti), op=ALU.mult), ["kp", "x%d" % (4 + ti)], ["kp"])
                    V(lambda e, ti=ti: e.scalar_tensor_tensor(out=rkb[:, ti, :], in0=kp[:], scalar=pcs["rk"][:, ti:ti + 1], in1=X(ti), op0=ALU.mult, op1=ALU.mult),
                      ["kp", "pc_rk", "x%d" % ti], [("rkb", ti)])
                    V(lambda e: e.tensor_tensor_scan(out=cc[:], data0=rmask[:], data1=lw[:], initial=0.0, op0=ALU.mult, op1=ALU.add), ["rmask", "lw"], ["cc"])
                    A(lambda e: e.activation(out=er[:], in_=cc[:], func=AF.Exp), ["cc"], ["er"])
                    V(lambda e, ti=ti: e.tensor_copy(out=WL[:, ti, :], in_=er[:].rearrange("p (c l) -> p c l", l=64)[:, :, 63]), ["er"], [("WL", ti)])
                    c3 = lambda t_: t_[:].rearrange("p (c l) -> p c l", l=64)
                    for pr in range(2):
                        hp = slice(pr * 64, pr * 64 + 64)
                        V(lambda e, ti=ti, pr=pr, hp=hp: e.tensor_tensor(out=AR[hp, ti, pr, :, 1, :], in0=er[hp, :].rearrange("p (c l) -> p c l", l=64),
                                                                       in1=uin[hp, ti, 1:TB + 1].rearrange("p (c l) -> p c l", l=64), op=ALU.mult),
                          ["er", "x%d" % ti, "AR"], [("AR", ti, pr, 1)])
                    V(lambda e: e.tensor_tensor(out=w1[:], in0=cc[:], in1=lw[:], op=ALU.subtract), ["cc", "lw"], ["w1"])
                    A(lambda e: e.activation(out=w1[:], in_=w1[:], func=AF.Exp), ["w1"], ["w1"])
                    for pr in range(2):
                        hp = slice(pr * 64, pr * 64 + 64)
                        V(lambda e, ti=ti, pr=pr, hp=hp: e.scalar_tensor_tensor(out=AR[hp, ti, pr, :, 0, :], in0=kkv[hp, :].rearrange("p (c l) -> p c l", l=64), scalar=-1.0,
                                                                              in1=w1[hp, :].rearrange("p (c l) -> p c l", l=64), op0=ALU.mult, op1=ALU.mult),
                          ["kkv", "w1", "AR"], [("AR", ti, pr, 0)])
                    A(lambda e: e.activation(out=w2t[:], in_=cc[:], func=AF.Exp, scale=-1.0), ["cc"], ["w2t"])
                    G(lambda e: e.tensor_tensor(out=kkv[:], in0=kkv[:], in1=av[:], op=ALU.mult), ["kkv", "av"], ["kkv"])
                    V(lambda e, ti=ti: e.tensor_tensor(out=BK[:, ti, :, 0, :], in0=c3(kkv), in1=c3(w2t), op=ALU.mult), ["kkv", "w2t"], [("BK", ti)])
                    V(lambda e, ti=ti: e.tensor_tensor(out=BK[:, ti, :, 1, :], in0=c3(kp), in1=c3(w2t), op=ALU.mult), ["kp", "w2t"], [("BK", ti)])
                arkeys = [("AR", ti, pr, x) for ti in range(4) for pr in range(2) for x in range(2)]
                bkkeys = [("BK", ti) for ti in range(4)]
                for cp in range(NCH // 2):
                    cks = (2 * cp, 2 * cp + 1)
                    bA = lambda q: self.bank(3 * q)
                    bB = lambda q: self.bank(3 * q + 1)
                    bM = lambda q: self.bank(3 * q + 2)
                    for q, c in enumerate(cks):
                        pv, kv = self.psb[q], "psb%d" % q
                        for ti in range(4):
                            PE(lambda e, ti=ti, c=c, pv=pv: e.transpose(out=pv[:, ti * 128:(ti + 1) * 128], in_=xvb[:, ti, c * 64:c * 64 + 128], identity=self.ident[:]),
                               [("xvb", ti), "ident"], [kv])
                        for ti in range(4):
                            PE(lambda e, ti=ti, c=c, pv=pv: e.transpose(out=pv[:, 512 + ti * 128:512 + (ti + 1) * 128], in_=BK[:, ti, c, :, :], identity=self.ident[:]),
                               bkkeys + ["ident"], [kv])
                        (pa, ka), (pb, kbk), (pm, km) = bA(q), bB(q), bM(q)
                        for h in range(8):
                            ti = h // 2
                            pg, kg = (pa, ka) if h < 4 else (pb, kbk)
                            PE(lambda e, h=h, ti=ti, c=c, pg=pg: e.matmul(out=pg[:, (h % 4) * 128:(h % 4 + 1) * 128], lhsT=BK[:, ti, c, :, :], rhs=AR[:, ti, h % 2, c, :, :],
                                                                      start=True, stop=True), arkeys + bkkeys, [kg])
                        for h in range(8):
                            ti = h // 2
                            PE(lambda e, h=h, ti=ti, c=c, pm=pm: e.matmul(out=pm[0:64, h * 64:(h + 1) * 64], lhsT=AR[:, ti, h % 2, c, 0, :], rhs=BK[:, ti, c, 0, :],
                                                                      start=True, stop=True), arkeys + bkkeys, [km])
                    for q, c in enumerate(cks):
                        pv, kv = self.psb[q], "psb%d" % q
                        (pa, ka), (pb, kbk), (pm, km) = bA(q), bB(q), bM(q)
                        A(lambda e, pv=pv, q=q: e.copy(out=Z[q][64:128, :, :], in_=pv[64:128, 0:512].rearrange("p (h v) -> p h v", v=64)), [kv], [("Z1", q)])
                        A(lambda e, pv=pv, q=q: e.copy(out=Zv[q][64:128, :, :], in_=pv[64:128, 0:512].rearrange("p (h v) -> p h v", v=64)), [kv, "Zv"], [("Zv1", q)])
                        A(lambda e, pv=pv, q=q: e.copy(out=BKtm[q][:], in_=pv[:, 512:1024].rearrange("p (h k) -> p h k", k=64)), [kv], [("BKtm", q)])
                        V(lambda e, pa=pa, q=q: e.tensor_tensor(out=Gm[q][:, 0:4, :], in0=pa[:, :].rearrange("p (h t) -> p h t", t=128), in1=mgb, op=ALU.mult),
                          [ka, "MG"], [("Gm", q, 0)])
                        V(lambda e, pb=pb, q=q: e.tensor_tensor(out=Gm[q][:, 4:8, :], in0=pb[:, :].rearrange("p (h t) -> p h t", t=128), in1=mgb, op=ALU.mult),
                          [kbk, "MG"], [("Gm", q, 1)])
                        V(lambda e, pm=pm, q=q: e.tensor_tensor(out=Bn[q][0][:], in0=pm[0:64, :].rearrange("p (h t) -> p h t", t=64), in1=mlb, op=ALU.mult),
                          [km, "ML"], [("Bn", q, 0)])
                        A(lambda e, q=q: e.copy(out=An[q][0][:], in_=Gm[q][0:64, :, 0:64]), [("Gm", q, 0), ("Gm", q, 1)], [("An", q, 0)])
                        V(lambda e, q=q: e.tensor_tensor(out=MT[q][:], in0=An[q][0][:], in1=idb, op=ALU.add), [("An", q, 0), "identf"], [("MT", q)])
                    for r in range(1, 7):
                        o, n_ = (r - 1) % 2, r % 2
                        for q in range(2):
                            (pa, ka), (pb, kbk), (pm, km) = bA(q), bB(q), bM(q)
                            a_o, b_o = An[q][o], Bn[q][o]
                            if r <= 4:
                                for h in range(8):
                                    PE(lambda e, h=h, pa=pa, a_o=a_o, b_o=b_o: e.matmul(out=pa[0:64, h * 64:(h + 1) * 64], lhsT=b_o[:, h, :], rhs=a_o[:, h, :], start=True, stop=True),
                                       [("An", q, o), ("Bn", q, o)], [ka])
                            if r <= 5:
                                for h in range(8):
                                    PE(lambda e, h=h, pb=pb, a_o=a_o, b_o=b_o: e.matmul(out=pb[0:64, h * 64:(h + 1) * 64], lhsT=a_o[:, h, :], rhs=b_o[:, h, :], start=True, stop=True),
                                       [("An", q, o), ("Bn", q, o)], [kbk])
                            if r >= 2:
                                for h in range(8):
                                    PE(lambda e, h=h, pm=pm, b_o=b_o, q=q: e.matmul(out=pm[0:64, h * 64:(h + 1) * 64], lhsT=b_o[:, h, :], rhs=MT[q][:, h, :], start=True, stop=True),
                                       [("Bn", q, o), ("MT", q)], [km])
                        for q in range(2):
                            (pa, ka), (pb, kbk), (pm, km) = bA(q), bB(q), bM(q)
                            if r <= 4:
                                A(lambda e, pa=pa, q=q, n_=n_: e.copy(out=An[q][n_][:], in_=pa[0:64, :].rearrange("p (h t) -> p h t", t=64)), [ka], [("An", q, n_)])
                            if r <= 5:
                                V(lambda e, pb=pb, q=q, n_=n_: e.tensor_copy(out=Bn[q][n_][:], in_=pb[0:64, :].rearrange("p (h t) -> p h t", t=64)), [kbk], [("Bn", q, n_)])
                            if r >= 2:
                                V(lambda e, pm=pm, q=q: e.tensor_tensor(out=MT[q][:], in0=MT[q][:], in1=pm[0:64, :].rearrange("p (h t) -> p h t", t=64), op=ALU.add),
                                  [("MT", q), km], [("MT", q)])
                    for q, c in enumerate(cks):
                        gmk = [("Gm", q, 0), ("Gm", q, 1)]
                        px, kx = bA(q)
                        for h in range(8):
                            ti = h // 2
                            PE(lambda e, h=h, ti=ti, c=c, px=px: e.matmul(out=px[0:64, h * 64:(h + 1) * 64], lhsT=AR[:, ti, h % 2, c, 0, :], rhs=STb[:, ti, :], start=True, stop=False),
                               arkeys + ["STb"], [kx])
                            PE(lambda e, h=h, px=px, q=q: e.matmul(out=px[0:64, h * 64:(h + 1) * 64], lhsT=Gm[q][:, h, 0:64], rhs=Zv[q][:, h, :], start=False, stop=True),
                               gmk + [("Zv1", q), "Zv"], [kx])
                        A(lambda e, px=px, q=q: e.copy(out=Xb[q][:], in_=px[0:64, :].rearrange("p (h v) -> p h v", v=64)), [kx], [("Xb", q)])
                        pu, ku = bB(q)
                        for h in range(8):
                            PE(lambda e, h=h, pu=pu, q=q: e.matmul(out=pu[0:64, h * 64:(h + 1) * 64], lhsT=MT[q][:, h, :], rhs=Xb[q][:, h, :], start=True, stop=True),
                               [("MT", q), ("Xb", q)], [ku])
                        A(lambda e, pu=pu, q=q: e.copy(out=Z[q][0:64, :, :], in_=pu[0:64, :].rearrange("p (h v) -> p h v", v=64)), [ku], [("Z0", q)])
                        zk = [("Z0", q), ("Z1", q)]
                        ps_, ks_ = bA(q)
                        for h in range(8):
                            ti, pr = h // 2, h % 2
                            PE(lambda e, h=h, ti=ti, pr=pr, ps_=ps_, q=q: e.matmul(out=ps_[pr * 64:(pr + 1) * 64, ti * 64:(ti + 1) * 64], lhsT=BKtm[q][:, h, :], rhs=Z[q][:, h, :], start=True, stop=True),
                               [("BKtm", q)] + zk, [ks_])
                        py, ky = bM(q)
                        ro = q * 64
                        for h in range(8):
                            ti = h // 2
                            PE(lambda e, h=h, ti=ti, c=c, py=py, ro=ro: e.matmul(out=py[ro:ro + 64, h * 64:(h + 1) * 64], lhsT=AR[:, ti, h % 2, c, 1, :], rhs=STb[:, ti, :],
                                                                             start=True, stop=False), arkeys + ["STb"], [ky])
                            PE(lambda e, h=h, py=py, ro=ro, q=q: e.matmul(out=py[ro:ro + 64, h * 64:(h + 1) * 64], lhsT=Gm[q][:, h, 64:128], rhs=Z[q][:, h, :], start=False, stop=True),
                               gmk + zk, [ky])
                        V(lambda e, ps_=ps_: e.tensor_tensor(out=stmp[:], in0=ST[:], in1=ps_[:, 0:256].rearrange("p (a v) -> p a v", v=64), op=ALU.add), ["ST", ks_], ["stmp"])
                        V(lambda e, c=c: e.tensor_tensor(out=ST[:], in0=stmp[:], in1=self.bc_last(WL[:, :, c], 64), op=ALU.mult), ["stmp"] + [("WL", ti) for ti in range(4)], ["ST"])
                        A(lambda e: e.copy(out=STb[:], in_=ST[:]), ["ST"], ["STb"])
                        A(lambda e, py=py, ro=ro: e.copy(out=ysb[ro:ro + 64, :], in_=py[ro:ro + 64, :]), [ky], [("ysb", q)])
                    tt = cp
                    r0 = t0 + tt * 128
                    ob = oi % 2
                    oi += 1
                    ysk = [("ysb", 0), ("ysb", 1)]
                    A(lambda e: e.activation(out=ysq[:], in_=ysb[:], func=AF.Square), ysk, ["ysq"])
                    V(lambda e: e.tensor_reduce(out=s1[:], in_=ysb[:].rearrange("p (h v) -> p h v", v=64), axis=AX.X, op=ALU.add), ysk, ["s1"])
                    V(lambda e: e.tensor_reduce(out=s2[:], in_=ysq[:].rearrange("p (h v) -> p h v", v=64), axis=AX.X, op=ALU.add), ["ysq"], ["s2"])
                    V(lambda e: e.tensor_scalar(out=mean[:], in0=s1[:], scalar1=1.0 / 64, scalar2=None, op0=ALU.mult), ["s1"], ["mean"])
                    V(lambda e: e.tensor_tensor(out=var[:], in0=mean[:], in1=mean[:], op=ALU.mult), ["mean"], ["var"])
                    V(lambda e: e.scalar_tensor_tensor(out=var[:], in0=s2[:], scalar=1.0 / 64, in1=var[:], op0=ALU.mult, op1=ALU.subtract), ["s2", "var"], ["var"])
                    A(lambda e: e.activation(out=var[:], in_=var[:], func=AF.Sqrt, bias=gneps[:]), ["var", "gneps"], ["var"])
                    V(lambda e: e.reciprocal(out=var[:], in_=var[:]), ["var"], ["var"])
                    y3 = lambda t_: t_[:].rearrange("p (h v) -> p h v", v=64)
                    V(lambda e: e.tensor_tensor(out=y3(yt1), in0=y3(ysb), in1=self.bc_last(mean[:, :], 64), op=ALU.subtract), ysk + ["mean"], ["yt1"])
                    V(lambda e: e.tensor_tensor(out=y3(yt1), in0=y3(yt1), in1=self.bc_last(var[:, :], 64), op=ALU.mult), ["yt1", "var"], ["yt1"])
                    G(lambda e: e.tensor_tensor(out=yt1[:], in0=yt1[:], in1=lnw_bc[:], op=ALU.mult), ["yt1", "lnw_bc"], ["yt1"])
                    G(lambda e: e.tensor_tensor(out=yt1[:], in0=yt1[:], in1=lnb_bc[:], op=ALU.add), ["yt1", "lnb_bc"], ["yt1"])
                    pb2, kbn = self.bank(1)
                    for ti in range(4):
                        PE(lambda e, ti=ti, tt=tt, pb2=pb2: e.matmul(out=pb2[:, ti * 2:ti * 2 + 2], lhsT=rkb[:, ti, tt * 128:(tt + 1) * 128], rhs=IND[:], start=True, stop=True),
                           [("rkb", ti), "IND"], [kbn])
                    V(lambda e, pb2=pb2: e.tensor_copy(out=bsum[:], in_=pb2[:, 0:8]), [kbn], ["bsum"])
                    pv2, kv2 = self.psb[0], "psb0"
                    for ti in range(4):
                        PE(lambda e, ti=ti, tt=tt, pv2=pv2: e.transpose(out=pv2[:, ti * 128:(ti + 1) * 128], in_=xvb[:, ti, 64 + tt * 128:64 + (tt + 1) * 128], identity=self.ident[:]),
                           [("xvb", ti), "ident"], [kv2])
                    V(lambda e, pv2=pv2: e.tensor_tensor(out=y3(yt2), in0=pv2[:, 0:512].rearrange("p (h v) -> p h v", v=64), in1=self.bc_last(bsum[:, :], 64), op=ALU.mult),
                      [kv2, "bsum"], ["yt2"])
                    V(lambda e: e.tensor_tensor(out=yt2[:], in0=yt2[:], in1=yt1[:], op=ALU.add), ["yt2", "yt1"], ["yt2"])
                    V(lambda e, tt=tt, ob=ob: e.tensor_tensor(out=yo[ob][:], in0=yt2[:], in1=g_sb[:, tt, :], op=ALU.mult), ["yt2", ("g_sb", tt)], ["yo%d" % ob])
                    kb.dma("gpsimd", y_tm[r0:r0 + 128, :], yo[ob][:], reads=["yo%d" % ob])
            kb.flush()


PARAM_NAMES = ["norm_mix", "norm_ffn", "norm_final", "w_in_even", "w_out_even", "lb_table", "a_norm",
               "b_mu", "b_w0", "b_w2", "b_a0", "b_a2", "b_g2", "b_kk", "b_ka", "b_rk", "b_ln_w", "b_ln_b",
               "w_in_odd", "w_out_odd", "c_lam_re", "c_lam_im", "c_log_step", "c_b_re", "c_b_im",
               "c_c_re", "c_c_im", "c_d", "c_glu_w", "c_glu_b", "d_conv_q", "d_conv_k", "d_i_bias",
               "d_f_bias", "d_norm", "ffn_gate", "ffn_up", "ffn_down"]


def build(T, shapes, dbg=False):
    nc = bass.Bass("TRN2", target_bir_lowering=False)
    x_d = nc.dram_tensor("x", [T, D], F32, kind="ExternalInput").ap()
    p = {n: nc.dram_tensor(n, list(shapes[n]), F32, kind="ExternalInput").ap() for n in PARAM_NAMES}
    out_d = nc.dram_tensor("out", [T, D], F32, kind="ExternalOutput").ap()
    kind = "ExternalOutput" if dbg else "Internal"
    scr = lambda name, shape, dt: nc.dram_tensor(name, shape, dt, kind=kind).ap()
    u0_fm = scr("u0_fm", [1024 + 1696, T], F32)
    u0_tm = scr("u0_tm", [T, 1024], F32)
    y0_tm = scr("y0_tm", [T, 1024], BF16)
    xm0 = scr("xm0", [T, D], F32)
    xf0 = scr("xf0", [T, D], F32)
    u1_fm = scr("u1_fm", [256 + 1536, T], F32)
    u1_tm = scr("u1_tm", [T, 1544], F32)
    yc_fm = scr("yc_fm", [256, T], BF16)
    yd_tm = scr("yd_tm", [T, 768], BF16)
    xm1 = scr("xm1", [T, D], F32)
    with ExitStack() as st:
        P = Prog(nc, st, T)
        P.make_masks(st)
        P.phase_proj(x_d, p["norm_mix"][0], p["w_in_even"][0],
                     [(0, 1024, "fm", u0_fm[0:1024, :]), (1024, 1024, "tm", u0_tm), (2048, 1696, "fm", u0_fm[1024:, :])])
        P.phase_hgrn2(u0_fm[0:512, :], u0_fm[512:1024, :], u0_tm[:, 0:512], u0_tm[:, 512:1024],
                      p["lb_table"], p["a_norm"][0], y0_tm[:, 0:512])
        P.phase_rwkv7(u0_fm[1024:, :], p["b_mu"][0], p["b_w0"][0], p["b_w2"][0], p["b_a0"][0], p["b_a2"][0], p["b_g2"][0],
                      p["b_kk"][0], p["b_ka"][0], p["b_rk"][0], p["b_ln_w"][0], p["b_ln_b"][0], y0_tm[:, 512:1024])
        P.phase_outproj(x_d, [("tm", y0_tm, 0, 8)], p["w_out_even"][0], xm0)
        P.phase_ffn(xm0, p["norm_ffn"][0], p["ffn_gate"][0], p["ffn_up"][0], p["ffn_down"][0], xf0)
        P.phase_proj(xf0, p["norm_mix"][1], p["w_in_odd"][0],
                     [(0, 256 + 1536, "fm", u1_fm), (256 + 1536, 1544, "tm", u1_tm)])
        P.phase_s5(u1_fm[0:256, :], p["c_lam_re"][0], p["c_lam_im"][0], p["c_log_step"][0], p["c_b_re"][0], p["c_b_im"][0],
                   p["c_c_re"][0], p["c_c_im"][0], p["c_d"][0], p["c_glu_w"][0], p["c_glu_b"][0], yc_fm)
        P.phase_mlstm(u1_fm[256:1024, :], u1_fm[1024:1792, :], u1_tm[:, 0:768], u1_tm[:, 768:1536], u1_tm[:, 1536:1544],
                      p["d_conv_q"][0], p["d_conv_k"][0], p["d_i_bias"][0], p["d_f_bias"][0], p["d_norm"][0], yd_tm)
        P.phase_outproj(xf0, [("fm", yc_fm, 0, 2), ("tm", yd_tm, 2, 6)], p["w_out_odd"][0], xm1)
        P.phase_ffn(xm1, p["norm_ffn"][1], p["ffn_gate"][1], p["ffn_up"][1], p["ffn_down"][1], out_d, gfin_row=p["norm_final"])
    return nc


def kernel(**inputs):
    x = np.asarray(inputs["x"], dtype=np.float32)
    B, T, _ = x.shape
    params = {n: np.ascontiguousarray(np.asarray(inputs[n], dtype=np.float32)) for n in PARAM_NAMES}
    shapes = {n: params[n].shape for n in PARAM_NAMES}
    nc = build(T, shapes)
    in_maps = []
    for b in range(B):
        m = {"x": np.ascontiguousarray(x[b])}
        m.update(params)
        in_maps.append(m)
    res = run_bass_kernel_spmd(nc, in_maps, core_ids=list(range(B)))
    return np.stack([np.asarray(r["out"], dtype=np.float32) for r in res.results], axis=0)
```
